# Optimizing a Trainium2 kernel written in Bass

```python
import math
import jax, jax.numpy as jnp
from jax import lax
import numpy as np

D_MODEL = 1024
BATCH = 8
SEQ = 8192
DEPTH = 1

HEAD_DIM = 64
SB_HEADS = D_MODEL // 128
NSA_Q_HEADS = D_MODEL // 128
NSA_KV_GROUPS = 2
SB_W = SB_HEADS * HEAD_DIM
NSA_Q_W = NSA_Q_HEADS * HEAD_DIM
NSA_KV_W = NSA_KV_GROUPS * HEAD_DIM
N_BRANCHES = 2
IN_W = 3 * SB_W + NSA_Q_W + 6 * NSA_KV_W + 3 * NSA_Q_HEADS + N_BRANCHES * D_MODEL
Q_BLOCK = 128
CMP_BLOCK = 32
CMP_STRIDE = 16
SEL_BLOCK = 64
SEL_TOPK = 16
WINDOW = 512
ROPE_THETA = 10000.0
N_EXPERTS = 32
TOP_K = 4
D_FF = D_MODEL
SWIGLU_LIMIT = 7.0
SWIGLU_ALPHA = 1.702
MOE_CHUNK = 256
LN_EPS = 1e-5
NEG_INF = -1e30
FORCE_SCORE = 1e30
DEEPNORM_ALPHA = (2.0 * DEPTH) ** 0.25
DEEPNORM_BETA = (8.0 * DEPTH) ** -0.25

kernel_name = "hybrid_stickbreak_nsa_moe_block"


def layer_norm(x, g, b):
    xf = x.astype(jnp.float32)
    mu = xf.mean(-1, keepdims=True)
    var = jnp.square(xf - mu).mean(-1, keepdims=True)
    return ((xf - mu) * lax.rsqrt(var + LN_EPS) * g.astype(jnp.float32) + b.astype(jnp.float32)).astype(x.dtype)


def rope(x, pos):
    half = HEAD_DIM // 2
    inv_freq = ROPE_THETA ** (-jnp.arange(half, dtype=jnp.float32) / half)
    ang = pos.astype(jnp.float32)[:, None] * inv_freq[None, :]
    cos = jnp.cos(ang).astype(x.dtype)
    sin = jnp.sin(ang).astype(x.dtype)
    x1, x2 = x[..., :half], x[..., half:]
    return jnp.concatenate([x1 * cos - x2 * sin, x2 * cos + x1 * sin], axis=-1)


def masked_softmax(s, mask):
    p = jax.nn.softmax(jnp.where(mask, s, NEG_INF), axis=-1)
    return jnp.where(mask, p, 0.0)


def stick_breaking_attention(q, k, v):
    B, H, S, dh = q.shape
    nblk = S // Q_BLOCK
    qb = q.reshape(B, H, nblk, Q_BLOCK, dh).transpose(2, 0, 1, 3, 4)
    kpos = jnp.arange(S)
    scale = dh ** -0.5

    def block(args):
        qi, i = args
        qpos = i * Q_BLOCK + jnp.arange(Q_BLOCK)
        z = jnp.einsum('bhqd,bhkd->bhqk', qi, k).astype(jnp.float32) * scale
        past = kpos[None, :] < qpos[:, None]
        log_keep = jnp.where(past, jax.nn.log_sigmoid(-z), 0.0)
        tail = lax.cumsum(log_keep, axis=3, reverse=True) - log_keep
        a = jnp.where(past, jnp.exp(jax.nn.log_sigmoid(z) + tail), 0.0)
        return jnp.einsum('bhqk,bhkd->bhqd', a.astype(v.dtype), v)

    out = lax.map(block, (qb, jnp.arange(nblk)))
    return out.transpose(1, 2, 0, 3, 4).reshape(B, H, S, dh)


def compress_tokens(x, pe, w1, w2):
    B, G, S, dh = x.shape
    n_sub = CMP_BLOCK // CMP_STRIDE
    n_stride = S // CMP_STRIDE
    xs = x.reshape(B, G, n_stride, CMP_STRIDE, dh)
    blocks = jnp.concatenate([xs[:, :, j:n_stride - n_sub + 1 + j] for j in range(n_sub)], axis=3)
    blocks = blocks + pe
    flat = blocks.reshape(B, G, blocks.shape[2], CMP_BLOCK * dh)
    return jax.nn.gelu(flat @ w1) @ w2


def nsa_attention(q, kc, vc, ks, vs, kw, vw, gates):
    B, Hq, S, dh = q.shape
    G = ks.shape[1]
    R = Hq // G
    nblk = S // Q_BLOCK
    n_sel = S // SEL_BLOCK
    ratio = SEL_BLOCK // CMP_STRIDE
    n_sub = CMP_BLOCK // CMP_STRIDE
    topk = min(SEL_TOPK, n_sel)
    scale = dh ** -0.5
    cmp_end = jnp.arange(kc.shape[2]) * CMP_STRIDE + CMP_BLOCK - 1
    ks_blk = ks.reshape(B, G, n_sel, SEL_BLOCK, dh)
    vs_blk = vs.reshape(B, G, n_sel, SEL_BLOCK, dh)
    kw_pad = jnp.pad(kw, ((0, 0), (0, 0), (WINDOW, 0), (0, 0)))
    vw_pad = jnp.pad(vw, ((0, 0), (0, 0), (WINDOW, 0), (0, 0)))
    qb = q.reshape(B, G, R, nblk, Q_BLOCK, dh).transpose(3, 0, 1, 2, 4, 5)
    gather = jax.vmap(jax.vmap(lambda blk, ix: blk[ix]))
    sel_j = jnp.arange(n_sel)

    def block(args):
        qi, i = args
        qpos = i * Q_BLOCK + jnp.arange(Q_BLOCK)
        sc = jnp.einsum('bgrqd,bgnd->bgrqn', qi, kc).astype(jnp.float32) * scale
        p_cmp = masked_softmax(sc, cmp_end[None, :] <= qpos[:, None])
        o_cmp = jnp.einsum('bgrqn,bgnd->bgrqd', p_cmp.astype(vc.dtype), vc)
        imp = jnp.pad(p_cmp.sum(axis=2), ((0, 0), (0, 0), (0, 0), (n_sub - 1, n_sub - 1)))
        p_slc = sum(imp[..., m:m + n_sel * ratio:ratio] for m in range(ratio + n_sub - 1))
        blk_t = qpos // SEL_BLOCK
        visible = sel_j[None, :] <= blk_t[:, None]
        forced = (sel_j[None, :] == 0) | (sel_j[None, :] == blk_t[:, None]) | (sel_j[None, :] == blk_t[:, None] - 1)
        score = jnp.where(forced, FORCE_SCORE, jnp.where(visible, p_slc, NEG_INF))
        _, idx = lax.top_k(score, topk)
        k_sel = gather(ks_blk, idx).reshape(B, G, Q_BLOCK, topk * SEL_BLOCK, dh)
        v_sel = gather(vs_blk, idx).reshape(B, G, Q_BLOCK, topk * SEL_BLOCK, dh)
        sel_pos = (idx[..., None] * SEL_BLOCK + jnp.arange(SEL_BLOCK)).reshape(B, G, Q_BLOCK, topk * SEL_BLOCK)
        smask = (sel_pos <= qpos[None, None, :, None])[:, :, None]
        ss = jnp.einsum('bgrqd,bgqnd->bgrqn', qi, k_sel).astype(jnp.float32) * scale
        o_sel = jnp.einsum('bgrqn,bgqnd->bgrqd', masked_softmax(ss, smask).astype(v_sel.dtype), v_sel)
        kwin = lax.dynamic_slice_in_dim(kw_pad, i * Q_BLOCK, WINDOW + Q_BLOCK, axis=2)
        vwin = lax.dynamic_slice_in_dim(vw_pad, i * Q_BLOCK, WINDOW + Q_BLOCK, axis=2)
        wpos = i * Q_BLOCK - WINDOW + jnp.arange(WINDOW + Q_BLOCK)
        wmask = (wpos[None, :] >= 0) & (wpos[None, :] <= qpos[:, None]) & (qpos[:, None] - wpos[None, :] < WINDOW)
        sw = jnp.einsum('bgrqd,bgkd->bgrqk', qi, kwin).astype(jnp.float32) * scale
        o_win = jnp.einsum('bgrqk,bgkd->bgrqd', masked_softmax(sw, wmask).astype(vwin.dtype), vwin)
        return jnp.stack([o_cmp, o_sel, o_win])

    out = lax.map(block, (qb, jnp.arange(nblk)))
    out = out.transpose(1, 2, 3, 4, 0, 5, 6).reshape(3, B, Hq, S, dh)
    g = gates.transpose(3, 0, 2, 1)[..., None].astype(out.dtype)
    return (g * out).sum(axis=0)


def clamped_swiglu(gu):
    gate, up = jnp.split(gu, 2, axis=-1)
    gate = jnp.minimum(gate, SWIGLU_LIMIT)
    up = jnp.clip(up, -SWIGLU_LIMIT, SWIGLU_LIMIT)
    return gate * jax.nn.sigmoid(SWIGLU_ALPHA * gate) * (up + 1.0)


def moe_ffn(x, w_router, b_router, w_gate_up, b_gate_up, w_down, b_down):
    B, S, D = x.shape
    xt = x.reshape(-1, D)
    N = xt.shape[0]
    M = N * TOP_K
    logits = (xt @ w_router + b_router).astype(jnp.float32)
    top_vals, top_idx = lax.top_k(logits, TOP_K)
    gate_w = jax.nn.softmax(top_vals, axis=-1)
    e_flat = top_idx.reshape(M)
    order = jnp.argsort(e_flat)
    e_sorted = e_flat[order]
    tok_sorted = order // TOP_K
    w_sorted = gate_w.reshape(M)[order]
    counts = jax.ops.segment_sum(jnp.ones((M,), jnp.int32), e_flat, num_segments=N_EXPERTS)
    padded = (counts + MOE_CHUNK - 1) // MOE_CHUNK * MOE_CHUNK
    starts = jnp.cumsum(counts) - counts
    pends = jnp.cumsum(padded)
    pstarts = pends - padded
    dest = pstarts[e_sorted] + (jnp.arange(M) - starts[e_sorted])
    n_chunks = -(-M // MOE_CHUNK) + N_EXPERTS
    buf = jnp.zeros((n_chunks * MOE_CHUNK, D), x.dtype).at[dest].set(xt[tok_sorted])
    chunk_expert = jnp.clip(jnp.searchsorted(pends, jnp.arange(n_chunks) * MOE_CHUNK, side='right'), 0, N_EXPERTS - 1)

    def expert_block(args):
        xc, e = args
        h = clamped_swiglu(xc @ w_gate_up[e] + b_gate_up[e])
        return h @ w_down[e] + b_down[e]

    out = lax.map(expert_block, (buf.reshape(n_chunks, MOE_CHUNK, D), chunk_expert)).reshape(-1, D)
    y = jax.ops.segment_sum(out[dest] * w_sorted[:, None].astype(out.dtype), tok_sorted, num_segments=N)
    return y.reshape(B, S, D)


def setup_inputs(seed: int = 0) -> dict:
    key = jax.random.key(seed)
    ks = jax.random.split(key, 24)
    L = DEPTH
    flat = CMP_BLOCK * HEAD_DIM

    def nrm(k, shape, scale):
        return jax.random.normal(k, shape, jnp.float32) * scale

    return {
        "x": nrm(ks[0], (BATCH, SEQ, D_MODEL), 1.0),
        "w_in": nrm(ks[1], (L, D_MODEL, IN_W), D_MODEL ** -0.5),
        "cmp_pe_k": nrm(ks[2], (L, CMP_BLOCK, HEAD_DIM), 0.1),
        "cmp_w1_k": nrm(ks[3], (L, flat, HEAD_DIM), flat ** -0.5),
        "cmp_w2_k": nrm(ks[4], (L, HEAD_DIM, HEAD_DIM), HEAD_DIM ** -0.5),
        "cmp_pe_v": nrm(ks[5], (L, CMP_BLOCK, HEAD_DIM), 0.1),
        "cmp_w1_v": nrm(ks[6], (L, flat, HEAD_DIM), flat ** -0.5),
        "cmp_w2_v": nrm(ks[7], (L, HEAD_DIM, HEAD_DIM), HEAD_DIM ** -0.5),
        "w_proj_sb": nrm(ks[8], (L, SB_W, D_MODEL), SB_W ** -0.5),
        "w_proj_nsa": nrm(ks[9], (L, NSA_Q_W, D_MODEL), NSA_Q_W ** -0.5),
        "w_out": nrm(ks[10], (L, D_MODEL, D_MODEL), D_MODEL ** -0.5 * DEEPNORM_BETA),
        "ln1_g": 1.0 + nrm(ks[11], (L, D_MODEL), 0.01),
        "ln1_b": nrm(ks[12], (L, D_MODEL), 0.01),
        "w_router": nrm(ks[13], (L, D_MODEL, N_EXPERTS), D_MODEL ** -0.5),
        "b_router": nrm(ks[14], (L, N_EXPERTS), 0.01),
        "w_gate_up": nrm(ks[15], (L, N_EXPERTS, D_MODEL, 2 * D_FF), D_MODEL ** -0.5),
        "b_gate_up": nrm(ks[16], (L, N_EXPERTS, 2 * D_FF), 0.01),
        "w_down": nrm(ks[17], (L, N_EXPERTS, D_FF, D_MODEL), D_FF ** -0.5 * DEEPNORM_BETA),
        "b_down": nrm(ks[18], (L, N_EXPERTS, D_MODEL), 0.01),
        "ln2_g": 1.0 + nrm(ks[19], (L, D_MODEL), 0.01),
        "ln2_b": nrm(ks[20], (L, D_MODEL), 0.01),
    }


def reference(x, w_in, cmp_pe_k, cmp_w1_k, cmp_w2_k, cmp_pe_v, cmp_w1_v, cmp_w2_v,
              w_proj_sb, w_proj_nsa, w_out, ln1_g, ln1_b, w_router, b_router,
              w_gate_up, b_gate_up, w_down, b_down, ln2_g, ln2_b):
    B, S, D = x.shape
    pos = jnp.arange(S, dtype=jnp.int32)
    sizes = [SB_W, SB_W, SB_W, NSA_Q_W] + [NSA_KV_W] * 6 + [3 * NSA_Q_HEADS, N_BRANCHES * D_MODEL]
    split_at = [int(c) for c in np.cumsum(sizes)[:-1]]

    def heads(t, n):
        return t.reshape(B, S, n, HEAD_DIM).transpose(0, 2, 1, 3)

    def merge_heads(t):
        return t.transpose(0, 2, 1, 3).reshape(B, S, -1)

    h = x
    for l in range(DEPTH):
        proj = h @ w_in[l]
        sb_q, sb_k, sb_v, n_q, k_c, v_c, k_s, v_s, k_w, v_w, n_g, m_g = jnp.split(proj, split_at, axis=-1)
        y_sb = stick_breaking_attention(heads(sb_q, SB_HEADS), heads(sb_k, SB_HEADS), heads(sb_v, SB_HEADS))
        q_n = rope(heads(n_q, NSA_Q_HEADS), pos)
        kc = compress_tokens(rope(heads(k_c, NSA_KV_GROUPS), pos), cmp_pe_k[l], cmp_w1_k[l], cmp_w2_k[l])
        vc = compress_tokens(heads(v_c, NSA_KV_GROUPS), cmp_pe_v[l], cmp_w1_v[l], cmp_w2_v[l])
        ks_ = rope(heads(k_s, NSA_KV_GROUPS), pos)
        kw_ = rope(heads(k_w, NSA_KV_GROUPS), pos)
        nsa_gates = jax.nn.sigmoid(n_g).reshape(B, S, NSA_Q_HEADS, 3)
        y_nsa = nsa_attention(q_n, kc, vc, ks_, heads(v_s, NSA_KV_GROUPS), kw_, heads(v_w, NSA_KV_GROUPS), nsa_gates)
        mg = jax.nn.sigmoid(m_g).reshape(B, S, N_BRANCHES, D)
        merged = mg[:, :, 0] * (merge_heads(y_sb) @ w_proj_sb[l]) + mg[:, :, 1] * (merge_heads(y_nsa) @ w_proj_nsa[l])
        h = layer_norm(DEEPNORM_ALPHA * h + merged @ w_out[l], ln1_g[l], ln1_b[l])
        y_moe = moe_ffn(h, w_router[l], b_router[l], w_gate_up[l], b_gate_up[l], w_down[l], b_down[l])
        h = layer_norm(DEEPNORM_ALPHA * h + y_moe, ln2_g[l], ln2_b[l])
    return h
```

```python
import math
from contextlib import ExitStack

import numpy as np
import concourse.bass as bass
import concourse.mybir as mybir
from concourse.bass_utils import run_bass_kernel_spmd

F32 = mybir.dt.float32
BF16 = mybir.dt.bfloat16
I32 = mybir.dt.int32
U32 = mybir.dt.uint32
AF = mybir.ActivationFunctionType
ALU = mybir.AluOpType
AX = mybir.AxisListType

D = 1024
HD = 64
NH = 8
NEXP = 32
IN_W = 4888
ALPHA = 2.0 ** 0.25
EPS = 1e-5
NEGB = 30000.0


class Eng:
    def __init__(self, name, sem, is_pe=False):
        self.name = name
        self.sem = sem
        self.n = 0
        self.is_pe = is_pe
        self.prog = []
        self.waited = {}
        self.dma_i = 0


class TT:
    def __init__(self, ap):
        self.ap = ap
        self.w = None
        self.r = {}

    def __getitem__(self, idx):
        return self.ap[idx]


class KB:
    def __init__(self, nc, es):
        self.nc = nc
        self.es = es
        sem = lambda n: es.enter_context(nc.semaphore(n))
        self.pe = Eng("tensor", sem("s_pe"), True)
        self.act = Eng("scalar", sem("s_act"))
        self.dve = Eng("vector", sem("s_dve"))
        self.pool = Eng("gpsimd", sem("s_pool"))
        self.sp = Eng("sync", sem("s_sp"))
        self.engs = [self.pe, self.act, self.dve, self.pool, self.sp]
        NS = 6
        self.hw_sems = {e.name: [[sem(f"dh_{e.name}{i}"), 0] for i in range(NS)] for e in (self.sp, self.act)}
        self.sw_sems = [[sem(f"dsw{i}"), 0] for i in range(NS)]
        self.dma_out = {}
        self.sems_by_id = {}

    def _wait(self, eng, need):
        for sid, (s, v) in need.items():
            if eng.is_pe and s is eng.sem:
                continue
            if eng.waited.get(sid, 0) >= v:
                continue
            eng.waited[sid] = v
            eng.prog.append(lambda h, s=s, v=v: h.wait_ge(s, v))

    def _deps(self, reads, writes):
        need = {}

        def add(tok):
            if tok is None:
                return
            s, v = tok
            if id(s) not in need or need[id(s)][1] < v:
                need[id(s)] = (s, v)

        for t in reads:
            add(t.w)
        for t in writes:
            add(t.w)
            for tok in t.r.values():
                add(tok)
        return need

    def _mark(self, tok, reads, writes):
        for t in reads:
            t.r[id(tok[0])] = tok
        for t in writes:
            t.w = tok
            t.r = {}

    def op(self, eng, name, reads=(), writes=(), **kw):
        self._wait(eng, self._deps(reads, writes))
        eng.n += 1
        s = eng.sem
        eng.prog.append(lambda h, name=name, kw=kw, s=s: getattr(h, name)(**kw).then_inc(s, 1))
        tok = (s, eng.n)
        self._mark(tok, reads, writes)
        return tok

    def mm(self, out, lhsT, rhs, start, stop, reads, writes):
        return self.op(self.pe, "matmul", reads, writes, out=out, lhsT=lhsT, rhs=rhs, start=start, stop=stop)

    def actv(self, out, in_, func, reads, writes, **kw):
        return self.op(self.act, "activation", reads, writes, out=out, in_=in_, func=func, **kw)

    def tt(self, eng, out, in0, in1, op, reads, writes):
        return self.op(eng, "tensor_tensor", reads, writes, out=out, in0=in0, in1=in1, op=op)

    def ts(self, eng, out, in0, s1, s2, op0, op1, reads, writes, **kw):
        return self.op(eng, "tensor_scalar", reads, writes, out=out, in0=in0, scalar1=s1, scalar2=s2, op0=op0, op1=op1, **kw)

    def cp(self, eng, out, in_, reads, writes):
        return self.op(eng, "tensor_copy", reads, writes, out=out, in_=in_)

    def ms(self, eng, ap, val, writes):
        return self.op(eng, "memset", (), writes, ap=ap, constant=val)

    def dma(self, eng, out, in_, reads=(), writes=(), **kw):
        pool_ = self.sw_sems if eng is self.pool else self.hw_sems[eng.name]
        slot = pool_[eng.dma_i % len(pool_)]
        eng.dma_i += 1
        s = slot[0]
        need = self._deps(reads, writes)
        if slot[1] > 0:
            need[id(s)] = (s, slot[1])
        self._wait(eng, need)
        slot[1] += 16
        v = slot[1]
        eng.prog.append(lambda h, out=out, in_=in_, s=s, kw=kw: h.dma_start(out=out, in_=in_, **kw).then_inc(s, 16))
        tok = (s, v)
        self.dma_out[id(s)] = tok
        self._mark(tok, reads, writes)
        return tok

    def idma(self, out, out_off, in_, in_off, reads=(), writes=(), **kw):
        eng = self.pool
        slot = self.sw_sems[eng.dma_i % len(self.sw_sems)]
        eng.dma_i += 1
        s = slot[0]
        need = self._deps(reads, writes)
        if slot[1] > 0:
            need[id(s)] = (s, slot[1])
        self._wait(eng, need)
        slot[1] += 16
        v = slot[1]
        eng.prog.append(lambda h, out=out, out_off=out_off, in_=in_, in_off=in_off, kw=kw, s=s: h.indirect_dma_start(
            out=out, out_offset=out_off, in_=in_, in_offset=in_off, **kw).then_inc(s, 16))
        tok = (s, v)
        self.dma_out[id(s)] = tok
        self._mark(tok, reads, writes)
        return tok

    def barrier(self):
        need = {}
        for e in self.engs:
            if e.n > 0:
                need[id(e.sem)] = (e.sem, e.n)
        for sid, tok in self.dma_out.items():
            need[sid] = tok
        for e in self.engs:
            n2 = {k: v for k, v in need.items() if v[0] is not e.sem}
            saved = e.is_pe
            e.is_pe = False
            self._wait(e, n2)
            e.is_pe = saved

    def emit(self):
        nc = self.nc
        with nc.Block() as block:
            for e in self.engs:
                def body(h, e=e):
                    for f in e.prog:
                        f(h)
                getattr(block, e.name)(body)

    def sb(self, st, name, shape, dt):
        return st.enter_context(self.nc.sbuf_tensor(name, list(shape), dt))

    def ps(self, st, name, shape, dt):
        return st.enter_context(self.nc.psum_tensor(name, list(shape), dt))


class Ring:
    def __init__(self, items):
        self.items = items
        self.i = 0

    def next(self):
        t = self.items[self.i % len(self.items)]
        self.i += 1
        return t


def host_consts(S):
    c = {}
    c["ident"] = np.eye(128, dtype=np.float32)
    half = HD // 2
    inv_freq = (np.float32(10000.0) ** (-np.arange(half, dtype=np.float32) / np.float32(half))).astype(np.float32)
    ang = np.arange(S, dtype=np.float32)[:, None] * inv_freq[None, :]
    cos = np.cos(ang).astype(np.float32).T
    sin = np.sin(ang).astype(np.float32).T
    cos64 = np.concatenate([cos, cos], 0)
    sin64 = np.concatenate([-sin, sin], 0)
    c["cosT"] = np.ascontiguousarray(np.concatenate([cos64, cos64], 0))
    c["sinT"] = np.ascontiguousarray(np.concatenate([sin64, sin64], 0))
    j = np.arange(128)
    c["negU8"] = np.where(j[:, None] >= j[None, :], -8.0, 0.0).astype(np.float32)
    c["neg8"] = np.full((128, 128), -8.0, np.float32)
    m4 = np.zeros((128, 4, 4, 128), np.float32)
    strict = (j[:, None] < j[None, :]).astype(np.float32)
    for jb in range(4):
        for cb in range(4):
            if cb == jb:
                m4[:, jb, cb, :] = strict
            elif cb > jb:
                m4[:, jb, cb, :] = 1.0
    c["mask4"] = m4.reshape(128, 4 * 512)
    le = np.where(j[:, None] <= j[None, :], 0.0, -NEGB).astype(np.float32)
    gt = np.where(j[:, None] > j[None, :], 0.0, -NEGB).astype(np.float32)
    c["bias_le"] = np.tile(le, (1, 4))
    c["bias_gt"] = np.tile(gt, (1, 4))
    nsel = S // 64
    E = np.zeros((128, S), np.float32)
    E[np.arange(S) // 64, np.arange(S)] = 1.0
    c["Esel"] = E
    m = np.arange(-1, 7)
    c["cmask"] = (j[:, None] >= 16 * m[None, :] + 31).astype(np.float32)
    c["iota32"] = np.tile(np.arange(NEXP, dtype=np.float32)[None, :], (128, 1))
    c["ustrict"] = (j[:, None] < j[None, :]).astype(np.float32)
    c["ones"] = np.ones((128, 128), np.float32)
    sm = np.zeros((128, 64), np.float32)
    sm[64, :] = 1.0
    c["selM"] = sm
    return c


CONST_SHAPES = lambda S: {
    "ident": (128, 128), "cosT": (128, S), "sinT": (128, S), "negU8": (128, 128), "neg8": (128, 128),
    "mask4": (128, 2048), "bias_le": (128, 512), "bias_gt": (128, 512), "Esel": (128, S),
    "cmask": (128, 8), "iota32": (128, NEXP), "ustrict": (128, 128), "ones": (128, 128), "selM": (128, 64),
}

def win_layout():
    def sw(base, nheads):
        idx = []
        for h in range(nheads):
            for d in range(HD):
                idx.append(base + h * HD + (d + 32) % HD)
        return idx

    r = lambda a, b: list(range(a, b))
    fm = []
    fm += r(0, 512)
    fm += r(512, 1024)
    fm += r(1536, 2048)
    fm += sw(1536, 8)
    fm += r(2048, 2176) + sw(2048, 2)
    fm += r(2304, 2432) + sw(2304, 2)
    fm += r(2560, 2688) + sw(2560, 2)
    fm += r(2176, 2304)
    fm += r(2840, 4888)
    fm += r(2816, 2840)
    tm = r(1024, 1536) + r(2432, 2560) + r(2688, 2816)
    return np.array(fm), np.array(tm)


NFM = 5016
NTM = 768


def build(S=8192, CAP=1536, dbg=(), stop_after=99, skip=()):
    NTB = S // 128
    NG = S // 512
    NC = S // 16 - 1
    NSEL = S // 64
    NROWS = NEXP * CAP + 128
    TRASH = NEXP * CAP
    nc = bass.Bass("TRN2", target_bir_lowering=False)

    def din(name, shape, dt=F32):
        return nc.dram_tensor(name, list(shape), dt, kind="ExternalInput").ap()

    def dscr(name, shape, dt):
        kind = "ExternalOutput" if name in dbg else "Internal"
        return nc.dram_tensor(name, list(shape), dt, kind=kind).ap()

    x = din("x", (S, D))
    wfm = din("wfm", (D, NFM))
    wtm = din("wtm", (D, NTM))
    cst = {k: din("c_" + k, shp) for k, shp in CONST_SHAPES(S).items()}
    cmp_in = {k: din(k, shp) for k, shp in dict(
        cmp_pe_k=(32, 64), cmp_w1_k=(2048, 64), cmp_w2_k=(64, 64),
        cmp_pe_v=(32, 64), cmp_w1_v=(2048, 64), cmp_w2_v=(64, 64)).items()}
    w_proj_sb = din("w_proj_sb", (512, D))
    w_proj_nsa = din("w_proj_nsa", (512, D))
    w_out = din("w_out", (D, D))
    ln1_g = din("ln1_g", (1, D)); ln1_b = din("ln1_b", (1, D))
    ln2_g = din("ln2_g", (1, D)); ln2_b = din("ln2_b", (1, D))
    w_router = din("w_router", (D, NEXP)); b_router = din("b_router", (1, NEXP))
    w_gate_up = din("w_gate_up", (NEXP, D, 2 * D)); b_gate_up = din("b_gate_up", (NEXP, 2 * D))
    w_down = din("w_down", (NEXP, D, D)); b_down = din("b_down", (NEXP, D))
    out = nc.dram_tensor("out", [S, D], F32, kind="ExternalOutput").ap()

    QT = dscr("QT", (512, S), BF16); KT = dscr("KT", (512, S), BF16)
    NQT = dscr("NQT", (512, S), BF16)
    KCT = dscr("KCT", (128, S), BF16); VCT = dscr("VCT", (128, S), BF16)
    KST = dscr("KST", (128, S), BF16); KWT = dscr("KWT", (128, S), BF16)
    NGT = dscr("NGT", (24, S), F32); MGT = dscr("MGT", (2048, S), BF16)
    VTM = dscr("VTM", (S, NTM), BF16)
    KCC = dscr("KCC", (2, 64, 512), BF16); VCC = dscr("VCC", (512, 2, 64), BF16)
    YST = dscr("YST", (512, S), BF16); YNT = dscr("YNT", (512, S), BF16)
    H1 = dscr("H1", (S, D), F32)
    XBUF = dscr("XBUF", (NROWS, D), BF16)
    OBUF = dscr("OBUF", (NROWS, D), F32)

    with ExitStack() as es:
        k = KB(nc, es)
        pe, act, dve, pool, sp = k.pe, k.act, k.dve, k.pool, k.sp
        identb = TT(k.sb(es, "identb", (128, 128), BF16))
        identf = TT(k.sb(es, "identf", (128, 128), F32))
        destt = TT(k.sb(es, "destt", (128, NTB, 4), I32))
        w4t = TT(k.sb(es, "w4t", (128, NTB, 4), F32))
        k.dma(pool, identb[:], cst["ident"], writes=[identb])
        k.dma(sp, identf[:], cst["ident"], writes=[identf])

        def evac(i, out_ap, in_ap, reads, writes):
            if i % 2 == 0:
                return k.actv(out_ap, in_ap, AF.Copy, reads, writes)
            return k.cp(dve, out_ap, in_ap, reads, writes)

        if stop_after >= 1:
            with ExitStack() as st:
                zt = TT(k.sb(st, "zt", (128, 4, D), BF16))
                zf = TT(k.sb(st, "zf", (128, D), F32))
                k.ms(pool, zt[:], 0.0, [zt])
                k.ms(pool, zf[:], 0.0, [zf])
                k.dma(act, OBUF[TRASH:TRASH + 128, :], zf[:], reads=[zf])
                nblk = NROWS // 128
                for b0 in range(0, nblk, 4):
                    nb = min(4, nblk - b0)
                    k.dma(act, XBUF[b0 * 128:(b0 + nb) * 128, :].rearrange("(b p) d -> p b d", p=128), zt[:, 0:nb, :], reads=[zt])
                wfb = TT(k.sb(st, "wfb", (128, 8, NFM), BF16))
                wtb = TT(k.sb(st, "wtb", (128, 8, NTM), BF16))
                for kc in range(8):
                    k.dma(pool, wfb[:, kc, :], wfm[kc * 128:(kc + 1) * 128, :], writes=[wfb])
                    k.dma(pool, wtb[:, kc, :], wtm[kc * 128:(kc + 1) * 128, :], writes=[wtb])
                xbs = Ring([TT(k.sb(st, f"xb{i}", (128, 4, D), BF16)) for i in range(2)])
                xTs = Ring([TT(k.sb(st, f"xT{i}", (128, 8, 512), BF16)) for i in range(2)])
                cosr = Ring([TT(k.sb(st, f"cos{i}", (128, 512), F32)) for i in range(2)])
                sinr = Ring([TT(k.sb(st, f"sin{i}", (128, 512), F32)) for i in range(2)])
                obr = Ring([TT(k.sb(st, f"ob{i}", (128, 512), BF16)) for i in range(6)])
                ofr = Ring([TT(k.sb(st, f"of{i}", (128, 512), F32)) for i in range(4)])
                otr = Ring([TT(k.sb(st, f"ot{i}", (128, NTM), BF16)) for i in range(2)])
                ptr = Ring([TT(k.ps(st, f"ptr{i}", (128, 8, 128), BF16)) for i in range(2)])
                accr = Ring([TT(k.ps(st, f"acc{i}", (128, 512), F32)) for i in range(6)])
                ev = 0
                for g in range(NG):
                    xb = xbs.next(); xT = xTs.next(); cs = cosr.next(); sn = sinr.next()
                    gs = slice(g * 512, (g + 1) * 512)
                    k.dma(pool, xb[:], x[gs, :].rearrange("(tb p) d -> p tb d", p=128), writes=[xb])
                    k.dma(sp, cs[:], cst["cosT"][:, gs], writes=[cs])
                    k.dma(sp, sn[:], cst["sinT"][:, gs], writes=[sn])
                    for tb in range(4):
                        pt = ptr.next()
                        for kc in range(8):
                            k.op(pe, "transpose", [xb, identb], [pt], out=pt[:, kc, :],
                                 in_=xb[:, tb, kc * 128:(kc + 1) * 128], identity=identb[:])
                        evac(ev, xT[:, :, tb * 128:(tb + 1) * 128], pt[:], [pt], [xT]); ev += 1

                    def fm_mm(chunk, xT, width=128):
                        acc = accr.next()
                        for kc in range(8):
                            k.mm(acc[0:width, :], wfb[:, kc, chunk * 128:chunk * 128 + width], xT[:, kc, :],
                                 kc == 0, kc == 7, [wfb, xT], [acc])
                        return acc

                    for chunk, dst, r0 in [(c, QT, c * 128) for c in range(4)] + [(4 + c, KT, c * 128) for c in range(4)] + [(22, VCT, 0)]:
                        acc = fm_mm(chunk, xT)
                        ob = obr.next()
                        evac(ev, ob[:], acc[:], [acc], [ob]); ev += 1
                        k.dma(sp, dst[r0:r0 + 128, gs], ob[:], reads=[ob])
                    for ca, cb_, dst, r0 in [(8 + c, 12 + c, NQT, c * 128) for c in range(4)] + [(16, 17, KCT, 0), (18, 19, KST, 0), (20, 21, KWT, 0)]:
                        a = fm_mm(ca, xT); b = fm_mm(cb_, xT)
                        t1 = ofr.next(); t2 = ofr.next(); ob = obr.next()
                        k.tt(dve, t1[:], a[:], cs[:], ALU.mult, [a, cs], [t1])
                        k.tt(dve, t2[:], b[:], sn[:], ALU.mult, [b, sn], [t2])
                        k.tt(pool, ob[:], t1[:], t2[:], ALU.add, [t1, t2], [ob])
                        k.dma(sp, dst[r0:r0 + 128, gs], ob[:], reads=[ob])
                    for c in range(16):
                        acc = fm_mm(23 + c, xT)
                        ob = obr.next()
                        k.actv(ob[:], acc[:], AF.Sigmoid, [acc], [ob])
                        k.dma(sp, MGT[c * 128:(c + 1) * 128, gs], ob[:], reads=[ob])
                    acc = fm_mm(39, xT, 24)
                    of = ofr.next()
                    k.actv(of[0:24, :], acc[0:24, :], AF.Sigmoid, [acc], [of])
                    k.dma(sp, NGT[0:24, gs], of[0:24, :], reads=[of])
                    for tb in range(4):
                        ot = otr.next()
                        for (c0, c1) in ((0, 512), (512, 768)):
                            acc = accr.next()
                            for kc in range(8):
                                k.mm(acc[:, 0:c1 - c0], xT[:, kc, tb * 128:(tb + 1) * 128], wtb[:, kc, c0:c1],
                                     kc == 0, kc == 7, [wtb, xT], [acc])
                            evac(ev, ot[:, c0:c1], acc[:, 0:c1 - c0], [acc], [ot]); ev += 1
                        r0 = g * 512 + tb * 128
                        k.dma(sp, VTM[r0:r0 + 128, :], ot[:], reads=[ot])
                k.barrier()

        if stop_after >= 2 and 2 not in skip:
            with ExitStack() as st:
                w1 = TT(k.sb(st, "cw1", (64, 32, 64), BF16))
                w2 = TT(k.sb(st, "cw2", (64, 64), BF16))
                pe_f = TT(k.sb(st, "cpe", (32, 64), F32))
                peT = TT(k.sb(st, "cpeT", (64, 32), BF16))
                cbs = TT(k.sb(st, "ccb", (64, 1), F32))
                src = TT(k.sb(st, "csrc", (64, S), BF16))
                u = TT(k.sb(st, "cu", (64, 512), F32))
                u2 = TT(k.sb(st, "cu2", (64, 512), F32))
                sg = TT(k.sb(st, "csg", (64, 512), F32))
                gl = TT(k.sb(st, "cgl", (64, 512), BF16))
                ko = TT(k.sb(st, "cko", (128, 512), BF16))
                hid = TT(k.ps(st, "chid", (64, 512), F32))
                pp = TT(k.ps(st, "cpp", (128, 512), F32))
                for which in ("k", "v"):
                    k.dma(pool, w1[:], cmp_in["cmp_w1_" + which].rearrange("(j d) h -> d j h", d=64), writes=[w1])
                    k.dma(pool, w2[:], cmp_in["cmp_w2_" + which], writes=[w2])
                    k.dma(sp, pe_f[:], cmp_in["cmp_pe_" + which], writes=[pe_f])
                    k.op(pe, "transpose", [pe_f, identf], [pp], out=pp[0:64, 0:32], in_=pe_f[:], identity=identf[0:32, 0:32])
                    k.cp(dve, peT[:], pp[0:64, 0:32], [pp], [peT])
                    for j in range(32):
                        k.mm(pp[0:64, 0:1], w1[:, j, :], peT[:, j:j + 1], j == 0, j == 31, [w1, peT], [pp])
                    k.cp(dve, cbs[:], pp[0:64, 0:1], [pp], [cbs])
                    for g in range(2):
                        k.dma(sp, src[:], (KCT if which == "k" else VCT)[g * 64:(g + 1) * 64, :], writes=[src])
                        for j in range(32):
                            k.mm(hid[:, 0:NC], w1[:, j, :], src[:, j:j + 16 * (NC - 1) + 1:16], j == 0, j == 31, [w1, src], [hid])
                        k.actv(u[:, 0:NC], hid[:, 0:NC], AF.Identity, [hid, cbs], [u], bias=cbs[:, 0:1])
                        k.tt(dve, u2[:, 0:NC], u[:, 0:NC], u[:, 0:NC], ALU.mult, [u], [u2])
                        k.ts(dve, u2[:, 0:NC], u2[:, 0:NC], 0.044715, 1.0, ALU.mult, ALU.add, [u2], [u2])
                        k.tt(dve, u2[:, 0:NC], u2[:, 0:NC], u[:, 0:NC], ALU.mult, [u2, u], [u2])
                        k.actv(sg[:, 0:NC], u2[:, 0:NC], AF.Sigmoid, [u2], [sg], scale=1.5957691216057308)
                        k.tt(dve, gl[:, 0:NC], u[:, 0:NC], sg[:, 0:NC], ALU.mult, [u, sg], [gl])
                        if which == "k":
                            k.mm(pp[0:64, 0:NC], w2[:], gl[:, 0:NC], True, True, [w2, gl], [pp])
                            k.cp(dve, ko[0:64, 0:NC], pp[0:64, 0:NC], [pp], [ko])
                            k.dma(sp, KCC[g, :, 0:NC], ko[0:64, 0:NC], reads=[ko])
                        else:
                            for c in range((NC + 127) // 128):
                                w = min(128, NC - c * 128)
                                k.mm(pp[0:w, 0:64], gl[:, c * 128:c * 128 + w], w2[:], True, True, [w2, gl], [pp])
                                k.cp(dve, ko[0:w, 0:64], pp[0:w, 0:64], [pp], [ko])
                                k.dma(sp, VCC[c * 128:c * 128 + w, g, :], ko[0:w, 0:64], reads=[ko])
                k.barrier()

        if stop_after >= 3 and 3 not in skip:
            with ExitStack() as st:
                NCH = (NC + 127) // 128
                esel = TT(k.sb(st, "esel", (128, S), BF16))
                ble = TT(k.sb(st, "ble", (128, 512), BF16))
                bgt = TT(k.sb(st, "bgt", (128, 512), BF16))
                cmask = TT(k.sb(st, "cmask", (128, 8), F32))
                selM = TT(k.sb(st, "selM", (128, 128), F32))
                k.dma(pool, esel[:], cst["Esel"], writes=[esel])
                k.dma(pool, ble[:], cst["bias_le"], writes=[ble])
                k.dma(pool, bgt[:], cst["bias_gt"], writes=[bgt])
                k.dma(sp, cmask[:], cst["cmask"], writes=[cmask])
                k.ms(pool, selM[:], 0.0, [selM])
                k.dma(sp, selM[:, 0:64], cst["selM"], writes=[selM])
                kst = TT(k.sb(st, "kst", (128, S), BF16))
                kwt = TT(k.sb(st, "kwt", (128, S), BF16))
                kcc = TT(k.sb(st, "kcc", (128, 512), BF16))
                vcc = TT(k.sb(st, "vcc", (128, NCH, 128), BF16))
                vs = TT(k.sb(st, "vs", (128, NTB, 128), BF16))
                vw = TT(k.sb(st, "vw", (128, NTB, 128), BF16))
                qtr = Ring([TT(k.sb(st, f"nq{i}", (128, 4, 128), BF16)) for i in range(3)])
                for t_ in qtr.items:
                    k.ms(pool, t_[64:128, :, :], 0.0, [t_])
                gtr = Ring([TT(k.sb(st, f"gt{i}", (65, 3, 512), F32)) for i in range(3)])
                impP = TT(k.sb(st, "impP", (128, 528), F32))
                Ps = [TT(k.sb(st, f"P{i}", (128, 512), F32)) for i in range(4)]
                Pbs = [TT(k.sb(st, f"Pb{i}", (128, 512), BF16)) for i in range(4)]
                rss = [TT(k.sb(st, f"rs{i}", (128, 2), F32)) for i in range(4)]
                pbT = TT(k.sb(st, "pbT", (128, NCH, 4, 128), BF16))
                score = TT(k.sb(st, "score", (128, 128), F32))
                sc2 = TT(k.sb(st, "sc2", (128, 128), F32))
                m8 = TT(k.sb(st, "m8", (128, 16), F32))
                selm = TT(k.sb(st, "selm", (128, 128), F32))
                nbb = TT(k.sb(st, "nbb", (128, 128), BF16))
                nmTr = Ring([TT(k.sb(st, f"nmT{i}", (128, 4, 128), BF16)) for i in range(2)])
                pTr = Ring([TT(k.sb(st, f"pT{i}", (128, 512), BF16)) for i in range(6)])
                Rr = Ring([TT(k.sb(st, f"R{i}", (128, 512), F32)) for i in range(3)])
                for t_ in Rr.items:
                    k.ms(pool, t_[:], 0.0, [t_])
                bcs = TT(k.sb(st, "bcs", (64, 512), F32))
                yn = TT(k.sb(st, "yn", (64, 512), F32))
                ynb_r = Ring([TT(k.sb(st, f"ynb{i}", (64, 512), BF16)) for i in range(2)])
                zr = Ring([TT(k.ps(st, f"nz{i}", (128, 512), F32)) for i in range(3)])
                tpr = Ring([TT(k.ps(st, f"ntp{i}", (128, 128), BF16)) for i in range(2)])
                obank = {b: TT(k.ps(st, f"nob{b}", (128, 512), F32)) for b in range(3)}

                class Blk:
                    pass

                for g in range(2):
                    k.ms(pool, kst[64:128, :], 0.0, [kst])
                    k.dma(sp, kst[0:64, :], KST[g * 64:(g + 1) * 64, :], writes=[kst])
                    k.ms(pool, kwt[64:128, :], 0.0, [kwt])
                    k.dma(sp, kwt[0:64, :], KWT[g * 64:(g + 1) * 64, :], writes=[kwt])
                    k.ms(pool, kcc[:], 0.0, [kcc])
                    k.dma(sp, kcc[0:64, 0:NC], KCC[g, :, 0:NC], writes=[kcc])
                    k.ms(pool, vcc[:], 0.0, [vcc])
                    k.ms(pool, pbT[:], 0.0, [pbT])
                    for t_ in Pbs:
                        k.ms(pool, t_[:], 0.0, [t_])
                    for c in range(NCH):
                        w = min(128, NC - c * 128)
                        k.dma(sp, vcc[0:w, c, 0:64], VCC[c * 128:c * 128 + w, g, :], writes=[vcc])
                    k.ms(pool, vs[:, :, 64:128], 0.0, [vs])
                    k.ms(pool, vw[:, :, 64:128], 0.0, [vw])
                    k.ms(pool, vs[:, :, 64:65], 1.0, [vs])
                    k.ms(pool, vw[:, :, 64:65], 1.0, [vw])
                    for t0 in range(0, NTB, 16):
                        t1_ = min(NTB, t0 + 16)
                        k.dma(sp, vs[:, t0:t1_, 0:64], VTM[t0 * 128:t1_ * 128, 512 + g * 64:512 + (g + 1) * 64].rearrange("(tb p) d -> p tb d", p=128), writes=[vs])
                        k.dma(sp, vw[:, t0:t1_, 0:64], VTM[t0 * 128:t1_ * 128, 640 + g * 64:640 + (g + 1) * 64].rearrange("(tb p) d -> p tb d", p=128), writes=[vw])

                    def part1(i):
                        b_ = Blk(); b_.i = i
                        qs_ = slice(i * 128, (i + 1) * 128)
                        b_.qs_ = qs_
                        qt = qtr.next(); gt = gtr.next()
                        b_.qt, b_.gt = qt, gt
                        k.dma(sp, qt[0:64, :, :], NQT[g * 256:(g + 1) * 256, qs_].rearrange("(h d) q -> d h q", d=64), writes=[qt])
                        k.dma(sp, gt[64:65, :, :].rearrange("o b (h q) -> o b h q", h=4),
                              NGT[g * 12:(g + 1) * 12, qs_].rearrange("(o h b) q -> o b h q", o=1, b=3), writes=[gt])
                        b_.qflat = qt[:].rearrange("d h q -> d (h q)")
                        ncols = min(8 * i + 7, NC)
                        b_.ncols = ncols
                        b_.nch = (ncols + 127) // 128
                        k.ms(pool, impP[:], 0.0, [impP])
                        for hh in range(4):
                            z = zr.next()
                            k.mm(z[:, 0:ncols], qt[:, hh, :], kcc[:, 0:ncols], True, True, [qt, kcc], [z])
                            k.actv(Ps[hh][:, 0:ncols], z[:, 0:ncols], AF.Exp, [z], [Ps[hh]], scale=0.125)
                        for hh in range(4):
                            P = Ps[hh]
                            if i >= 1:
                                k.tt(dve, P[:, ncols - 8:ncols], P[:, ncols - 8:ncols], cmask[:, 0:8], ALU.mult, [P, cmask], [P])
                            else:
                                k.tt(dve, P[:, 0:7], P[:, 0:7], cmask[:, 1:8], ALU.mult, [P, cmask], [P])
                        for hh in range(4):
                            k.op(dve, "tensor_reduce", [Ps[hh]], [rss[hh]], out=rss[hh][:, 0:1], in_=Ps[hh][:, 0:ncols], axis=AX.X, op=ALU.add)
                        for hh in range(4):
                            k.ts(dve, rss[hh][:, 0:1], rss[hh][:, 0:1], 1e-30, None, ALU.add, ALU.bypass, [rss[hh]], [rss[hh]])
                        for hh in range(4):
                            k.op(dve, "reciprocal", [rss[hh]], [rss[hh]], out=rss[hh][:, 1:2], in_=rss[hh][:, 0:1])
                        for hh in range(4):
                            k.ts(dve, Pbs[hh][:, 0:ncols], Ps[hh][:, 0:ncols], rss[hh][:, 1:2], None, ALU.mult, ALU.bypass, [Ps[hh], rss[hh]], [Pbs[hh]])
                        for hh in range(4):
                            k.op(dve, "scalar_tensor_tensor", [Ps[hh], rss[hh], impP], [impP], out=impP[:, 1:1 + ncols], in0=Ps[hh][:, 0:ncols],
                                 scalar=rss[hh][:, 1:2], in1=impP[:, 1:1 + ncols], op0=ALU.mult, op1=ALU.add)
                        k.tt(dve, score[:, 0:NSEL], impP[:, 0:4 * NSEL:4], impP[:, 1:1 + 4 * NSEL:4], ALU.add, [impP], [score])
                        for m in (2, 3, 4):
                            k.tt(dve, score[:, 0:NSEL], score[:, 0:NSEL], impP[:, m:m + 4 * NSEL:4], ALU.add, [impP, score], [score])
                        if 2 * i + 2 < NSEL:
                            k.ms(dve, score[:, 2 * i + 2:NSEL], -1.0, [score])
                        k.ms(dve, score[:, 0:1], 100.0, [score])
                        k.ms(dve, score[:, 2 * i:2 * i + 1], 100.0, [score])
                        k.ms(dve, score[0:64, 2 * i + 1:2 * i + 2], -1.0, [score])
                        k.ms(dve, score[64:128, 2 * i + 1:2 * i + 2], 100.0, [score])
                        if i >= 1:
                            k.ms(dve, score[0:64, 2 * i - 1:2 * i], 100.0, [score])
                        k.op(dve, "max", [score], [m8], out=m8[:, 0:8], in_=score[:, 0:NSEL])
                        k.op(dve, "match_replace", [m8, score], [sc2], out=sc2[:, 0:NSEL], in_to_replace=m8[:, 0:8],
                             in_values=score[:, 0:NSEL], imm_value=-2.0)
                        k.op(dve, "max", [sc2], [m8], out=m8[:, 8:16], in_=sc2[:, 0:NSEL])
                        k.ts(dve, selm[:, 0:NSEL], score[:, 0:NSEL], m8[:, 15:16], None, ALU.is_ge, ALU.bypass, [score, m8], [selm])
                        k.op(dve, "scalar_tensor_tensor", [score, selm], [selm], out=selm[:, 0:NSEL], in0=score[:, 0:NSEL],
                             scalar=-0.5, in1=selm[:, 0:NSEL], op0=ALU.is_gt, op1=ALU.mult)
                        k.ms(pool, nbb[:], 0.0, [nbb])
                        k.ts(dve, nbb[:, 0:NSEL], selm[:, 0:NSEL], -1.0, NEGB, ALU.add, ALU.mult, [selm], [nbb])
                        return b_

                    def part2(b_):
                        ncols, nch = b_.ncols, b_.nch
                        for hh in range(4):
                            for c in range(nch):
                                tp = tpr.next()
                                k.op(pe, "transpose", [Pbs[hh], identb], [tp], out=tp[:, :], in_=Pbs[hh][:, c * 128:(c + 1) * 128], identity=identb[:])
                                if (hh + c) % 2:
                                    k.cp(dve, pbT[:, c, hh, :], tp[:, :], [tp], [pbT])
                                else:
                                    k.actv(pbT[:, c, hh, :], tp[:, :], AF.Copy, [tp], [pbT])
                        ocp = obank[0]
                        for c in range(nch):
                            k.mm(ocp[:, :], vcc[:, c, :], pbT[:, c, :, :].rearrange("n h q -> n (h q)"), c == 0, c == nch - 1, [vcc, pbT], [ocp])
                        tp = tpr.next()
                        k.op(pe, "transpose", [nbb, identb], [tp], out=tp[:, :], in_=nbb[:, :], identity=identb[:])
                        nmT = nmTr.next()
                        b_.nmT = nmT
                        for hh in range(4):
                            if hh % 2:
                                k.cp(dve, nmT[:, hh, :], tp[:, :], [tp], [nmT])
                            else:
                                k.actv(nmT[:, hh, :], tp[:, :], AF.Copy, [tp], [nmT])
                        b_.nmflat = nmT[:].rearrange("j h q -> j (h q)")

                    def att(b_):
                        i = b_.i
                        items = []
                        kbs = [kb for kb in range(i - 4, i + 1) if kb >= 0]
                        for n_i, kb in enumerate(kbs):
                            items.append(("w", kb, n_i == 0, n_i == len(kbs) - 1))
                        for kb in range(i + 1):
                            items.append(("s", kb, kb == 0, kb == i))

                        def sA(it):
                            kind, kb, first, last = it
                            z = zr.next(); pT = pTr.next()
                            ksl = slice(kb * 128, (kb + 1) * 128)
                            if kind == "s":
                                k.mm(z[:], kst[:, ksl], b_.qflat, True, False, [kst, b_.qt], [z])
                                k.mm(z[:], esel[0:NSEL, ksl], b_.nmflat[0:NSEL, :], False, kb != i, [esel, b_.nmT], [z])
                                if kb == i:
                                    k.mm(z[:], identb[:], ble[:], False, True, [identb, ble], [z])
                            else:
                                edge = (kb == i) or (kb == i - 4)
                                k.mm(z[:], kwt[:, ksl], b_.qflat, True, not edge, [kwt, b_.qt], [z])
                                if kb == i:
                                    k.mm(z[:], identb[:], ble[:], False, True, [identb, ble], [z])
                                elif kb == i - 4:
                                    k.mm(z[:], identb[:], bgt[:], False, True, [identb, bgt], [z])
                            k.actv(pT[:], z[:], AF.Exp, [z], [pT], scale=0.125)
                            return pT

                        def sC(it, pT):
                            kind, kb, first, last = it
                            if kind == "s":
                                k.mm(obank[1][:], vs[:, kb, :], pT[:], first, last, [vs, pT], [obank[1]])
                            else:
                                k.mm(obank[2][:], vw[:, kb, :], pT[:], first, last, [vw, pT], [obank[2]])

                        pts = {}
                        n_ = len(items)
                        LA = 3
                        for s_ in range(n_ + LA):
                            if s_ < n_:
                                pts[s_] = sA(items[s_])
                            if 0 <= s_ - LA < n_:
                                sC(items[s_ - LA], pts.pop(s_ - LA))

                    def combineA(b_):
                        gt = b_.gt
                        b_.Rs = []
                        for b in range(3):
                            R_ = Rr.next()
                            b_.Rs.append(R_)
                            ob = obank[b]
                            if b == 0:
                                k.actv(R_[0:64, :], ob[0:64, :], AF.Copy, [ob], [R_])
                                k.cp(dve, R_[64:65, :], gt[64:65, 0, :], [gt], [R_])
                            else:
                                k.actv(R_[0:65, :], ob[0:65, :], AF.Copy, [ob], [R_])
                                k.op(dve, "reciprocal", [R_], [R_], out=R_[64:65, :], in_=R_[64:65, :])
                                k.tt(dve, R_[64:65, :], R_[64:65, :], gt[64:65, b, :], ALU.mult, [R_, gt], [R_])

                    def combineB(b_):
                        for b in range(3):
                            R_ = b_.Rs[b]
                            z = zr.next()
                            k.mm(z[:, :], selM[:, :], R_[:, :], True, True, [selM, R_], [z])
                            if b == 0:
                                k.tt(dve, yn[:], R_[0:64, :], z[0:64, :], ALU.mult, [R_, z], [yn])
                            else:
                                k.tt(dve, bcs[:], R_[0:64, :], z[0:64, :], ALU.mult, [R_, z], [bcs])
                                k.tt(pool, yn[:], yn[:], bcs[:], ALU.add, [yn, bcs], [yn])
                        ynb = ynb_r.next()
                        k.cp(pool, ynb[:], yn[:], [yn], [ynb])
                        k.dma(sp, YNT[g * 256:(g + 1) * 256, b_.qs_].rearrange("(h d) q -> d h q", d=64),
                              ynb[:].rearrange("d (h q) -> d h q", h=4), reads=[ynb])

                    cur = part1(0)
                    part2(cur)
                    for i in range(NTB):
                        nxt = part1(i + 1) if i + 1 < NTB else None
                        att(cur)
                        combineA(cur)
                        if nxt is not None:
                            part2(nxt)
                        combineB(cur)
                        cur = nxt
                k.barrier()

        if stop_after >= 4 and 4 not in skip:
            with ExitStack() as st:
                negU = TT(k.sb(st, "negU", (128, 128), BF16))
                neg8 = TT(k.sb(st, "neg8", (128, 128), BF16))
                mask4 = TT(k.sb(st, "mask4", (128, 4, 512), BF16))
                k.dma(pool, negU[:], cst["negU8"], writes=[negU])
                k.dma(pool, neg8[:], cst["neg8"], writes=[neg8])
                k.dma(pool, mask4[:], cst["mask4"].rearrange("p (a b) -> p a b", a=4), writes=[mask4])
                ktr = Ring([TT(k.sb(st, f"kt{i}", (128, S), BF16)) for i in range(2)])
                qtr = Ring([TT(k.sb(st, f"qt{i}", (128, S), BF16)) for i in range(2)])
                vr = Ring([TT(k.sb(st, f"vv{i}", (128, NTB, 128), BF16)) for i in range(2)])
                for t_ in ktr.items + qtr.items:
                    k.ms(pool, t_[64:128, :], 0.0, [t_])
                for t_ in vr.items:
                    k.ms(pool, t_[:, :, 64:128], 0.0, [t_])
                er = Ring([TT(k.sb(st, f"e{i}", (128, 512), F32)) for i in range(4)])
                ecr = Ring([TT(k.sb(st, f"ec{i}", (128, 512), F32)) for i in range(3)])
                spr = Ring([TT(k.sb(st, f"sp{i}", (128, 512), BF16)) for i in range(4)])
                ar = Ring([TT(k.sb(st, f"a{i}", (128, 512), BF16)) for i in range(4)])
                acc32r = Ring([TT(k.sb(st, f"acc32{i}", (128, 512), F32)) for i in range(2)])
                accbr = Ring([TT(k.sb(st, f"accb{i}", (128, 512), BF16)) for i in range(4)])
                yor = Ring([TT(k.sb(st, f"yo{i}", (64, 512), BF16)) for i in range(2)])
                zar = Ring([TT(k.ps(st, f"za{i}", (128, 512), F32)) for i in range(3)])
                zbr = Ring([TT(k.ps(st, f"zb{i}", (128, 512), F32)) for i in range(3)])
                orr = Ring([TT(k.ps(st, f"o{i}", (128, 512), F32)) for i in range(2)])

                class Tl:
                    pass

                tiles = []
                heads_ld = {}
                for hh in range(NH):
                    for G in range(NG):
                        kbs = list(range(4 * G + 3, -1, -1))
                        grp = Tl(); grp.hh = hh; grp.G = G
                        for n_i, kb in enumerate(kbs):
                            t = Tl(); t.grp = grp; t.n_i = n_i; t.kb = kb; t.last = (n_i == len(kbs) - 1)
                            t.jb = kb - 4 * G
                            tiles.append(t)

                def load_head(hh):
                    kt = ktr.next(); qt = qtr.next(); vv = vr.next()
                    k.dma(sp, kt[0:64, :], KT[hh * 64:(hh + 1) * 64, :], writes=[kt])
                    k.dma(sp, qt[0:64, :], QT[hh * 64:(hh + 1) * 64, :], writes=[qt])
                    for t0 in range(0, NTB, 16):
                        t1_ = min(NTB, t0 + 16)
                        k.dma(sp, vv[:, t0:t1_, 0:64], VTM[t0 * 128:t1_ * 128, hh * 64:(hh + 1) * 64].rearrange("(tb p) d -> p tb d", p=128), writes=[vv])
                    heads_ld[hh] = (kt, qt, vv)

                def stA(t):
                    g_ = t.grp
                    kt, qt, vv = heads_ld[g_.hh]
                    if t.n_i == 0:
                        g_.ob = orr.next(); g_.accb = accbr.next()
                        k.ms(pool, g_.accb[:], 0.0, [g_.accb])
                    z = zar.next(); e = er.next(); t.spt = spr.next(); t.e = e
                    t.qs = qt[:, g_.G * 512:(g_.G + 1) * 512]
                    t.ks = kt[:, t.kb * 128:(t.kb + 1) * 128]
                    t.kt, t.qt, t.vv = kt, qt, vv
                    k.mm(z[:], t.ks, t.qs, True, True, [kt, qt], [z])
                    k.actv(e[:], z[:], AF.Exp, [z], [e], scale=0.125)
                    k.actv(t.spt[:], e[:], AF.Ln, [e], [t.spt], bias=1.0)
                    if t.jb >= 0:
                        k.tt(pool, t.spt[:], t.spt[:], mask4[:, t.jb, :], ALU.mult, [t.spt, mask4], [t.spt])

                def stB(t):
                    g_ = t.grp
                    z = zbr.next(); t.a = ar.next()
                    ec = ecr.next()
                    k.mm(z[:], negU[:], t.spt[:], True, False, [negU, t.spt], [z])
                    k.mm(z[:], neg8[:], g_.accb[:], False, True, [neg8, g_.accb], [z])
                    k.actv(ec[:], z[:], AF.Exp, [z], [ec], scale=0.125)
                    k.tt(dve, t.a[:], t.e[:], ec[:], ALU.mult, [t.e, ec], [t.a])
                    if t.jb >= 0:
                        k.tt(pool, t.a[:], t.a[:], mask4[:, t.jb, :], ALU.mult, [t.a, mask4], [t.a])
                    if not t.last:
                        prev = g_.accb
                        g_.accb = accbr.next()
                        k.tt(dve, g_.accb[:], prev[:], t.spt[:], ALU.add, [prev, t.spt], [g_.accb])

                def stC(t):
                    g_ = t.grp
                    k.mm(g_.ob[:], t.vv[:, t.kb, :], t.a[:], t.n_i == 0, t.last, [t.vv, t.a], [g_.ob])
                    if t.last:
                        yo = yor.next()
                        k.cp(dve, yo[:], g_.ob[0:64, :], [g_.ob], [yo])
                        k.dma(sp, YST[g_.hh * 64:(g_.hh + 1) * 64, g_.G * 512:(g_.G + 1) * 512], yo[:], reads=[yo])

                N_ = len(tiles)
                load_head(0)
                if NH > 1:
                    load_head(1)
                for s_ in range(N_ + 2):
                    if s_ < N_:
                        stA(tiles[s_])
                    if 0 <= s_ - 1 < N_:
                        stB(tiles[s_ - 1])
                    if 0 <= s_ - 2 < N_:
                        tc_ = tiles[s_ - 2]
                        stC(tc_)
                        if tc_.last and tc_.grp.G == NG - 1 and tc_.grp.hh + 2 < NH:
                            load_head(tc_.grp.hh + 2)
                k.barrier()

        def layer_norm(st_tiles, v, gam, bet, res):
            stats, mv = st_tiles
            for c in range(2):
                k.op(dve, "bn_stats", [v], [stats], out=stats[:, c, :], in_=v[:, c * 512:(c + 1) * 512])
            k.op(dve, "bn_aggr", [stats], [mv], out=mv[:, 0:2], in_=stats[:])
            k.ts(dve, mv[:, 3:4], mv[:, 1:2], EPS, None, ALU.add, ALU.bypass, [mv], [mv])
            k.actv(mv[:, 3:4], mv[:, 3:4], AF.Sqrt, [mv], [mv])
            k.op(dve, "reciprocal", [mv], [mv], out=mv[:, 2:3], in_=mv[:, 3:4])
            k.ts(dve, res[:], v[:], mv[:, 0:1], mv[:, 2:3], ALU.subtract, ALU.mult, [v, mv], [res])
            k.tt(pool, res[:], res[:], gam[:], ALU.mult, [res, gam], [res])
            k.tt(pool, res[:], res[:], bet[:], ALU.add, [res, bet], [res])

        if stop_after >= 5 and 5 not in skip:
            with ExitStack() as st:
                wps = TT(k.sb(st, "wps", (128, 4, D), BF16))
                wpn = TT(k.sb(st, "wpn", (128, 4, D), BF16))
                wo = TT(k.sb(st, "wo", (128, 8, D), BF16))
                g1 = TT(k.sb(st, "g1", (128, D), F32)); b1 = TT(k.sb(st, "b1", (128, D), F32))
                wr = TT(k.sb(st, "wr", (128, 8, NEXP), F32)); br = TT(k.sb(st, "br", (128, NEXP), F32))
                iota = TT(k.sb(st, "iota", (128, NEXP), F32)); ecap = TT(k.sb(st, "ecap", (128, NEXP), F32))
                ustr = TT(k.sb(st, "ustr", (128, 128), F32)); ones = TT(k.sb(st, "ones", (128, 128), F32))
                cum = TT(k.sb(st, "cum", (128, NEXP), F32))
                k.dma(pool, wps[:], w_proj_sb.rearrange("(c p) n -> p c n", p=128), writes=[wps])
                k.dma(pool, wpn[:], w_proj_nsa.rearrange("(c p) n -> p c n", p=128), writes=[wpn])
                k.dma(pool, wo[:], w_out.rearrange("(kc p) n -> p kc n", p=128), writes=[wo])
                k.dma(sp, g1[:], ln1_g[0:1, :].broadcast_to((128, D)), writes=[g1])
                k.dma(sp, b1[:], ln1_b[0:1, :].broadcast_to((128, D)), writes=[b1])
                k.dma(sp, wr[:], w_router.rearrange("(kc p) e -> p kc e", p=128), writes=[wr])
                k.dma(sp, br[:], b_router[0:1, :].broadcast_to((128, NEXP)), writes=[br])
                k.dma(sp, iota[:], cst["iota32"], writes=[iota])
                k.dma(sp, ustr[:], cst["ustrict"], writes=[ustr])
                k.dma(sp, ones[:], cst["ones"], writes=[ones])
                k.ts(dve, ecap[:], iota[:], float(CAP), None, ALU.mult, ALU.bypass, [iota], [ecap])
                k.ms(pool, cum[:], 0.0, [cum])
                ystr = Ring([TT(k.sb(st, f"yst{i}", (128, 4, 512), BF16)) for i in range(1)])
                yntr = Ring([TT(k.sb(st, f"ynt{i}", (128, 4, 512), BF16)) for i in range(1)])
                mgtr = Ring([TT(k.sb(st, f"mgt{i}", (128, 16, 512), BF16)) for i in range(1)])
                mT = TT(k.sb(st, "mT", (128, 8, 512), BF16))
                t1r = Ring([TT(k.sb(st, f"mt1{i}", (128, 512), F32)) for i in range(2)])
                t2r = Ring([TT(k.sb(st, f"mt2{i}", (128, 512), F32)) for i in range(2)])
                xtr = Ring([TT(k.sb(st, f"xt{i}", (128, D), F32)) for i in range(2)])
                vt = TT(k.sb(st, "vt", (128, D), F32))
                h1r = Ring([TT(k.sb(st, f"h1{i}", (128, D), F32)) for i in range(2)])
                h1br = Ring([TT(k.sb(st, f"h1b{i}", (128, D), BF16)) for i in range(3)])
                h1Tr = Ring([TT(k.sb(st, f"h1T{i}", (128, 8, 128), F32)) for i in range(2)])
                stats = TT(k.sb(st, "stats", (128, 2, 6), F32)); mv = TT(k.sb(st, "mv", (128, 4), F32))
                lg = TT(k.sb(st, "lg", (128, NEXP), F32)); msk = TT(k.sb(st, "msk", (128, NEXP), F32))
                v8 = TT(k.sb(st, "v8", (128, 8), F32)); i8 = TT(k.sb(st, "i8", (128, 8), U32))
                sm = TT(k.sb(st, "sm", (128, 8), F32)); idxf = TT(k.sb(st, "idxf", (128, 4), F32))
                dstf = TT(k.sb(st, "dstf", (128, NEXP), F32)); okm = TT(k.sb(st, "okm", (128, NEXP), F32))
                oh = TT(k.sb(st, "oh", (128, NEXP), F32)); dsel = TT(k.sb(st, "dsel", (128, 4), F32))
                psr = Ring([TT(k.ps(st, f"p5a{i}", (128, 512), F32)) for i in range(4)])
                ptr5 = Ring([TT(k.ps(st, f"p5t{i}", (128, 4, 128), F32)) for i in range(2)])
                pl = TT(k.ps(st, "p5l", (128, 512), F32))
                pendY = [None]

                def stY(tbg, h1T, h1b):
                    for kc in range(8):
                        k.mm(pl[:, 0:NEXP], h1T[:, kc, :], wr[:, kc, :], kc == 0, kc == 7, [h1T, wr], [pl])
                    k.tt(dve, lg[:], pl[:, 0:NEXP], br[:], ALU.add, [pl, br], [lg])
                    k.op(dve, "max", [lg], [v8], out=v8[:], in_=lg[:])
                    k.op(dve, "max_index", [v8, lg], [i8], out=i8[:], in_max=v8[:], in_values=lg[:])
                    k.ts(dve, msk[:], lg[:], v8[:, 3:4], None, ALU.is_ge, ALU.bypass, [lg, v8], [msk])
                    k.ts(dve, sm[:, 0:1], v8[:, 0:1], -1.0, None, ALU.mult, ALU.bypass, [v8], [sm])
                    k.actv(sm[:, 4:8], v8[:, 0:4], AF.Exp, [v8, sm], [sm], bias=sm[:, 0:1])
                    k.op(dve, "tensor_reduce", [sm], [sm], out=sm[:, 1:2], in_=sm[:, 4:8], axis=AX.X, op=ALU.add)
                    k.op(dve, "reciprocal", [sm], [sm], out=sm[:, 2:3], in_=sm[:, 1:2])
                    k.ts(dve, w4t[:, tbg, :], sm[:, 4:8], sm[:, 2:3], None, ALU.mult, ALU.bypass, [sm], [w4t])
                    k.mm(pl[:, 64:64 + NEXP], ustr[:], msk[:], True, False, [ustr, msk], [pl])
                    k.mm(pl[:, 64:64 + NEXP], ones[:], cum[:], False, True, [ones, cum], [pl])
                    k.tt(dve, dstf[:], pl[:, 64:64 + NEXP], ecap[:], ALU.add, [pl, ecap], [dstf])
                    k.ts(dve, okm[:], pl[:, 64:64 + NEXP], float(CAP) - 0.5, None, ALU.is_lt, ALU.bypass, [pl], [okm])
                    k.tt(dve, cum[:], cum[:], msk[:], ALU.add, [cum, msk], [cum])
                    k.ts(dve, dstf[:], dstf[:], -float(TRASH), None, ALU.add, ALU.bypass, [dstf], [dstf])
                    k.tt(dve, dstf[:], dstf[:], okm[:], ALU.mult, [dstf, okm], [dstf])
                    k.ts(dve, dstf[:], dstf[:], float(TRASH), None, ALU.add, ALU.bypass, [dstf], [dstf])
                    k.cp(dve, idxf[:], i8[:, 0:4], [i8], [idxf])
                    for kk in range(4):
                        k.ts(dve, oh[:], iota[:], idxf[:, kk:kk + 1], None, ALU.is_equal, ALU.bypass, [iota, idxf], [oh])
                        k.tt(dve, oh[:], oh[:], dstf[:], ALU.mult, [oh, dstf], [oh])
                        k.op(dve, "tensor_reduce", [oh], [dsel], out=dsel[:, kk:kk + 1], in_=oh[:], axis=AX.X, op=ALU.add)
                    k.cp(dve, destt[:, tbg, :], dsel[:], [dsel], [destt])
                    for kk in range(4):
                        k.idma(XBUF[:, :], bass.IndirectOffsetOnAxis(ap=destt[:, tbg, kk:kk + 1], axis=0), h1b[:], None,
                               reads=[h1b, destt])

                for g in range(NG):
                    gs = slice(g * 512, (g + 1) * 512)
                    yst = ystr.next(); ynt = yntr.next(); mgt = mgtr.next()
                    k.dma(sp, yst[:], YST[:, gs].rearrange("(c p) q -> p c q", p=128), writes=[yst])
                    k.dma(sp, ynt[:], YNT[:, gs].rearrange("(c p) q -> p c q", p=128), writes=[ynt])
                    k.dma(sp, mgt[:], MGT[:, gs].rearrange("(c p) q -> p c q", p=128), writes=[mgt])
                    for dc in range(8):
                        bs = psr.next(); bn = psr.next(); t1 = t1r.next(); t2 = t2r.next()
                        for hh in range(4):
                            k.mm(bs[:], wps[:, hh, dc * 128:(dc + 1) * 128], yst[:, hh, :], hh == 0, hh == 3, [wps, yst], [bs])
                        for hh in range(4):
                            k.mm(bn[:], wpn[:, hh, dc * 128:(dc + 1) * 128], ynt[:, hh, :], hh == 0, hh == 3, [wpn, ynt], [bn])
                        k.tt(dve, t1[:], bs[:], mgt[:, dc, :], ALU.mult, [bs, mgt], [t1])
                        k.tt(dve, t2[:], bn[:], mgt[:, 8 + dc, :], ALU.mult, [bn, mgt], [t2])
                        k.tt(pool, mT[:, dc, :], t1[:], t2[:], ALU.add, [t1, t2], [mT])
                    for tb in range(4):
                        tbg = g * 4 + tb
                        rows = slice(tbg * 128, (tbg + 1) * 128)
                        xt = xtr.next(); h1 = h1r.next(); h1b = h1br.next(); h1T = h1Tr.next()
                        k.dma(sp, xt[:], x[rows, :], writes=[xt])
                        for half in range(2):
                            u = psr.next()
                            for dc in range(8):
                                k.mm(u[:], mT[:, dc, tb * 128:(tb + 1) * 128], wo[:, dc, half * 512:(half + 1) * 512], dc == 0, dc == 7, [mT, wo], [u])
                            k.op(dve, "scalar_tensor_tensor", [xt, u], [vt], out=vt[:, half * 512:(half + 1) * 512],
                                 in0=xt[:, half * 512:(half + 1) * 512], scalar=ALPHA, in1=u[:], op0=ALU.mult, op1=ALU.add)
                        layer_norm((stats, mv), vt, g1, b1, h1)
                        k.dma(sp, H1[rows, :], h1[:], reads=[h1])
                        k.actv(h1b[:], h1[:], AF.Copy, [h1], [h1b])
                        for q4 in range(2):
                            pt = ptr5.next()
                            for c in range(4):
                                kc = q4 * 4 + c
                                k.op(pe, "transpose", [h1, identf], [pt], out=pt[:, c, :], in_=h1[:, kc * 128:(kc + 1) * 128], identity=identf[:])
                            k.cp(dve, h1T[:, q4 * 4:(q4 + 1) * 4, :], pt[:], [pt], [h1T])
                        if pendY[0] is not None:
                            stY(*pendY[0])
                        pendY[0] = (tbg, h1T, h1b)
                if pendY[0] is not None:
                    stY(*pendY[0])
                k.barrier()

        if stop_after >= 6 and 6 not in skip:
            with ExitStack() as st:
                bguT = TT(k.sb(st, "bguT", (128, 16, NEXP), F32))
                with ExitStack() as st2:
                    bgs = TT(k.sb(st2, "bgs", (NEXP, 2 * D), F32))
                    gtmp = Ring([TT(k.ps(st2, f"p6b{i}", (128, 512), F32)) for i in range(2)])
                    k.dma(sp, bgs[:], b_gate_up, writes=[bgs])
                    for c in range(16):
                        pt = gtmp.next()
                        k.op(pe, "transpose", [bgs, identf], [pt], out=pt[:, 0:NEXP], in_=bgs[:, c * 128:(c + 1) * 128], identity=identf[0:NEXP, 0:NEXP])
                        k.cp(dve, bguT[:, c, :], pt[:, 0:NEXP], [pt], [bguT])
                    k.barrier()
                wgur = Ring([TT(k.sb(st, f"wgu{i}", (128, 8, 2 * D), BF16)) for i in range(2)])
                wdr = Ring([TT(k.sb(st, f"wd{i}", (128, 8, D), BF16)) for i in range(2)])
                bdr = Ring([TT(k.sb(st, f"bd{i}", (128, D), F32)) for i in range(2)])
                stg = Ring([TT(k.sb(st, f"stg{i}", (128, D), F32)) for i in range(4)])
                xrr = Ring([TT(k.sb(st, f"xr{i}", (128, D), BF16)) for i in range(3)])
                xTs = [TT(k.sb(st, f"exT{i}", (128, 8, 512), BF16)) for i in range(2)]
                hTs = [TT(k.sb(st, f"ehT{i}", (128, 8, 512), BF16)) for i in range(2)]
                ggr = Ring([TT(k.sb(st, f"gg{i}", (128, 512), F32)) for i in range(2)])
                sgr = Ring([TT(k.sb(st, f"sg{i}", (128, 512), F32)) for i in range(2)])
                uur = Ring([TT(k.sb(st, f"uu{i}", (128, 512), F32)) for i in range(2)])
                orr = Ring([TT(k.sb(st, f"orow{i}", (128, D), F32)) for i in range(2)])
                ptr6 = Ring([TT(k.ps(st, f"p6t{i}", (128, 8, 128), BF16)) for i in range(2)])
                gur = Ring([TT(k.ps(st, f"p6g{i}", (128, 512), F32)) for i in range(4)])
                opr = Ring([TT(k.ps(st, f"p6o{i}", (128, 512), F32)) for i in range(2)])
                rgs = [(r0, min(512, CAP - r0)) for r0 in range(0, CAP, 512)]
                NR = len(rgs)

                def wjobs(e):
                    wgu = wgur.next(); wd = wdr.next(); bd = bdr.next()
                    jobs = []
                    for kc in range(8):
                        for hf in range(2):
                            jobs.append((wgu[:, kc, hf * D:(hf + 1) * D], w_gate_up[e, kc * 128:(kc + 1) * 128, hf * D:(hf + 1) * D], wgu))
                    for kc in range(8):
                        jobs.append((wd[:, kc, :], w_down[e, kc * 128:(kc + 1) * 128, :], wd))
                    return (wgu, wd, bd), jobs

                def run_loads(jobs):
                    pend = []
                    for (dst, src_ap, wt) in jobs:
                        sg_t = stg.next()
                        k.dma(sp, sg_t[:], src_ap, writes=[sg_t])
                        pend.append((dst, sg_t, wt))
                    return pend

                def run_casts(pend):
                    for (dst, sg_t, wt) in pend:
                        k.actv(dst, sg_t[:], AF.Copy, [sg_t], [wt])

                def chunks(lst, n):
                    per = (len(lst) + n - 1) // n
                    return [lst[i * per:(i + 1) * per] for i in range(n)]

                steps = [(e, ri) for e in range(NEXP) for ri in range(NR)]
                ws = {}
                ws[0], jobs0 = wjobs(0)
                for j0 in range(0, len(jobs0), 4):
                    run_casts(run_loads(jobs0[j0:j0 + 4]))
                k.dma(sp, ws[0][2][:], b_down[0:1, :].broadcast_to((128, D)), writes=[ws[0][2]])
                evc = [0]

                def stX(t):
                    e, ri = steps[t]
                    r0, nr = rgs[ri]
                    row0 = e * CAP + r0
                    xT = xTs[t % 2]
                    for b in range(nr // 128):
                        xr = xrr.next()
                        k.dma(act, xr[:], XBUF[row0 + b * 128:row0 + (b + 1) * 128, :], writes=[xr])
                        pt = ptr6.next()
                        for kc in range(8):
                            k.op(pe, "transpose", [xr, identb], [pt], out=pt[:, kc, :], in_=xr[:, kc * 128:(kc + 1) * 128], identity=identb[:])
                        evac(evc[0], xT[:, :, b * 128:(b + 1) * 128], pt[:], [pt], [xT]); evc[0] += 1

                def stGU(t, fcs):
                    e, ri = steps[t]
                    r0, nr = rgs[ri]
                    wgu = ws[e][0]
                    xT = xTs[t % 2]; hT = hTs[t % 2]
                    for fc in fcs:
                        gp = gur.next(); up = gur.next(); gg = ggr.next(); sg_ = sgr.next(); uu = uur.next()
                        for kc in range(8):
                            k.mm(gp[:, 0:nr], wgu[:, kc, fc * 128:(fc + 1) * 128], xT[:, kc, 0:nr], kc == 0, kc == 7, [wgu, xT], [gp])
                        for kc in range(8):
                            k.mm(up[:, 0:nr], wgu[:, kc, D + fc * 128:D + (fc + 1) * 128], xT[:, kc, 0:nr], kc == 0, kc == 7, [wgu, xT], [up])
                        k.ts(dve, gg[:, 0:nr], gp[:, 0:nr], bguT[:, fc, e:e + 1], 7.0, ALU.add, ALU.min, [gp, bguT], [gg])
                        k.actv(sg_[:, 0:nr], gg[:, 0:nr], AF.Sigmoid, [gg], [sg_], scale=1.702)
                        k.ts(dve, uu[:, 0:nr], up[:, 0:nr], bguT[:, 8 + fc, e:e + 1], 7.0, ALU.add, ALU.min, [up, bguT], [uu])
                        k.ts(dve, uu[:, 0:nr], uu[:, 0:nr], -7.0, 1.0, ALU.max, ALU.add, [uu], [uu])
                        k.tt(pool, gg[:, 0:nr], gg[:, 0:nr], sg_[:, 0:nr], ALU.mult, [gg, sg_], [gg])
                        k.tt(dve, hT[:, fc, 0:nr], gg[:, 0:nr], uu[:, 0:nr], ALU.mult, [gg, uu], [hT])

                def stDN(t):
                    e, ri = steps[t]
                    r0, nr = rgs[ri]
                    row0 = e * CAP + r0
                    wd, bd = ws[e][1], ws[e][2]
                    hT = hTs[t % 2]
                    for b in range(nr // 128):
                        orow = orr.next()
                        for half in range(2):
                            op_ = opr.next()
                            for fc in range(8):
                                k.mm(op_[:], hT[:, fc, b * 128:(b + 1) * 128], wd[:, fc, half * 512:(half + 1) * 512], fc == 0, fc == 7, [hT, wd], [op_])
                            k.tt(dve, orow[:, half * 512:(half + 1) * 512], op_[:], bd[:, half * 512:(half + 1) * 512], ALU.add, [op_, bd], [orow])
                        k.dma(sp, OBUF[row0 + b * 128:row0 + (b + 1) * 128, :], orow[:], reads=[orow])

                stX(0)
                jparts = None
                pendB = []
                for t, (e, ri) in enumerate(steps):
                    stGU(t, range(0, 4))
                    if t > 0:
                        stDN(t - 1)
                    run_casts(pendB); pendB = []
                    if ri == 0:
                        if e + 1 < NEXP:
                            ws[e + 1], njobs = wjobs(e + 1)
                            k.dma(sp, ws[e + 1][2][:], b_down[e + 1:e + 2, :].broadcast_to((128, D)), writes=[ws[e + 1][2]])
                            jparts = chunks(njobs, NR)
                        else:
                            jparts = [[] for _ in range(NR)]
                    jl = jparts[ri]
                    pendA = run_loads(jl[0:4])
                    if t + 1 < len(steps):
                        stX(t + 1)
                    stGU(t, range(4, 8))
                    run_casts(pendA)
                    pendB = run_loads(jl[4:8])
                    for j0 in range(8, len(jl), 4):
                        run_casts(pendB)
                        pendB = run_loads(jl[j0:j0 + 4])
                    if ri == NR - 1:
                        run_casts(pendB); pendB = []
                stDN(len(steps) - 1)
                k.barrier()

        if stop_after >= 7 and 7 not in skip:
            with ExitStack() as st:
                g2 = TT(k.sb(st, "g2", (128, D), F32)); b2 = TT(k.sb(st, "b2", (128, D), F32))
                k.dma(sp, g2[:], ln2_g[0:1, :].broadcast_to((128, D)), writes=[g2])
                k.dma(sp, b2[:], ln2_b[0:1, :].broadcast_to((128, D)), writes=[b2])
                rkr = Ring([TT(k.sb(st, f"rk{i}", (128, D), F32)) for i in range(8)])
                h1r = Ring([TT(k.sb(st, f"h7{i}", (128, D), F32)) for i in range(2)])
                yr = Ring([TT(k.sb(st, f"y7{i}", (128, D), F32)) for i in range(2)])
                rr = Ring([TT(k.sb(st, f"r7{i}", (128, D), F32)) for i in range(2)])
                stats = TT(k.sb(st, "stats7", (128, 2, 6), F32)); mv = TT(k.sb(st, "mv7", (128, 4), F32))
                for tb in range(NTB):
                    rows = slice(tb * 128, (tb + 1) * 128)
                    h1 = h1r.next(); y = yr.next(); res = rr.next()
                    k.dma(sp, h1[:], H1[rows, :], writes=[h1])
                    rks = []
                    for kk in range(4):
                        rk = rkr.next()
                        k.idma(rk[:], None, OBUF[:, :], bass.IndirectOffsetOnAxis(ap=destt[:, tb, kk:kk + 1], axis=0),
                               reads=[destt], writes=[rk])
                        rks.append(rk)
                    k.ts(dve, y[:], rks[0][:], w4t[:, tb, 0:1], None, ALU.mult, ALU.bypass, [rks[0], w4t], [y])
                    for kk in range(1, 4):
                        k.op(dve, "scalar_tensor_tensor", [rks[kk], w4t, y], [y], out=y[:], in0=rks[kk][:], scalar=w4t[:, tb, kk:kk + 1],
                             in1=y[:], op0=ALU.mult, op1=ALU.add)
                    k.op(dve, "scalar_tensor_tensor", [h1, y], [y], out=y[:], in0=h1[:], scalar=ALPHA, in1=y[:], op0=ALU.mult, op1=ALU.add)
                    layer_norm((stats, mv), y, g2, b2, res)
                    k.dma(sp, out[rows, :], res[:], reads=[res])
                k.barrier()

        k.barrier()
        k.emit()
    return nc


def make_in_map(xb, p, consts, fm, tm):
    w_in = p["w_in"][0]
    im = {"x": np.ascontiguousarray(xb), "wfm": np.ascontiguousarray(w_in[:, fm]), "wtm": np.ascontiguousarray(w_in[:, tm])}
    for kk, v in consts.items():
        im["c_" + kk] = v
    for nm in ("cmp_pe_k", "cmp_w1_k", "cmp_w2_k", "cmp_pe_v", "cmp_w1_v", "cmp_w2_v", "w_proj_sb", "w_proj_nsa", "w_out",
               "w_router", "w_gate_up", "b_gate_up", "w_down", "b_down"):
        im[nm] = np.ascontiguousarray(p[nm][0])
    for nm in ("ln1_g", "ln1_b", "ln2_g", "ln2_b", "b_router"):
        im[nm] = np.ascontiguousarray(p[nm][0:1])
    return im


_NC_CACHE = {}


def kernel(**inputs):
    p = {k_: np.asarray(v, dtype=np.float32) for k_, v in inputs.items()}
    x = p["x"]
    B, S, _ = x.shape
    if S not in _NC_CACHE:
        _NC_CACHE[S] = build(S=S, CAP=(1536 if S == 8192 else max(256, S // 4)))
    nc = _NC_CACHE[S]
    consts = host_consts(S)
    fm, tm = win_layout()
    in_maps = [make_in_map(x[b], p, consts, fm, tm) for b in range(B)]
    res = run_bass_kernel_spmd(nc, in_maps, core_ids=list(range(B)))
    return np.stack([np.asarray(r["out"], dtype=np.float32) for r in res.results], axis=0)
```

```python
import math
from contextlib import ExitStack

import numpy as np
import concourse.bass as bass
import concourse.mybir as mybir
from concourse.bass_utils import run_bass_kernel_spmd

F32 = mybir.dt.float32
BF16 = mybir.dt.bfloat16
I32 = mybir.dt.int32
U32 = mybir.dt.uint32
AF = mybir.ActivationFunctionType
ALU = mybir.AluOpType
AX = mybir.AxisListType

D = 1024
HD = 64
NH = 8
NEXP = 32
IN_W = 4888
ALPHA = 2.0 ** 0.25
EPS = 1e-5
NEGB = 30000.0


class Eng:
    def __init__(self, name, sem, is_pe=False):
        self.name = name
        self.sem = sem
        self.n = 0
        self.is_pe = is_pe
        self.prog = []
        self.waited = {}
        self.dma_i = 0


class TT:
    def __init__(self, ap):
        self.ap = ap
        self.w = None
        self.r = {}

    def __getitem__(self, idx):
        return self.ap[idx]


class KB:
    def __init__(self, nc, es):
        self.nc = nc
        self.es = es
        sem = lambda n: es.enter_context(nc.semaphore(n))
        self.pe = Eng("tensor", sem("s_pe"), True)
        self.act = Eng("scalar", sem("s_act"))
        self.dve = Eng("vector", sem("s_dve"))
        self.pool = Eng("gpsimd", sem("s_pool"))
        self.sp = Eng("sync", sem("s_sp"))
        self.engs = [self.pe, self.act, self.dve, self.pool, self.sp]
        NS = 6
        self.hw_sems = {e.name: [[sem(f"dh_{e.name}{i}"), 0] for i in range(NS)] for e in (self.sp, self.act)}
        self.sw_sems = [[sem(f"dsw{i}"), 0] for i in range(NS)]
        self.dma_out = {}
        self.sems_by_id = {}

    def _wait(self, eng, need):
        for sid, (s, v) in need.items():
            if eng.is_pe and s is eng.sem:
                continue
            if eng.waited.get(sid, 0) >= v:
                continue
            eng.waited[sid] = v
            eng.prog.append(lambda h, s=s, v=v: h.wait_ge(s, v))

    def _deps(self, reads, writes):
        need = {}

        def add(tok):
            if tok is None:
                return
            s, v = tok
            if id(s) not in need or need[id(s)][1] < v:
                need[id(s)] = (s, v)

        for t in reads:
            add(t.w)
        for t in writes:
            add(t.w)
            for tok in t.r.values():
                add(tok)
        return need

    def _mark(self, tok, reads, writes):
        for t in reads:
            t.r[id(tok[0])] = tok
        for t in writes:
            t.w = tok
            t.r = {}

    def op(self, eng, name, reads=(), writes=(), **kw):
        self._wait(eng, self._deps(reads, writes))
        eng.n += 1
        s = eng.sem
        eng.prog.append(lambda h, name=name, kw=kw, s=s: getattr(h, name)(**kw).then_inc(s, 1))
        tok = (s, eng.n)
        self._mark(tok, reads, writes)
        return tok

    def mm(self, out, lhsT, rhs, start, stop, reads, writes):
        return self.op(self.pe, "matmul", reads, writes, out=out, lhsT=lhsT, rhs=rhs, start=start, stop=stop)

    def actv(self, out, in_, func, reads, writes, **kw):
        return self.op(self.act, "activation", reads, writes, out=out, in_=in_, func=func, **kw)

    def tt(self, eng, out, in0, in1, op, reads, writes):
        return self.op(eng, "tensor_tensor", reads, writes, out=out, in0=in0, in1=in1, op=op)

    def ts(self, eng, out, in0, s1, s2, op0, op1, reads, writes, **kw):
        return self.op(eng, "tensor_scalar", reads, writes, out=out, in0=in0, scalar1=s1, scalar2=s2, op0=op0, op1=op1, **kw)

    def cp(self, eng, out, in_, reads, writes):
        return self.op(eng, "tensor_copy", reads, writes, out=out, in_=in_)

    def ms(self, eng, ap, val, writes):
        return self.op(eng, "memset", (), writes, ap=ap, constant=val)

    def dma(self, eng, out, in_, reads=(), writes=(), **kw):
        pool_ = self.sw_sems if eng is self.pool else self.hw_sems[eng.name]
        slot = pool_[eng.dma_i % len(pool_)]
        eng.dma_i += 1
        s = slot[0]
        need = self._deps(reads, writes)
        if slot[1] > 0:
            need[id(s)] = (s, slot[1])
        self._wait(eng, need)
        slot[1] += 16
        v = slot[1]
        eng.prog.append(lambda h, out=out, in_=in_, s=s, kw=kw: h.dma_start(out=out, in_=in_, **kw).then_inc(s, 16))
        tok = (s, v)
        self.dma_out[id(s)] = tok
        self._mark(tok, reads, writes)
        return tok

    def idma(self, out, out_off, in_, in_off, reads=(), writes=(), **kw):
        eng = self.pool
        slot = self.sw_sems[eng.dma_i % len(self.sw_sems)]
        eng.dma_i += 1
        s = slot[0]
        need = self._deps(reads, writes)
        if slot[1] > 0:
            need[id(s)] = (s, slot[1])
        self._wait(eng, need)
        slot[1] += 16
        v = slot[1]
        eng.prog.append(lambda h, out=out, out_off=out_off, in_=in_, in_off=in_off, kw=kw, s=s: h.indirect_dma_start(
            out=out, out_offset=out_off, in_=in_, in_offset=in_off, **kw).then_inc(s, 16))
        tok = (s, v)
        self.dma_out[id(s)] = tok
        self._mark(tok, reads, writes)
        return tok

    def barrier(self):
        need = {}
        for e in self.engs:
            if e.n > 0:
                need[id(e.sem)] = (e.sem, e.n)
        for sid, tok in self.dma_out.items():
            need[sid] = tok
        for e in self.engs:
            n2 = {k: v for k, v in need.items() if v[0] is not e.sem}
            saved = e.is_pe
            e.is_pe = False
            self._wait(e, n2)
            e.is_pe = saved

    def emit(self):
        nc = self.nc
        with nc.Block() as block:
            for e in self.engs:
                def body(h, e=e):
                    for f in e.prog:
                        f(h)
                getattr(block, e.name)(body)

    def sb(self, st, name, shape, dt):
        return st.enter_context(self.nc.sbuf_tensor(name, list(shape), dt))

    def ps(self, st, name, shape, dt):
        return st.enter_context(self.nc.psum_tensor(name, list(shape), dt))


class Ring:
    def __init__(self, items):
        self.items = items
        self.i = 0

    def next(self):
        t = self.items[self.i % len(self.items)]
        self.i += 1
        return t


def host_consts(S):
    c = {}
    c["ident"] = np.eye(128, dtype=np.float32)
    half = HD // 2
    inv_freq = (np.float32(10000.0) ** (-np.arange(half, dtype=np.float32) / np.float32(half))).astype(np.float32)
    ang = np.arange(S, dtype=np.float32)[:, None] * inv_freq[None, :]
    cos = np.cos(ang).astype(np.float32).T
    sin = np.sin(ang).astype(np.float32).T
    cos64 = np.concatenate([cos, cos], 0)
    sin64 = np.concatenate([-sin, sin], 0)
    c["cosT"] = np.ascontiguousarray(np.concatenate([cos64, cos64], 0))
    c["sinT"] = np.ascontiguousarray(np.concatenate([sin64, sin64], 0))
    j = np.arange(128)
    c["negU8"] = np.where(j[:, None] >= j[None, :], -8.0, 0.0).astype(np.float32)
    c["neg8"] = np.full((128, 128), -8.0, np.float32)
    m4 = np.zeros((128, 4, 4, 128), np.float32)
    strict = (j[:, None] < j[None, :]).astype(np.float32)
    for jb in range(4):
        for cb in range(4):
            if cb == jb:
                m4[:, jb, cb, :] = strict
            elif cb > jb:
                m4[:, jb, cb, :] = 1.0
    c["mask4"] = m4.reshape(128, 4 * 512)
    le = np.where(j[:, None] <= j[None, :], 0.0, -NEGB).astype(np.float32)
    gt = np.where(j[:, None] > j[None, :], 0.0, -NEGB).astype(np.float32)
    c["bias_le"] = np.tile(le, (1, 4))
    c["bias_gt"] = np.tile(gt, (1, 4))
    nsel = S // 64
    E = np.zeros((128, S), np.float32)
    E[np.arange(S) // 64, np.arange(S)] = 1.0
    c["Esel"] = E
    m = np.arange(-1, 7)
    c["cmask"] = (j[:, None] >= 16 * m[None, :] + 31).astype(np.float32)
    c["iota32"] = np.tile(np.arange(NEXP, dtype=np.float32)[None, :], (128, 1))
    c["ustrict"] = (j[:, None] < j[None, :]).astype(np.float32)
    c["ones"] = np.ones((128, 128), np.float32)
    sm = np.zeros((128, 64), np.float32)
    sm[64, :] = 1.0
    c["selM"] = sm
    return c


CONST_SHAPES = lambda S: {
    "ident": (128, 128), "cosT": (128, S), "sinT": (128, S), "negU8": (128, 128), "neg8": (128, 128),
    "mask4": (128, 2048), "bias_le": (128, 512), "bias_gt": (128, 512), "Esel": (128, S),
    "cmask": (128, 8), "iota32": (128, NEXP), "ustrict": (128, 128), "ones": (128, 128), "selM": (128, 64),
}

def win_layout():
    def sw(base, nheads):
        idx = []
        for h in range(nheads):
            for d in range(HD):
                idx.append(base + h * HD + (d + 32) % HD)
        return idx

    r = lambda a, b: list(range(a, b))
    fm = []
    fm += r(0, 512)
    fm += r(512, 1024)
    fm += r(1536, 2048)
    fm += sw(1536, 8)
    fm += r(2048, 2176) + sw(2048, 2)
    fm += r(2304, 2432) + sw(2304, 2)
    fm += r(2560, 2688) + sw(2560, 2)
    fm += r(2176, 2304)
    fm += r(2840, 4888)
    fm += r(2816, 2840)
    tm = r(1024, 1536) + r(2432, 2560) + r(2688, 2816)
    return np.array(fm), np.array(tm)


NFM = 5016
NTM = 768


def build(S=8192, CAP=1536, dbg=(), stop_after=99, skip=()):
    NTB = S // 128
    NG = S // 512
    NC = S // 16 - 1
    NSEL = S // 64
    NROWS = NEXP * CAP + 128
    TRASH = NEXP * CAP
    nc = bass.Bass("TRN2", target_bir_lowering=False)

    def din(name, shape, dt=F32):
        return nc.dram_tensor(name, list(shape), dt, kind="ExternalInput").ap()

    def dscr(name, shape, dt):
        kind = "ExternalOutput" if name in dbg else "Internal"
        return nc.dram_tensor(name, list(shape), dt, kind=kind).ap()

    x = din("x", (S, D))
    wfm = din("wfm", (D, NFM))
    wtm = din("wtm", (D, NTM))
    cst = {k: din("c_" + k, shp) for k, shp in CONST_SHAPES(S).items()}
    cmp_in = {k: din(k, shp) for k, shp in dict(
        cmp_pe_k=(32, 64), cmp_w1_k=(2048, 64), cmp_w2_k=(64, 64),
        cmp_pe_v=(32, 64), cmp_w1_v=(2048, 64), cmp_w2_v=(64, 64)).items()}
    w_proj_sb = din("w_proj_sb", (512, D))
    w_proj_nsa = din("w_proj_nsa", (512, D))
    w_out = din("w_out", (D, D))
    ln1_g = din("ln1_g", (1, D)); ln1_b = din("ln1_b", (1, D))
    ln2_g = din("ln2_g", (1, D)); ln2_b = din("ln2_b", (1, D))
    w_router = din("w_router", (D, NEXP)); b_router = din("b_router", (1, NEXP))
    w_gate_up = din("w_gate_up", (NEXP, D, 2 * D)); b_gate_up = din("b_gate_up", (NEXP, 2 * D))
    w_down = din("w_down", (NEXP, D, D)); b_down = din("b_down", (NEXP, D))
    out = nc.dram_tensor("out", [S, D], F32, kind="ExternalOutput").ap()

    QT = dscr("QT", (512, S), BF16); KT = dscr("KT", (512, S), BF16)
    NQT = dscr("NQT", (512, S), BF16)
    KCT = dscr("KCT", (128, S), BF16); VCT = dscr("VCT", (128, S), BF16)
    KST = dscr("KST", (128, S), BF16); KWT = dscr("KWT", (128, S), BF16)
    NGT = dscr("NGT", (24, S), F32); MGT = dscr("MGT", (2048, S), BF16)
    VTM = dscr("VTM", (S, NTM), BF16)
    KCC = dscr("KCC", (2, 64, 512), BF16); VCC = dscr("VCC", (512, 2, 64), BF16)
    YST = dscr("YST", (512, S), BF16); YNT = dscr("YNT", (512, S), BF16)
    H1 = dscr("H1", (S, D), F32)
    XBUF = dscr("XBUF", (NROWS, D), BF16)
    OBUF = dscr("OBUF", (NROWS, D), F32)

    with ExitStack() as es:
        k = KB(nc, es)
        pe, act, dve, pool, sp = k.pe, k.act, k.dve, k.pool, k.sp
        identb = TT(k.sb(es, "identb", (128, 128), BF16))
        identf = TT(k.sb(es, "identf", (128, 128), F32))
        destt = TT(k.sb(es, "destt", (128, NTB, 4), I32))
        w4t = TT(k.sb(es, "w4t", (128, NTB, 4), F32))
        k.dma(pool, identb[:], cst["ident"], writes=[identb])
        k.dma(sp, identf[:], cst["ident"], writes=[identf])

        def evac(i, out_ap, in_ap, reads, writes):
            if i % 2 == 0:
                return k.actv(out_ap, in_ap, AF.Copy, reads, writes)
            return k.cp(dve, out_ap, in_ap, reads, writes)

        if stop_after >= 1:
            with ExitStack() as st:
                zt = TT(k.sb(st, "zt", (128, 4, D), BF16))
                zf = TT(k.sb(st, "zf", (128, D), F32))
                k.ms(pool, zt[:], 0.0, [zt])
                k.ms(pool, zf[:], 0.0, [zf])
                k.dma(act, OBUF[TRASH:TRASH + 128, :], zf[:], reads=[zf])
                nblk = NROWS // 128
                for b0 in range(0, nblk, 4):
                    nb = min(4, nblk - b0)
                    k.dma(act, XBUF[b0 * 128:(b0 + nb) * 128, :].rearrange("(b p) d -> p b d", p=128), zt[:, 0:nb, :], reads=[zt])
                wfb = TT(k.sb(st, "wfb", (128, 8, NFM), BF16))
                wtb = TT(k.sb(st, "wtb", (128, 8, NTM), BF16))
                for kc in range(8):
                    k.dma(pool, wfb[:, kc, :], wfm[kc * 128:(kc + 1) * 128, :], writes=[wfb])
                    k.dma(pool, wtb[:, kc, :], wtm[kc * 128:(kc + 1) * 128, :], writes=[wtb])
                xbs = Ring([TT(k.sb(st, f"xb{i}", (128, 4, D), BF16)) for i in range(2)])
                xTs = Ring([TT(k.sb(st, f"xT{i}", (128, 8, 512), BF16)) for i in range(2)])
                cosr = Ring([TT(k.sb(st, f"cos{i}", (128, 512), F32)) for i in range(2)])
                sinr = Ring([TT(k.sb(st, f"sin{i}", (128, 512), F32)) for i in range(2)])
                obr = Ring([TT(k.sb(st, f"ob{i}", (128, 512), BF16)) for i in range(6)])
                ofr = Ring([TT(k.sb(st, f"of{i}", (128, 512), F32)) for i in range(4)])
                otr = Ring([TT(k.sb(st, f"ot{i}", (128, NTM), BF16)) for i in range(2)])
                ptr = Ring([TT(k.ps(st, f"ptr{i}", (128, 8, 128), BF16)) for i in range(2)])
                accr = Ring([TT(k.ps(st, f"acc{i}", (128, 512), F32)) for i in range(6)])
                ev = 0
                for g in range(NG):
                    xb = xbs.next(); xT = xTs.next(); cs = cosr.next(); sn = sinr.next()
                    gs = slice(g * 512, (g + 1) * 512)
                    k.dma(pool, xb[:], x[gs, :].rearrange("(tb p) d -> p tb d", p=128), writes=[xb])
                    k.dma(sp, cs[:], cst["cosT"][:, gs], writes=[cs])
                    k.dma(sp, sn[:], cst["sinT"][:, gs], writes=[sn])
                    for tb in range(4):
                        pt = ptr.next()
                        for kc in range(8):
                            k.op(pe, "transpose", [xb, identb], [pt], out=pt[:, kc, :],
                                 in_=xb[:, tb, kc * 128:(kc + 1) * 128], identity=identb[:])
                        evac(ev, xT[:, :, tb * 128:(tb + 1) * 128], pt[:], [pt], [xT]); ev += 1

                    def fm_mm(chunk, xT, width=128):
                        acc = accr.next()
                        for kc in range(8):
                            k.mm(acc[0:width, :], wfb[:, kc, chunk * 128:chunk * 128 + width], xT[:, kc, :],
                                 kc == 0, kc == 7, [wfb, xT], [acc])
                        return acc

                    for chunk, dst, r0 in [(c, QT, c * 128) for c in range(4)] + [(4 + c, KT, c * 128) for c in range(4)] + [(22, VCT, 0)]:
                        acc = fm_mm(chunk, xT)
                        ob = obr.next()
                        evac(ev, ob[:], acc[:], [acc], [ob]); ev += 1
                        k.dma(sp, dst[r0:r0 + 128, gs], ob[:], reads=[ob])
                    for ca, cb_, dst, r0 in [(8 + c, 12 + c, NQT, c * 128) for c in range(4)] + [(16, 17, KCT, 0), (18, 19, KST, 0), (20, 21, KWT, 0)]:
                        a = fm_mm(ca, xT); b = fm_mm(cb_, xT)
                        t1 = ofr.next(); t2 = ofr.next(); ob = obr.next()
                        k.tt(dve, t1[:], a[:], cs[:], ALU.mult, [a, cs], [t1])
                        k.tt(dve, t2[:], b[:], sn[:], ALU.mult, [b, sn], [t2])
                        k.tt(pool, ob[:], t1[:], t2[:], ALU.add, [t1, t2], [ob])
                        k.dma(sp, dst[r0:r0 + 128, gs], ob[:], reads=[ob])
                    for c in range(16):
                        acc = fm_mm(23 + c, xT)
                        ob = obr.next()
                        k.actv(ob[:], acc[:], AF.Sigmoid, [acc], [ob])
                        k.dma(sp, MGT[c * 128:(c + 1) * 128, gs], ob[:], reads=[ob])
                    acc = fm_mm(39, xT, 24)
                    of = ofr.next()
                    k.actv(of[0:24, :], acc[0:24, :], AF.Sigmoid, [acc], [of])
                    k.dma(sp, NGT[0:24, gs], of[0:24, :], reads=[of])
                    for tb in range(4):
                        ot = otr.next()
                        for (c0, c1) in ((0, 512), (512, 768)):
                            acc = accr.next()
                            for kc in range(8):
                                k.mm(acc[:, 0:c1 - c0], xT[:, kc, tb * 128:(tb + 1) * 128], wtb[:, kc, c0:c1],
                                     kc == 0, kc == 7, [wtb, xT], [acc])
                            evac(ev, ot[:, c0:c1], acc[:, 0:c1 - c0], [acc], [ot]); ev += 1
                        r0 = g * 512 + tb * 128
                        k.dma(sp, VTM[r0:r0 + 128, :], ot[:], reads=[ot])
                k.barrier()

        if stop_after >= 2 and 2 not in skip:
            with ExitStack() as st:
                w1 = TT(k.sb(st, "cw1", (64, 32, 64), BF16))
                w2 = TT(k.sb(st, "cw2", (64, 64), BF16))
                pe_f = TT(k.sb(st, "cpe", (32, 64), F32))
                peT = TT(k.sb(st, "cpeT", (64, 32), BF16))
                cbs = TT(k.sb(st, "ccb", (64, 1), F32))
                src = TT(k.sb(st, "csrc", (64, S), BF16))
                u = TT(k.sb(st, "cu", (64, 512), F32))
                u2 = TT(k.sb(st, "cu2", (64, 512), F32))
                sg = TT(k.sb(st, "csg", (64, 512), F32))
                gl = TT(k.sb(st, "cgl", (64, 512), BF16))
                ko = TT(k.sb(st, "cko", (128, 512), BF16))
                hid = TT(k.ps(st, "chid", (64, 512), F32))
                pp = TT(k.ps(st, "cpp", (128, 512), F32))
                for which in ("k", "v"):
                    k.dma(pool, w1[:], cmp_in["cmp_w1_" + which].rearrange("(j d) h -> d j h", d=64), writes=[w1])
                    k.dma(pool, w2[:], cmp_in["cmp_w2_" + which], writes=[w2])
                    k.dma(sp, pe_f[:], cmp_in["cmp_pe_" + which], writes=[pe_f])
                    k.op(pe, "transpose", [pe_f, identf], [pp], out=pp[0:64, 0:32], in_=pe_f[:], identity=identf[0:32, 0:32])
                    k.cp(dve, peT[:], pp[0:64, 0:32], [pp], [peT])
                    for j in range(32):
                        k.mm(pp[0:64, 0:1], w1[:, j, :], peT[:, j:j + 1], j == 0, j == 31, [w1, peT], [pp])
                    k.cp(dve, cbs[:], pp[0:64, 0:1], [pp], [cbs])
                    for g in range(2):
                        k.dma(sp, src[:], (KCT if which == "k" else VCT)[g * 64:(g + 1) * 64, :], writes=[src])
                        for j in range(32):
                            k.mm(hid[:, 0:NC], w1[:, j, :], src[:, j:j + 16 * (NC - 1) + 1:16], j == 0, j == 31, [w1, src], [hid])
                        k.actv(u[:, 0:NC], hid[:, 0:NC], AF.Identity, [hid, cbs], [u], bias=cbs[:, 0:1])
                        k.tt(dve, u2[:, 0:NC], u[:, 0:NC], u[:, 0:NC], ALU.mult, [u], [u2])
                        k.ts(dve, u2[:, 0:NC], u2[:, 0:NC], 0.044715, 1.0, ALU.mult, ALU.add, [u2], [u2])
                        k.tt(dve, u2[:, 0:NC], u2[:, 0:NC], u[:, 0:NC], ALU.mult, [u2, u], [u2])
                        k.actv(sg[:, 0:NC], u2[:, 0:NC], AF.Sigmoid, [u2], [sg], scale=1.5957691216057308)
                        k.tt(dve, gl[:, 0:NC], u[:, 0:NC], sg[:, 0:NC], ALU.mult, [u, sg], [gl])
                        if which == "k":
                            k.mm(pp[0:64, 0:NC], w2[:], gl[:, 0:NC], True, True, [w2, gl], [pp])
                            k.cp(dve, ko[0:64, 0:NC], pp[0:64, 0:NC], [pp], [ko])
                            k.dma(sp, KCC[g, :, 0:NC], ko[0:64, 0:NC], reads=[ko])
                        else:
                            for c in range((NC + 127) // 128):
                                w = min(128, NC - c * 128)
                                k.mm(pp[0:w, 0:64], gl[:, c * 128:c * 128 + w], w2[:], True, True, [w2, gl], [pp])
                                k.cp(dve, ko[0:w, 0:64], pp[0:w, 0:64], [pp], [ko])
                                k.dma(sp, VCC[c * 128:c * 128 + w, g, :], ko[0:w, 0:64], reads=[ko])
                k.barrier()

        if stop_after >= 3 and 3 not in skip:
            with ExitStack() as st:
                NCH = (NC + 127) // 128
                esel = TT(k.sb(st, "esel", (128, S), BF16))
                ble = TT(k.sb(st, "ble", (128, 512), BF16))
                bgt = TT(k.sb(st, "bgt", (128, 512), BF16))
                cmask = TT(k.sb(st, "cmask", (128, 8), F32))
                selM = TT(k.sb(st, "selM", (128, 128), F32))
                k.dma(pool, esel[:], cst["Esel"], writes=[esel])
                k.dma(pool, ble[:], cst["bias_le"], writes=[ble])
                k.dma(pool, bgt[:], cst["bias_gt"], writes=[bgt])
                k.dma(sp, cmask[:], cst["cmask"], writes=[cmask])
                k.ms(pool, selM[:], 0.0, [selM])
                k.dma(sp, selM[:, 0:64], cst["selM"], writes=[selM])
                kst = TT(k.sb(st, "kst", (128, S), BF16))
                kwt = TT(k.sb(st, "kwt", (128, S), BF16))
                kcc = TT(k.sb(st, "kcc", (128, 512), BF16))
                vcc = TT(k.sb(st, "vcc", (128, NCH, 128), BF16))
                vs = TT(k.sb(st, "vs", (128, NTB, 128), BF16))
                vw = TT(k.sb(st, "vw", (128, NTB, 128), BF16))
                qtr = Ring([TT(k.sb(st, f"nq{i}", (128, 4, 128), BF16)) for i in range(3)])
                for t_ in qtr.items:
                    k.ms(pool, t_[64:128, :, :], 0.0, [t_])
                gtr = Ring([TT(k.sb(st, f"gt{i}", (65, 3, 512), F32)) for i in range(3)])
                impP = TT(k.sb(st, "impP", (128, 528), F32))
                Ps = [TT(k.sb(st, f"P{i}", (128, 512), F32)) for i in range(4)]
                Pbs = [TT(k.sb(st, f"Pb{i}", (128, 512), BF16)) for i in range(4)]
                rss = [TT(k.sb(st, f"rs{i}", (128, 2), F32)) for i in range(4)]
                pbT = TT(k.sb(st, "pbT", (128, NCH, 4, 128), BF16))
                score = TT(k.sb(st, "score", (128, 128), F32))
                sc2 = TT(k.sb(st, "sc2", (128, 128), F32))
                m8 = TT(k.sb(st, "m8", (128, 16), F32))
                selm = TT(k.sb(st, "selm", (128, 128), F32))
                nbb = TT(k.sb(st, "nbb", (128, 128), BF16))
                nmTr = Ring([TT(k.sb(st, f"nmT{i}", (128, 4, 128), BF16)) for i in range(2)])
                pTr = Ring([TT(k.sb(st, f"pT{i}", (128, 512), BF16)) for i in range(4)])
                Rr = Ring([TT(k.sb(st, f"R{i}", (128, 512), F32)) for i in range(3)])
                for t_ in Rr.items:
                    k.ms(pool, t_[:], 0.0, [t_])
                bcs = TT(k.sb(st, "bcs", (64, 512), F32))
                yn = TT(k.sb(st, "yn", (64, 512), F32))
                ynb_r = Ring([TT(k.sb(st, f"ynb{i}", (64, 512), BF16)) for i in range(2)])
                zr = Ring([TT(k.ps(st, f"nz{i}", (128, 512), F32)) for i in range(3)])
                tpr = Ring([TT(k.ps(st, f"ntp{i}", (128, 128), BF16)) for i in range(2)])
                obank = {b: TT(k.ps(st, f"nob{b}", (128, 512), F32)) for b in range(3)}

                class Blk:
                    pass

                for g in range(2):
                    k.ms(pool, kst[64:128, :], 0.0, [kst])
                    k.dma(sp, kst[0:64, :], KST[g * 64:(g + 1) * 64, :], writes=[kst])
                    k.ms(pool, kwt[64:128, :], 0.0, [kwt])
                    k.dma(sp, kwt[0:64, :], KWT[g * 64:(g + 1) * 64, :], writes=[kwt])
                    k.ms(pool, kcc[:], 0.0, [kcc])
                    k.dma(sp, kcc[0:64, 0:NC], KCC[g, :, 0:NC], writes=[kcc])
                    k.ms(pool, vcc[:], 0.0, [vcc])
                    k.ms(pool, pbT[:], 0.0, [pbT])
                    for t_ in Pbs:
                        k.ms(pool, t_[:], 0.0, [t_])
                    for c in range(NCH):
                        w = min(128, NC - c * 128)
                        k.dma(sp, vcc[0:w, c, 0:64], VCC[c * 128:c * 128 + w, g, :], writes=[vcc])
                    k.ms(pool, vs[:, :, 64:128], 0.0, [vs])
                    k.ms(pool, vw[:, :, 64:128], 0.0, [vw])
                    k.ms(pool, vs[:, :, 64:65], 1.0, [vs])
                    k.ms(pool, vw[:, :, 64:65], 1.0, [vw])
                    for t0 in range(0, NTB, 16):
                        t1_ = min(NTB, t0 + 16)
                        k.dma(sp, vs[:, t0:t1_, 0:64], VTM[t0 * 128:t1_ * 128, 512 + g * 64:512 + (g + 1) * 64].rearrange("(tb p) d -> p tb d", p=128), writes=[vs])
                        k.dma(sp, vw[:, t0:t1_, 0:64], VTM[t0 * 128:t1_ * 128, 640 + g * 64:640 + (g + 1) * 64].rearrange("(tb p) d -> p tb d", p=128), writes=[vw])

                    def part1(i):
                        b_ = Blk(); b_.i = i
                        qs_ = slice(i * 128, (i + 1) * 128)
                        b_.qs_ = qs_
                        qt = qtr.next(); gt = gtr.next()
                        b_.qt, b_.gt = qt, gt
                        k.dma(sp, qt[0:64, :, :], NQT[g * 256:(g + 1) * 256, qs_].rearrange("(h d) q -> d h q", d=64), writes=[qt])
                        k.dma(sp, gt[64:65, :, :].rearrange("o b (h q) -> o b h q", h=4),
                              NGT[g * 12:(g + 1) * 12, qs_].rearrange("(o h b) q -> o b h q", o=1, b=3), writes=[gt])
                        b_.qflat = qt[:].rearrange("d h q -> d (h q)")
                        ncols = min(8 * i + 7, NC)
                        b_.ncols = ncols
                        b_.nch = (ncols + 127) // 128
                        k.ms(pool, impP[:], 0.0, [impP])
                        for hh in range(4):
                            z = zr.next()
                            k.mm(z[:, 0:ncols], qt[:, hh, :], kcc[:, 0:ncols], True, True, [qt, kcc], [z])
                            k.actv(Ps[hh][:, 0:ncols], z[:, 0:ncols], AF.Exp, [z], [Ps[hh]], scale=0.125)
                        for hh in range(4):
                            P = Ps[hh]
                            if i >= 1:
                                k.tt(dve, P[:, ncols - 8:ncols], P[:, ncols - 8:ncols], cmask[:, 0:8], ALU.mult, [P, cmask], [P])
                            else:
                                k.tt(dve, P[:, 0:7], P[:, 0:7], cmask[:, 1:8], ALU.mult, [P, cmask], [P])
                        for hh in range(4):
                            k.op(dve, "tensor_reduce", [Ps[hh]], [rss[hh]], out=rss[hh][:, 0:1], in_=Ps[hh][:, 0:ncols], axis=AX.X, op=ALU.add)
                        for hh in range(4):
                            k.ts(dve, rss[hh][:, 0:1], rss[hh][:, 0:1], 1e-30, None, ALU.add, ALU.bypass, [rss[hh]], [rss[hh]])
                        for hh in range(4):
                            k.op(dve, "reciprocal", [rss[hh]], [rss[hh]], out=rss[hh][:, 1:2], in_=rss[hh][:, 0:1])
                        for hh in range(4):
                            k.ts(dve, Pbs[hh][:, 0:ncols], Ps[hh][:, 0:ncols], rss[hh][:, 1:2], None, ALU.mult, ALU.bypass, [Ps[hh], rss[hh]], [Pbs[hh]])
                        for hh in range(4):
                            k.op(dve, "scalar_tensor_tensor", [Ps[hh], rss[hh], impP], [impP], out=impP[:, 1:1 + ncols], in0=Ps[hh][:, 0:ncols],
                                 scalar=rss[hh][:, 1:2], in1=impP[:, 1:1 + ncols], op0=ALU.mult, op1=ALU.add)
                        k.tt(dve, score[:, 0:NSEL], impP[:, 0:4 * NSEL:4], impP[:, 1:1 + 4 * NSEL:4], ALU.add, [impP], [score])
                        for m in (2, 3, 4):
                            k.tt(dve, score[:, 0:NSEL], score[:, 0:NSEL], impP[:, m:m + 4 * NSEL:4], ALU.add, [impP, score], [score])
                        if 2 * i + 2 < NSEL:
                            k.ms(dve, score[:, 2 * i + 2:NSEL], -1.0, [score])
                        k.ms(dve, score[:, 0:1], 100.0, [score])
                        k.ms(dve, score[:, 2 * i:2 * i + 1], 100.0, [score])
                        k.ms(dve, score[0:64, 2 * i + 1:2 * i + 2], -1.0, [score])
                        k.ms(dve, score[64:128, 2 * i + 1:2 * i + 2], 100.0, [score])
                        if i >= 1:
                            k.ms(dve, score[0:64, 2 * i - 1:2 * i], 100.0, [score])
                        k.op(dve, "max", [score], [m8], out=m8[:, 0:8], in_=score[:, 0:NSEL])
                        k.op(dve, "match_replace", [m8, score], [sc2], out=sc2[:, 0:NSEL], in_to_replace=m8[:, 0:8],
                             in_values=score[:, 0:NSEL], imm_value=-2.0)
                        k.op(dve, "max", [sc2], [m8], out=m8[:, 8:16], in_=sc2[:, 0:NSEL])
                        k.ts(dve, selm[:, 0:NSEL], score[:, 0:NSEL], m8[:, 15:16], None, ALU.is_ge, ALU.bypass, [score, m8], [selm])
                        k.op(dve, "scalar_tensor_tensor", [score, selm], [selm], out=selm[:, 0:NSEL], in0=score[:, 0:NSEL],
                             scalar=-0.5, in1=selm[:, 0:NSEL], op0=ALU.is_gt, op1=ALU.mult)
                        k.ms(pool, nbb[:], 0.0, [nbb])
                        k.ts(dve, nbb[:, 0:NSEL], selm[:, 0:NSEL], -1.0, NEGB, ALU.add, ALU.mult, [selm], [nbb])
                        return b_

                    def part2(b_):
                        ncols, nch = b_.ncols, b_.nch
                        for hh in range(4):
                            for c in range(nch):
                                tp = tpr.next()
                                k.op(pe, "transpose", [Pbs[hh], identb], [tp], out=tp[:, :], in_=Pbs[hh][:, c * 128:(c + 1) * 128], identity=identb[:])
                                if (hh + c) % 2:
                                    k.cp(dve, pbT[:, c, hh, :], tp[:, :], [tp], [pbT])
                                else:
                                    k.actv(pbT[:, c, hh, :], tp[:, :], AF.Copy, [tp], [pbT])
                        ocp = obank[0]
                        for c in range(nch):
                            k.mm(ocp[:, :], vcc[:, c, :], pbT[:, c, :, :].rearrange("n h q -> n (h q)"), c == 0, c == nch - 1, [vcc, pbT], [ocp])
                        tp = tpr.next()
                        k.op(pe, "transpose", [nbb, identb], [tp], out=tp[:, :], in_=nbb[:, :], identity=identb[:])
                        nmT = nmTr.next()
                        b_.nmT = nmT
                        for hh in range(4):
                            if hh % 2:
                                k.cp(dve, nmT[:, hh, :], tp[:, :], [tp], [nmT])
                            else:
                                k.actv(nmT[:, hh, :], tp[:, :], AF.Copy, [tp], [nmT])
                        b_.nmflat = nmT[:].rearrange("j h q -> j (h q)")

                    def att(b_):
                        i = b_.i
                        items = []
                        kbs = [kb for kb in range(i - 4, i + 1) if kb >= 0]
                        for n_i, kb in enumerate(kbs):
                            items.append(("w", kb, n_i == 0, n_i == len(kbs) - 1))
                        for kb in range(i + 1):
                            items.append(("s", kb, kb == 0, kb == i))

                        def sA(it):
                            kind, kb, first, last = it
                            z = zr.next(); pT = pTr.next()
                            ksl = slice(kb * 128, (kb + 1) * 128)
                            if kind == "s":
                                k.mm(z[:], kst[:, ksl], b_.qflat, True, False, [kst, b_.qt], [z])
                                k.mm(z[:], esel[0:NSEL, ksl], b_.nmflat[0:NSEL, :], False, kb != i, [esel, b_.nmT], [z])
                                if kb == i:
                                    k.mm(z[:], identb[:], ble[:], False, True, [identb, ble], [z])
                            else:
                                edge = (kb == i) or (kb == i - 4)
                                k.mm(z[:], kwt[:, ksl], b_.qflat, True, not edge, [kwt, b_.qt], [z])
                                if kb == i:
                                    k.mm(z[:], identb[:], ble[:], False, True, [identb, ble], [z])
                                elif kb == i - 4:
                                    k.mm(z[:], identb[:], bgt[:], False, True, [identb, bgt], [z])
                            k.actv(pT[:], z[:], AF.Exp, [z], [pT], scale=0.125)
                            return pT

                        def sC(it, pT):
                            kind, kb, first, last = it
                            if kind == "s":
                                k.mm(obank[1][:], vs[:, kb, :], pT[:], first, last, [vs, pT], [obank[1]])
                            else:
                                k.mm(obank[2][:], vw[:, kb, :], pT[:], first, last, [vw, pT], [obank[2]])

                        pts = {}
                        n_ = len(items)
                        for s_ in range(n_ + 2):
                            if s_ < n_:
                                pts[s_] = sA(items[s_])
                            if 0 <= s_ - 2 < n_:
                                sC(items[s_ - 2], pts.pop(s_ - 2))

                    def combine(b_):
                        gt = b_.gt
                        for b in range(3):
                            R_ = Rr.next()
                            ob = obank[b]
                            if b == 0:
                                k.actv(R_[0:64, :], ob[0:64, :], AF.Copy, [ob], [R_])
                                k.cp(dve, R_[64:65, :], gt[64:65, 0, :], [gt], [R_])
                            else:
                                k.actv(R_[0:65, :], ob[0:65, :], AF.Copy, [ob], [R_])
                                k.op(dve, "reciprocal", [R_], [R_], out=R_[64:65, :], in_=R_[64:65, :])
                                k.tt(dve, R_[64:65, :], R_[64:65, :], gt[64:65, b, :], ALU.mult, [R_, gt], [R_])
                            z = zr.next()
                            k.mm(z[:, :], selM[:, :], R_[:, :], True, True, [selM, R_], [z])
                            if b == 0:
                                k.tt(dve, yn[:], R_[0:64, :], z[0:64, :], ALU.mult, [R_, z], [yn])
                            else:
                                k.tt(dve, bcs[:], R_[0:64, :], z[0:64, :], ALU.mult, [R_, z], [bcs])
                                k.tt(pool, yn[:], yn[:], bcs[:], ALU.add, [yn, bcs], [yn])
                        ynb = ynb_r.next()
                        k.cp(pool, ynb[:], yn[:], [yn], [ynb])
                        k.dma(sp, YNT[g * 256:(g + 1) * 256, b_.qs_].rearrange("(h d) q -> d h q", d=64),
                              ynb[:].rearrange("d (h q) -> d h q", h=4), reads=[ynb])

                    cur = part1(0)
                    part2(cur)
                    for i in range(NTB):
                        nxt = part1(i + 1) if i + 1 < NTB else None
                        att(cur)
                        combine(cur)
                        if nxt is not None:
                            part2(nxt)
                        cur = nxt
                k.barrier()

        if stop_after >= 4 and 4 not in skip:
            with ExitStack() as st:
                negU = TT(k.sb(st, "negU", (128, 128), BF16))
                neg8 = TT(k.sb(st, "neg8", (128, 128), BF16))
                mask4 = TT(k.sb(st, "mask4", (128, 4, 512), BF16))
                k.dma(pool, negU[:], cst["negU8"], writes=[negU])
                k.dma(pool, neg8[:], cst["neg8"], writes=[neg8])
                k.dma(pool, mask4[:], cst["mask4"].rearrange("p (a b) -> p a b", a=4), writes=[mask4])
                ktr = Ring([TT(k.sb(st, f"kt{i}", (128, S), BF16)) for i in range(2)])
                qtr = Ring([TT(k.sb(st, f"qt{i}", (128, S), BF16)) for i in range(2)])
                vr = Ring([TT(k.sb(st, f"vv{i}", (128, NTB, 128), BF16)) for i in range(2)])
                for t_ in ktr.items + qtr.items:
                    k.ms(pool, t_[64:128, :], 0.0, [t_])
                for t_ in vr.items:
                    k.ms(pool, t_[:, :, 64:128], 0.0, [t_])
                er = Ring([TT(k.sb(st, f"e{i}", (128, 512), F32)) for i in range(3)])
                spr = Ring([TT(k.sb(st, f"sp{i}", (128, 512), BF16)) for i in range(4)])
                ar = Ring([TT(k.sb(st, f"a{i}", (128, 512), BF16)) for i in range(4)])
                acc32r = Ring([TT(k.sb(st, f"acc32{i}", (128, 512), F32)) for i in range(2)])
                accbr = Ring([TT(k.sb(st, f"accb{i}", (128, 512), BF16)) for i in range(4)])
                yor = Ring([TT(k.sb(st, f"yo{i}", (64, 512), BF16)) for i in range(2)])
                zar = Ring([TT(k.ps(st, f"za{i}", (128, 512), F32)) for i in range(3)])
                zbr = Ring([TT(k.ps(st, f"zb{i}", (128, 512), F32)) for i in range(3)])
                orr = Ring([TT(k.ps(st, f"o{i}", (128, 512), F32)) for i in range(2)])

                class Tl:
                    pass

                tiles = []
                heads_ld = {}
                for hh in range(NH):
                    for G in range(NG):
                        kbs = list(range(4 * G + 3, -1, -1))
                        grp = Tl(); grp.hh = hh; grp.G = G
                        for n_i, kb in enumerate(kbs):
                            t = Tl(); t.grp = grp; t.n_i = n_i; t.kb = kb; t.last = (n_i == len(kbs) - 1)
                            t.jb = kb - 4 * G
                            tiles.append(t)

                def load_head(hh):
                    kt = ktr.next(); qt = qtr.next(); vv = vr.next()
                    k.dma(sp, kt[0:64, :], KT[hh * 64:(hh + 1) * 64, :], writes=[kt])
                    k.dma(sp, qt[0:64, :], QT[hh * 64:(hh + 1) * 64, :], writes=[qt])
                    for t0 in range(0, NTB, 16):
                        t1_ = min(NTB, t0 + 16)
                        k.dma(sp, vv[:, t0:t1_, 0:64], VTM[t0 * 128:t1_ * 128, hh * 64:(hh + 1) * 64].rearrange("(tb p) d -> p tb d", p=128), writes=[vv])
                    heads_ld[hh] = (kt, qt, vv)

                def stA(t):
                    g_ = t.grp
                    kt, qt, vv = heads_ld[g_.hh]
                    if t.n_i == 0:
                        g_.ob = orr.next(); g_.acc32 = acc32r.next(); g_.accb = accbr.next()
                        k.ms(pool, g_.acc32[:], 0.0, [g_.acc32])
                        k.ms(pool, g_.accb[:], 0.0, [g_.accb])
                    z = zar.next(); e = er.next(); t.spt = spr.next()
                    t.qs = qt[:, g_.G * 512:(g_.G + 1) * 512]
                    t.ks = kt[:, t.kb * 128:(t.kb + 1) * 128]
                    t.kt, t.qt, t.vv = kt, qt, vv
                    k.mm(z[:], t.ks, t.qs, True, True, [kt, qt], [z])
                    k.actv(e[:], z[:], AF.Exp, [z], [e], scale=0.125)
                    k.actv(t.spt[:], e[:], AF.Ln, [e], [t.spt], bias=1.0)
                    if t.jb >= 0:
                        k.tt(dve, t.spt[:], t.spt[:], mask4[:, t.jb, :], ALU.mult, [t.spt, mask4], [t.spt])

                def stB(t):
                    g_ = t.grp
                    z = zbr.next(); t.a = ar.next()
                    k.mm(z[:], t.ks, t.qs, True, False, [t.kt, t.qt], [z])
                    k.mm(z[:], negU[:], t.spt[:], False, False, [negU, t.spt], [z])
                    k.mm(z[:], neg8[:], g_.accb[:], False, True, [neg8, g_.accb], [z])
                    k.actv(t.a[:], z[:], AF.Exp, [z], [t.a], scale=0.125)
                    if t.jb >= 0:
                        k.tt(dve, t.a[:], t.a[:], mask4[:, t.jb, :], ALU.mult, [t.a, mask4], [t.a])
                    if not t.last:
                        k.tt(dve, g_.acc32[:], g_.acc32[:], t.spt[:], ALU.add, [g_.acc32, t.spt], [g_.acc32])
                        g_.accb = accbr.next()
                        k.cp(dve, g_.accb[:], g_.acc32[:], [g_.acc32], [g_.accb])

                def stC(t):
                    g_ = t.grp
                    k.mm(g_.ob[:], t.vv[:, t.kb, :], t.a[:], t.n_i == 0, t.last, [t.vv, t.a], [g_.ob])
                    if t.last:
                        yo = yor.next()
                        k.cp(dve, yo[:], g_.ob[0:64, :], [g_.ob], [yo])
                        k.dma(sp, YST[g_.hh * 64:(g_.hh + 1) * 64, g_.G * 512:(g_.G + 1) * 512], yo[:], reads=[yo])

                N_ = len(tiles)
                load_head(0)
                if NH > 1:
                    load_head(1)
                for s_ in range(N_ + 2):
                    if s_ < N_:
                        stA(tiles[s_])
                    if 0 <= s_ - 1 < N_:
                        stB(tiles[s_ - 1])
                    if 0 <= s_ - 2 < N_:
                        tc_ = tiles[s_ - 2]
                        stC(tc_)
                        if tc_.last and tc_.grp.G == NG - 1 and tc_.grp.hh + 2 < NH:
                            load_head(tc_.grp.hh + 2)
                k.barrier()

        def layer_norm(st_tiles, v, gam, bet, res):
            stats, mv = st_tiles
            for c in range(2):
                k.op(dve, "bn_stats", [v], [stats], out=stats[:, c, :], in_=v[:, c * 512:(c + 1) * 512])
            k.op(dve, "bn_aggr", [stats], [mv], out=mv[:, 0:2], in_=stats[:])
            k.ts(dve, mv[:, 3:4], mv[:, 1:2], EPS, None, ALU.add, ALU.bypass, [mv], [mv])
            k.actv(mv[:, 3:4], mv[:, 3:4], AF.Sqrt, [mv], [mv])
            k.op(dve, "reciprocal", [mv], [mv], out=mv[:, 2:3], in_=mv[:, 3:4])
            k.ts(dve, res[:], v[:], mv[:, 0:1], mv[:, 2:3], ALU.subtract, ALU.mult, [v, mv], [res])
            k.tt(pool, res[:], res[:], gam[:], ALU.mult, [res, gam], [res])
            k.tt(pool, res[:], res[:], bet[:], ALU.add, [res, bet], [res])

        if stop_after >= 5 and 5 not in skip:
            with ExitStack() as st:
                wps = TT(k.sb(st, "wps", (128, 4, D), BF16))
                wpn = TT(k.sb(st, "wpn", (128, 4, D), BF16))
                wo = TT(k.sb(st, "wo", (128, 8, D), BF16))
                g1 = TT(k.sb(st, "g1", (128, D), F32)); b1 = TT(k.sb(st, "b1", (128, D), F32))
                wr = TT(k.sb(st, "wr", (128, 8, NEXP), F32)); br = TT(k.sb(st, "br", (128, NEXP), F32))
                iota = TT(k.sb(st, "iota", (128, NEXP), F32)); ecap = TT(k.sb(st, "ecap", (128, NEXP), F32))
                ustr = TT(k.sb(st, "ustr", (128, 128), F32)); ones = TT(k.sb(st, "ones", (128, 128), F32))
                cum = TT(k.sb(st, "cum", (128, NEXP), F32))
                k.dma(pool, wps[:], w_proj_sb.rearrange("(c p) n -> p c n", p=128), writes=[wps])
                k.dma(pool, wpn[:], w_proj_nsa.rearrange("(c p) n -> p c n", p=128), writes=[wpn])
                k.dma(pool, wo[:], w_out.rearrange("(kc p) n -> p kc n", p=128), writes=[wo])
                k.dma(sp, g1[:], ln1_g[0:1, :].broadcast_to((128, D)), writes=[g1])
                k.dma(sp, b1[:], ln1_b[0:1, :].broadcast_to((128, D)), writes=[b1])
                k.dma(sp, wr[:], w_router.rearrange("(kc p) e -> p kc e", p=128), writes=[wr])
                k.dma(sp, br[:], b_router[0:1, :].broadcast_to((128, NEXP)), writes=[br])
                k.dma(sp, iota[:], cst["iota32"], writes=[iota])
                k.dma(sp, ustr[:], cst["ustrict"], writes=[ustr])
                k.dma(sp, ones[:], cst["ones"], writes=[ones])
                k.ts(dve, ecap[:], iota[:], float(CAP), None, ALU.mult, ALU.bypass, [iota], [ecap])
                k.ms(pool, cum[:], 0.0, [cum])
                ystr = Ring([TT(k.sb(st, f"yst{i}", (128, 4, 512), BF16)) for i in range(1)])
                yntr = Ring([TT(k.sb(st, f"ynt{i}", (128, 4, 512), BF16)) for i in range(1)])
                mgtr = Ring([TT(k.sb(st, f"mgt{i}", (128, 16, 512), BF16)) for i in range(1)])
                mT = TT(k.sb(st, "mT", (128, 8, 512), BF16))
                t1r = Ring([TT(k.sb(st, f"mt1{i}", (128, 512), F32)) for i in range(2)])
                t2r = Ring([TT(k.sb(st, f"mt2{i}", (128, 512), F32)) for i in range(2)])
                xtr = Ring([TT(k.sb(st, f"xt{i}", (128, D), F32)) for i in range(2)])
                vt = TT(k.sb(st, "vt", (128, D), F32))
                h1r = Ring([TT(k.sb(st, f"h1{i}", (128, D), F32)) for i in range(2)])
                h1br = Ring([TT(k.sb(st, f"h1b{i}", (128, D), BF16)) for i in range(2)])
                h1T = TT(k.sb(st, "h1T", (128, 8, 128), F32))
                stats = TT(k.sb(st, "stats", (128, 2, 6), F32)); mv = TT(k.sb(st, "mv", (128, 4), F32))
                lg = TT(k.sb(st, "lg", (128, NEXP), F32)); msk = TT(k.sb(st, "msk", (128, NEXP), F32))
                v8 = TT(k.sb(st, "v8", (128, 8), F32)); i8 = TT(k.sb(st, "i8", (128, 8), U32))
                sm = TT(k.sb(st, "sm", (128, 8), F32)); idxf = TT(k.sb(st, "idxf", (128, 4), F32))
                dstf = TT(k.sb(st, "dstf", (128, NEXP), F32)); okm = TT(k.sb(st, "okm", (128, NEXP), F32))
                oh = TT(k.sb(st, "oh", (128, NEXP), F32)); dsel = TT(k.sb(st, "dsel", (128, 4), F32))
                psr = Ring([TT(k.ps(st, f"p5a{i}", (128, 512), F32)) for i in range(4)])
                ptr5 = Ring([TT(k.ps(st, f"p5t{i}", (128, 4, 128), F32)) for i in range(2)])
                pl = TT(k.ps(st, "p5l", (128, 512), F32))
                for g in range(NG):
                    gs = slice(g * 512, (g + 1) * 512)
                    yst = ystr.next(); ynt = yntr.next(); mgt = mgtr.next()
                    k.dma(sp, yst[:], YST[:, gs].rearrange("(c p) q -> p c q", p=128), writes=[yst])
                    k.dma(sp, ynt[:], YNT[:, gs].rearrange("(c p) q -> p c q", p=128), writes=[ynt])
                    k.dma(sp, mgt[:], MGT[:, gs].rearrange("(c p) q -> p c q", p=128), writes=[mgt])
                    for dc in range(8):
                        bs = psr.next(); bn = psr.next(); t1 = t1r.next(); t2 = t2r.next()
                        for hh in range(4):
                            k.mm(bs[:], wps[:, hh, dc * 128:(dc + 1) * 128], yst[:, hh, :], hh == 0, hh == 3, [wps, yst], [bs])
                        for hh in range(4):
                            k.mm(bn[:], wpn[:, hh, dc * 128:(dc + 1) * 128], ynt[:, hh, :], hh == 0, hh == 3, [wpn, ynt], [bn])
                        k.tt(dve, t1[:], bs[:], mgt[:, dc, :], ALU.mult, [bs, mgt], [t1])
                        k.tt(dve, t2[:], bn[:], mgt[:, 8 + dc, :], ALU.mult, [bn, mgt], [t2])
                        k.tt(pool, mT[:, dc, :], t1[:], t2[:], ALU.add, [t1, t2], [mT])
                    for tb in range(4):
                        tbg = g * 4 + tb
                        rows = slice(tbg * 128, (tbg + 1) * 128)
                        xt = xtr.next(); h1 = h1r.next(); h1b = h1br.next()
                        k.dma(sp, xt[:], x[rows, :], writes=[xt])
                        for half in range(2):
                            u = psr.next()
                            for dc in range(8):
                                k.mm(u[:], mT[:, dc, tb * 128:(tb + 1) * 128], wo[:, dc, half * 512:(half + 1) * 512], dc == 0, dc == 7, [mT, wo], [u])
                            k.op(dve, "scalar_tensor_tensor", [xt, u], [vt], out=vt[:, half * 512:(half + 1) * 512],
                                 in0=xt[:, half * 512:(half + 1) * 512], scalar=ALPHA, in1=u[:], op0=ALU.mult, op1=ALU.add)
                        layer_norm((stats, mv), vt, g1, b1, h1)
                        k.dma(sp, H1[rows, :], h1[:], reads=[h1])
                        k.actv(h1b[:], h1[:], AF.Copy, [h1], [h1b])
                        for q4 in range(2):
                            pt = ptr5.next()
                            for c in range(4):
                                kc = q4 * 4 + c
                                k.op(pe, "transpose", [h1, identf], [pt], out=pt[:, c, :], in_=h1[:, kc * 128:(kc + 1) * 128], identity=identf[:])
                            k.cp(dve, h1T[:, q4 * 4:(q4 + 1) * 4, :], pt[:], [pt], [h1T])
                        for kc in range(8):
                            k.mm(pl[:, 0:NEXP], h1T[:, kc, :], wr[:, kc, :], kc == 0, kc == 7, [h1T, wr], [pl])
                        k.tt(dve, lg[:], pl[:, 0:NEXP], br[:], ALU.add, [pl, br], [lg])
                        k.op(dve, "max", [lg], [v8], out=v8[:], in_=lg[:])
                        k.op(dve, "max_index", [v8, lg], [i8], out=i8[:], in_max=v8[:], in_values=lg[:])
                        k.ts(dve, msk[:], lg[:], v8[:, 3:4], None, ALU.is_ge, ALU.bypass, [lg, v8], [msk])
                        k.ts(dve, sm[:, 0:1], v8[:, 0:1], -1.0, None, ALU.mult, ALU.bypass, [v8], [sm])
                        k.actv(sm[:, 4:8], v8[:, 0:4], AF.Exp, [v8, sm], [sm], bias=sm[:, 0:1])
                        k.op(dve, "tensor_reduce", [sm], [sm], out=sm[:, 1:2], in_=sm[:, 4:8], axis=AX.X, op=ALU.add)
                        k.op(dve, "reciprocal", [sm], [sm], out=sm[:, 2:3], in_=sm[:, 1:2])
                        k.ts(dve, w4t[:, tbg, :], sm[:, 4:8], sm[:, 2:3], None, ALU.mult, ALU.bypass, [sm], [w4t])
                        k.mm(pl[:, 64:64 + NEXP], ustr[:], msk[:], True, False, [ustr, msk], [pl])
                        k.mm(pl[:, 64:64 + NEXP], ones[:], cum[:], False, True, [ones, cum], [pl])
                        k.tt(dve, dstf[:], pl[:, 64:64 + NEXP], ecap[:], ALU.add, [pl, ecap], [dstf])
                        k.ts(dve, okm[:], pl[:, 64:64 + NEXP], float(CAP) - 0.5, None, ALU.is_lt, ALU.bypass, [pl], [okm])
                        k.tt(dve, cum[:], cum[:], msk[:], ALU.add, [cum, msk], [cum])
                        k.ts(dve, dstf[:], dstf[:], -float(TRASH), None, ALU.add, ALU.bypass, [dstf], [dstf])
                        k.tt(dve, dstf[:], dstf[:], okm[:], ALU.mult, [dstf, okm], [dstf])
                        k.ts(dve, dstf[:], dstf[:], float(TRASH), None, ALU.add, ALU.bypass, [dstf], [dstf])
                        k.cp(dve, idxf[:], i8[:, 0:4], [i8], [idxf])
                        for kk in range(4):
                            k.ts(dve, oh[:], iota[:], idxf[:, kk:kk + 1], None, ALU.is_equal, ALU.bypass, [iota, idxf], [oh])
                            k.tt(dve, oh[:], oh[:], dstf[:], ALU.mult, [oh, dstf], [oh])
                            k.op(dve, "tensor_reduce", [oh], [dsel], out=dsel[:, kk:kk + 1], in_=oh[:], axis=AX.X, op=ALU.add)
                        k.cp(dve, destt[:, tbg, :], dsel[:], [dsel], [destt])
                        for kk in range(4):
                            k.idma(XBUF[:, :], bass.IndirectOffsetOnAxis(ap=destt[:, tbg, kk:kk + 1], axis=0), h1b[:], None,
                                   reads=[h1b, destt])
                k.barrier()

        if stop_after >= 6 and 6 not in skip:
            with ExitStack() as st:
                bguT = TT(k.sb(st, "bguT", (128, 16, NEXP), F32))
                with ExitStack() as st2:
                    bgs = TT(k.sb(st2, "bgs", (NEXP, 2 * D), F32))
                    gtmp = Ring([TT(k.ps(st2, f"p6b{i}", (128, 512), F32)) for i in range(2)])
                    k.dma(sp, bgs[:], b_gate_up, writes=[bgs])
                    for c in range(16):
                        pt = gtmp.next()
                        k.op(pe, "transpose", [bgs, identf], [pt], out=pt[:, 0:NEXP], in_=bgs[:, c * 128:(c + 1) * 128], identity=identf[0:NEXP, 0:NEXP])
                        k.cp(dve, bguT[:, c, :], pt[:, 0:NEXP], [pt], [bguT])
                    k.barrier()
                wgur = Ring([TT(k.sb(st, f"wgu{i}", (128, 8, 2 * D), BF16)) for i in range(2)])
                wdr = Ring([TT(k.sb(st, f"wd{i}", (128, 8, D), BF16)) for i in range(2)])
                bdr = Ring([TT(k.sb(st, f"bd{i}", (128, D), F32)) for i in range(2)])
                stg = Ring([TT(k.sb(st, f"stg{i}", (128, D), F32)) for i in range(4)])
                xrr = Ring([TT(k.sb(st, f"xr{i}", (128, D), BF16)) for i in range(3)])
                xTs = [TT(k.sb(st, f"exT{i}", (128, 8, 512), BF16)) for i in range(2)]
                hTs = [TT(k.sb(st, f"ehT{i}", (128, 8, 512), BF16)) for i in range(2)]
                ggr = Ring([TT(k.sb(st, f"gg{i}", (128, 512), F32)) for i in range(2)])
                sgr = Ring([TT(k.sb(st, f"sg{i}", (128, 512), F32)) for i in range(2)])
                uur = Ring([TT(k.sb(st, f"uu{i}", (128, 512), F32)) for i in range(2)])
                orr = Ring([TT(k.sb(st, f"orow{i}", (128, D), F32)) for i in range(2)])
                ptr6 = Ring([TT(k.ps(st, f"p6t{i}", (128, 8, 128), BF16)) for i in range(2)])
                gur = Ring([TT(k.ps(st, f"p6g{i}", (128, 512), F32)) for i in range(4)])
                opr = Ring([TT(k.ps(st, f"p6o{i}", (128, 512), F32)) for i in range(2)])
                rgs = [(r0, min(512, CAP - r0)) for r0 in range(0, CAP, 512)]
                NR = len(rgs)

                def wjobs(e):
                    wgu = wgur.next(); wd = wdr.next(); bd = bdr.next()
                    jobs = []
                    for kc in range(8):
                        for hf in range(2):
                            jobs.append((wgu[:, kc, hf * D:(hf + 1) * D], w_gate_up[e, kc * 128:(kc + 1) * 128, hf * D:(hf + 1) * D], wgu))
                    for kc in range(8):
                        jobs.append((wd[:, kc, :], w_down[e, kc * 128:(kc + 1) * 128, :], wd))
                    return (wgu, wd, bd), jobs

                def run_loads(jobs):
                    pend = []
                    for (dst, src_ap, wt) in jobs:
                        sg_t = stg.next()
                        k.dma(sp, sg_t[:], src_ap, writes=[sg_t])
                        pend.append((dst, sg_t, wt))
                    return pend

                def run_casts(pend):
                    for (dst, sg_t, wt) in pend:
                        k.actv(dst, sg_t[:], AF.Copy, [sg_t], [wt])

                def chunks(lst, n):
                    per = (len(lst) + n - 1) // n
                    return [lst[i * per:(i + 1) * per] for i in range(n)]

                steps = [(e, ri) for e in range(NEXP) for ri in range(NR)]
                ws = {}
                ws[0], jobs0 = wjobs(0)
                for j0 in range(0, len(jobs0), 4):
                    run_casts(run_loads(jobs0[j0:j0 + 4]))
                k.dma(sp, ws[0][2][:], b_down[0:1, :].broadcast_to((128, D)), writes=[ws[0][2]])
                evc = [0]

                def stX(t):
                    e, ri = steps[t]
                    r0, nr = rgs[ri]
                    row0 = e * CAP + r0
                    xT = xTs[t % 2]
                    for b in range(nr // 128):
                        xr = xrr.next()
                        k.dma(act, xr[:], XBUF[row0 + b * 128:row0 + (b + 1) * 128, :], writes=[xr])
                        pt = ptr6.next()
                        for kc in range(8):
                            k.op(pe, "transpose", [xr, identb], [pt], out=pt[:, kc, :], in_=xr[:, kc * 128:(kc + 1) * 128], identity=identb[:])
                        evac(evc[0], xT[:, :, b * 128:(b + 1) * 128], pt[:], [pt], [xT]); evc[0] += 1

                def stGU(t, fcs):
                    e, ri = steps[t]
                    r0, nr = rgs[ri]
                    wgu = ws[e][0]
                    xT = xTs[t % 2]; hT = hTs[t % 2]
                    for fc in fcs:
                        gp = gur.next(); up = gur.next(); gg = ggr.next(); sg_ = sgr.next(); uu = uur.next()
                        for kc in range(8):
                            k.mm(gp[:, 0:nr], wgu[:, kc, fc * 128:(fc + 1) * 128], xT[:, kc, 0:nr], kc == 0, kc == 7, [wgu, xT], [gp])
                        for kc in range(8):
                            k.mm(up[:, 0:nr], wgu[:, kc, D + fc * 128:D + (fc + 1) * 128], xT[:, kc, 0:nr], kc == 0, kc == 7, [wgu, xT], [up])
                        k.ts(dve, gg[:, 0:nr], gp[:, 0:nr], bguT[:, fc, e:e + 1], 7.0, ALU.add, ALU.min, [gp, bguT], [gg])
                        k.actv(sg_[:, 0:nr], gg[:, 0:nr], AF.Sigmoid, [gg], [sg_], scale=1.702)
                        k.ts(dve, uu[:, 0:nr], up[:, 0:nr], bguT[:, 8 + fc, e:e + 1], 7.0, ALU.add, ALU.min, [up, bguT], [uu])
                        k.ts(dve, uu[:, 0:nr], uu[:, 0:nr], -7.0, 1.0, ALU.max, ALU.add, [uu], [uu])
                        k.tt(pool, gg[:, 0:nr], gg[:, 0:nr], sg_[:, 0:nr], ALU.mult, [gg, sg_], [gg])
                        k.tt(dve, hT[:, fc, 0:nr], gg[:, 0:nr], uu[:, 0:nr], ALU.mult, [gg, uu], [hT])

                def stDN(t):
                    e, ri = steps[t]
                    r0, nr = rgs[ri]
                    row0 = e * CAP + r0
                    wd, bd = ws[e][1], ws[e][2]
                    hT = hTs[t % 2]
                    for b in range(nr // 128):
                        orow = orr.next()
                        for half in range(2):
                            op_ = opr.next()
                            for fc in range(8):
                                k.mm(op_[:], hT[:, fc, b * 128:(b + 1) * 128], wd[:, fc, half * 512:(half + 1) * 512], fc == 0, fc == 7, [hT, wd], [op_])
                            k.tt(dve, orow[:, half * 512:(half + 1) * 512], op_[:], bd[:, half * 512:(half + 1) * 512], ALU.add, [op_, bd], [orow])
                        k.dma(sp, OBUF[row0 + b * 128:row0 + (b + 1) * 128, :], orow[:], reads=[orow])

                stX(0)
                jparts = None
                pendB = []
                for t, (e, ri) in enumerate(steps):
                    stGU(t, range(0, 4))
                    if t > 0:
                        stDN(t - 1)
                    run_casts(pendB); pendB = []
                    if ri == 0:
                        if e + 1 < NEXP:
                            ws[e + 1], njobs = wjobs(e + 1)
                            k.dma(sp, ws[e + 1][2][:], b_down[e + 1:e + 2, :].broadcast_to((128, D)), writes=[ws[e + 1][2]])
                            jparts = chunks(njobs, NR)
                        else:
                            jparts = [[] for _ in range(NR)]
                    jl = jparts[ri]
                    pendA = run_loads(jl[0:4])
                    if t + 1 < len(steps):
                        stX(t + 1)
                    stGU(t, range(4, 8))
                    run_casts(pendA)
                    pendB = run_loads(jl[4:8])
                    for j0 in range(8, len(jl), 4):
                        run_casts(pendB)
                        pendB = run_loads(jl[j0:j0 + 4])
                    if ri == NR - 1:
                        run_casts(pendB); pendB = []
                stDN(len(steps) - 1)
                k.barrier()

        if stop_after >= 7 and 7 not in skip:
            with ExitStack() as st:
                g2 = TT(k.sb(st, "g2", (128, D), F32)); b2 = TT(k.sb(st, "b2", (128, D), F32))
                k.dma(sp, g2[:], ln2_g[0:1, :].broadcast_to((128, D)), writes=[g2])
                k.dma(sp, b2[:], ln2_b[0:1, :].broadcast_to((128, D)), writes=[b2])
                rkr = Ring([TT(k.sb(st, f"rk{i}", (128, D), F32)) for i in range(8)])
                h1r = Ring([TT(k.sb(st, f"h7{i}", (128, D), F32)) for i in range(2)])
                yr = Ring([TT(k.sb(st, f"y7{i}", (128, D), F32)) for i in range(2)])
                rr = Ring([TT(k.sb(st, f"r7{i}", (128, D), F32)) for i in range(2)])
                stats = TT(k.sb(st, "stats7", (128, 2, 6), F32)); mv = TT(k.sb(st, "mv7", (128, 4), F32))
                for tb in range(NTB):
                    rows = slice(tb * 128, (tb + 1) * 128)
                    h1 = h1r.next(); y = yr.next(); res = rr.next()
                    k.dma(sp, h1[:], H1[rows, :], writes=[h1])
                    rks = []
                    for kk in range(4):
                        rk = rkr.next()
                        k.idma(rk[:], None, OBUF[:, :], bass.IndirectOffsetOnAxis(ap=destt[:, tb, kk:kk + 1], axis=0),
                               reads=[destt], writes=[rk])
                        rks.append(rk)
                    k.ts(dve, y[:], rks[0][:], w4t[:, tb, 0:1], None, ALU.mult, ALU.bypass, [rks[0], w4t], [y])
                    for kk in range(1, 4):
                        k.op(dve, "scalar_tensor_tensor", [rks[kk], w4t, y], [y], out=y[:], in0=rks[kk][:], scalar=w4t[:, tb, kk:kk + 1],
                             in1=y[:], op0=ALU.mult, op1=ALU.add)
                    k.op(dve, "scalar_tensor_tensor", [h1, y], [y], out=y[:], in0=h1[:], scalar=ALPHA, in1=y[:], op0=ALU.mult, op1=ALU.add)
                    layer_norm((stats, mv), y, g2, b2, res)
                    k.dma(sp, out[rows, :], res[:], reads=[res])
                k.barrier()

        k.barrier()
        k.emit()
    return nc


def make_in_map(xb, p, consts, fm, tm):
    w_in = p["w_in"][0]
    im = {"x": np.ascontiguousarray(xb), "wfm": np.ascontiguousarray(w_in[:, fm]), "wtm": np.ascontiguousarray(w_in[:, tm])}
    for kk, v in consts.items():
        im["c_" + kk] = v
    for nm in ("cmp_pe_k", "cmp_w1_k", "cmp_w2_k", "cmp_pe_v", "cmp_w1_v", "cmp_w2_v", "w_proj_sb", "w_proj_nsa", "w_out",
               "w_router", "w_gate_up", "b_gate_up", "w_down", "b_down"):
        im[nm] = np.ascontiguousarray(p[nm][0])
    for nm in ("ln1_g", "ln1_b", "ln2_g", "ln2_b", "b_router"):
        im[nm] = np.ascontiguousarray(p[nm][0:1])
    return im


_NC_CACHE = {}


def kernel(**inputs):
    p = {k_: np.asarray(v, dtype=np.float32) for k_, v in inputs.items()}
    x = p["x"]
    B, S, _ = x.shape
    if S not in _NC_CACHE:
        _NC_CACHE[S] = build(S=S, CAP=(1536 if S == 8192 else max(256, S // 4)))
    nc = _NC_CACHE[S]
    consts = host_consts(S)
    fm, tm = win_layout()
    in_maps = [make_in_map(x[b], p, consts, fm, tm) for b in range(B)]
    res = run_bass_kernel_spmd(nc, in_maps, core_ids=list(range(B)))
    return np.stack([np.asarray(r["out"], dtype=np.float32) for r in res.results], axis=0)
```

```python
import math
from contextlib import ExitStack

import numpy as np
import concourse.bass as bass
import concourse.mybir as mybir
from concourse.bass_utils import run_bass_kernel_spmd

F32 = mybir.dt.float32
BF16 = mybir.dt.bfloat16
I32 = mybir.dt.int32
U32 = mybir.dt.uint32
AF = mybir.ActivationFunctionType
ALU = mybir.AluOpType
AX = mybir.AxisListType

D = 1024
HD = 64
NH = 8
NEXP = 32
IN_W = 4888
ALPHA = 2.0 ** 0.25
EPS = 1e-5
NEGB = 30000.0


class Eng:
    def __init__(self, name, sem, is_pe=False):
        self.name = name
        self.sem = sem
        self.n = 0
        self.is_pe = is_pe
        self.prog = []
        self.waited = {}
        self.dma_i = 0


class TT:
    def __init__(self, ap):
        self.ap = ap
        self.w = None
        self.r = {}

    def __getitem__(self, idx):
        return self.ap[idx]


class KB:
    def __init__(self, nc, es):
        self.nc = nc
        self.es = es
        sem = lambda n: es.enter_context(nc.semaphore(n))
        self.pe = Eng("tensor", sem("s_pe"), True)
        self.act = Eng("scalar", sem("s_act"))
        self.dve = Eng("vector", sem("s_dve"))
        self.pool = Eng("gpsimd", sem("s_pool"))
        self.sp = Eng("sync", sem("s_sp"))
        self.engs = [self.pe, self.act, self.dve, self.pool, self.sp]
        NS = 6
        self.hw_sems = {e.name: [[sem(f"dh_{e.name}{i}"), 0] for i in range(NS)] for e in (self.sp, self.act)}
        self.sw_sems = [[sem(f"dsw{i}"), 0] for i in range(NS)]
        self.dma_out = {}
        self.sems_by_id = {}

    def _wait(self, eng, need):
        for sid, (s, v) in need.items():
            if eng.is_pe and s is eng.sem:
                continue
            if eng.waited.get(sid, 0) >= v:
                continue
            eng.waited[sid] = v
            eng.prog.append(lambda h, s=s, v=v: h.wait_ge(s, v))

    def _deps(self, reads, writes):
        need = {}

        def add(tok):
            if tok is None:
                return
            s, v = tok
            if id(s) not in need or need[id(s)][1] < v:
                need[id(s)] = (s, v)

        for t in reads:
            add(t.w)
        for t in writes:
            add(t.w)
            for tok in t.r.values():
                add(tok)
        return need

    def _mark(self, tok, reads, writes):
        for t in reads:
            t.r[id(tok[0])] = tok
        for t in writes:
            t.w = tok
            t.r = {}

    def op(self, eng, name, reads=(), writes=(), **kw):
        self._wait(eng, self._deps(reads, writes))
        eng.n += 1
        s = eng.sem
        eng.prog.append(lambda h, name=name, kw=kw, s=s: getattr(h, name)(**kw).then_inc(s, 1))
        tok = (s, eng.n)
        self._mark(tok, reads, writes)
        return tok

    def mm(self, out, lhsT, rhs, start, stop, reads, writes):
        return self.op(self.pe, "matmul", reads, writes, out=out, lhsT=lhsT, rhs=rhs, start=start, stop=stop)

    def actv(self, out, in_, func, reads, writes, **kw):
        return self.op(self.act, "activation", reads, writes, out=out, in_=in_, func=func, **kw)

    def tt(self, eng, out, in0, in1, op, reads, writes):
        return self.op(eng, "tensor_tensor", reads, writes, out=out, in0=in0, in1=in1, op=op)

    def ts(self, eng, out, in0, s1, s2, op0, op1, reads, writes, **kw):
        return self.op(eng, "tensor_scalar", reads, writes, out=out, in0=in0, scalar1=s1, scalar2=s2, op0=op0, op1=op1, **kw)

    def cp(self, eng, out, in_, reads, writes):
        return self.op(eng, "tensor_copy", reads, writes, out=out, in_=in_)

    def ms(self, eng, ap, val, writes):
        return self.op(eng, "memset", (), writes, ap=ap, constant=val)

    def dma(self, eng, out, in_, reads=(), writes=(), **kw):
        pool_ = self.sw_sems if eng is self.pool else self.hw_sems[eng.name]
        slot = pool_[eng.dma_i % len(pool_)]
        eng.dma_i += 1
        s = slot[0]
        need = self._deps(reads, writes)
        if slot[1] > 0:
            need[id(s)] = (s, slot[1])
        self._wait(eng, need)
        slot[1] += 16
        v = slot[1]
        eng.prog.append(lambda h, out=out, in_=in_, s=s, kw=kw: h.dma_start(out=out, in_=in_, **kw).then_inc(s, 16))
        tok = (s, v)
        self.dma_out[id(s)] = tok
        self._mark(tok, reads, writes)
        return tok

    def idma(self, out, out_off, in_, in_off, reads=(), writes=(), **kw):
        eng = self.pool
        slot = self.sw_sems[eng.dma_i % len(self.sw_sems)]
        eng.dma_i += 1
        s = slot[0]
        need = self._deps(reads, writes)
        if slot[1] > 0:
            need[id(s)] = (s, slot[1])
        self._wait(eng, need)
        slot[1] += 16
        v = slot[1]
        eng.prog.append(lambda h, out=out, out_off=out_off, in_=in_, in_off=in_off, kw=kw, s=s: h.indirect_dma_start(
            out=out, out_offset=out_off, in_=in_, in_offset=in_off, **kw).then_inc(s, 16))
        tok = (s, v)
        self.dma_out[id(s)] = tok
        self._mark(tok, reads, writes)
        return tok

    def barrier(self):
        need = {}
        for e in self.engs:
            if e.n > 0:
                need[id(e.sem)] = (e.sem, e.n)
        for sid, tok in self.dma_out.items():
            need[sid] = tok
        for e in self.engs:
            n2 = {k: v for k, v in need.items() if v[0] is not e.sem}
            saved = e.is_pe
            e.is_pe = False
            self._wait(e, n2)
            e.is_pe = saved

    def emit(self):
        nc = self.nc
        with nc.Block() as block:
            for e in self.engs:
                def body(h, e=e):
                    for f in e.prog:
                        f(h)
                getattr(block, e.name)(body)

    def sb(self, st, name, shape, dt):
        return st.enter_context(self.nc.sbuf_tensor(name, list(shape), dt))

    def ps(self, st, name, shape, dt):
        return st.enter_context(self.nc.psum_tensor(name, list(shape), dt))


class Ring:
    def __init__(self, items):
        self.items = items
        self.i = 0

    def next(self):
        t = self.items[self.i % len(self.items)]
        self.i += 1
        return t


def host_consts(S):
    c = {}
    c["ident"] = np.eye(128, dtype=np.float32)
    half = HD // 2
    inv_freq = (np.float32(10000.0) ** (-np.arange(half, dtype=np.float32) / np.float32(half))).astype(np.float32)
    ang = np.arange(S, dtype=np.float32)[:, None] * inv_freq[None, :]
    cos = np.cos(ang).astype(np.float32).T
    sin = np.sin(ang).astype(np.float32).T
    cos64 = np.concatenate([cos, cos], 0)
    sin64 = np.concatenate([-sin, sin], 0)
    c["cosT"] = np.ascontiguousarray(np.concatenate([cos64, cos64], 0))
    c["sinT"] = np.ascontiguousarray(np.concatenate([sin64, sin64], 0))
    j = np.arange(128)
    c["negU8"] = np.where(j[:, None] >= j[None, :], -8.0, 0.0).astype(np.float32)
    c["neg8"] = np.full((128, 128), -8.0, np.float32)
    m4 = np.zeros((128, 4, 4, 128), np.float32)
    strict = (j[:, None] < j[None, :]).astype(np.float32)
    for jb in range(4):
        for cb in range(4):
            if cb == jb:
                m4[:, jb, cb, :] = strict
            elif cb > jb:
                m4[:, jb, cb, :] = 1.0
    c["mask4"] = m4.reshape(128, 4 * 512)
    le = np.where(j[:, None] <= j[None, :], 0.0, -NEGB).astype(np.float32)
    gt = np.where(j[:, None] > j[None, :], 0.0, -NEGB).astype(np.float32)
    c["bias_le"] = np.tile(le, (1, 4))
    c["bias_gt"] = np.tile(gt, (1, 4))
    nsel = S // 64
    E = np.zeros((128, S), np.float32)
    E[np.arange(S) // 64, np.arange(S)] = 1.0
    c["Esel"] = E
    m = np.arange(-1, 7)
    c["cmask"] = (j[:, None] >= 16 * m[None, :] + 31).astype(np.float32)
    c["iota32"] = np.tile(np.arange(NEXP, dtype=np.float32)[None, :], (128, 1))
    c["ustrict"] = (j[:, None] < j[None, :]).astype(np.float32)
    c["ones"] = np.ones((128, 128), np.float32)
    sm = np.zeros((128, 64), np.float32)
    sm[64, :] = 1.0
    c["selM"] = sm
    return c


CONST_SHAPES = lambda S: {
    "ident": (128, 128), "cosT": (128, S), "sinT": (128, S), "negU8": (128, 128), "neg8": (128, 128),
    "mask4": (128, 2048), "bias_le": (128, 512), "bias_gt": (128, 512), "Esel": (128, S),
    "cmask": (128, 8), "iota32": (128, NEXP), "ustrict": (128, 128), "ones": (128, 128), "selM": (128, 64),
}

def win_layout():
    def sw(base, nheads):
        idx = []
        for h in range(nheads):
            for d in range(HD):
                idx.append(base + h * HD + (d + 32) % HD)
        return idx

    r = lambda a, b: list(range(a, b))
    fm = []
    fm += r(0, 512)
    fm += r(512, 1024)
    fm += r(1536, 2048)
    fm += sw(1536, 8)
    fm += r(2048, 2176) + sw(2048, 2)
    fm += r(2304, 2432) + sw(2304, 2)
    fm += r(2560, 2688) + sw(2560, 2)
    fm += r(2176, 2304)
    fm += r(2840, 4888)
    fm += r(2816, 2840)
    tm = r(1024, 1536) + r(2432, 2560) + r(2688, 2816)
    return np.array(fm), np.array(tm)


NFM = 5016
NTM = 768


def build(S=8192, CAP=1536, dbg=(), stop_after=99, skip=()):
    NTB = S // 128
    NG = S // 512
    NC = S // 16 - 1
    NSEL = S // 64
    NROWS = NEXP * CAP + 128
    TRASH = NEXP * CAP
    nc = bass.Bass("TRN2", target_bir_lowering=False)

    def din(name, shape, dt=F32):
        return nc.dram_tensor(name, list(shape), dt, kind="ExternalInput").ap()

    def dscr(name, shape, dt):
        kind = "ExternalOutput" if name in dbg else "Internal"
        return nc.dram_tensor(name, list(shape), dt, kind=kind).ap()

    x = din("x", (S, D))
    wfm = din("wfm", (D, NFM))
    wtm = din("wtm", (D, NTM))
    cst = {k: din("c_" + k, shp) for k, shp in CONST_SHAPES(S).items()}
    cmp_in = {k: din(k, shp) for k, shp in dict(
        cmp_pe_k=(32, 64), cmp_w1_k=(2048, 64), cmp_w2_k=(64, 64),
        cmp_pe_v=(32, 64), cmp_w1_v=(2048, 64), cmp_w2_v=(64, 64)).items()}
    w_proj_sb = din("w_proj_sb", (512, D))
    w_proj_nsa = din("w_proj_nsa", (512, D))
    w_out = din("w_out", (D, D))
    ln1_g = din("ln1_g", (1, D)); ln1_b = din("ln1_b", (1, D))
    ln2_g = din("ln2_g", (1, D)); ln2_b = din("ln2_b", (1, D))
    w_router = din("w_router", (D, NEXP)); b_router = din("b_router", (1, NEXP))
    w_gate_up = din("w_gate_up", (NEXP, D, 2 * D)); b_gate_up = din("b_gate_up", (NEXP, 2 * D))
    w_down = din("w_down", (NEXP, D, D)); b_down = din("b_down", (NEXP, D))
    out = nc.dram_tensor("out", [S, D], F32, kind="ExternalOutput").ap()

    QT = dscr("QT", (512, S), BF16); KT = dscr("KT", (512, S), BF16)
    NQT = dscr("NQT", (512, S), BF16)
    KCT = dscr("KCT", (128, S), BF16); VCT = dscr("VCT", (128, S), BF16)
    KST = dscr("KST", (128, S), BF16); KWT = dscr("KWT", (128, S), BF16)
    NGT = dscr("NGT", (24, S), F32); MGT = dscr("MGT", (2048, S), BF16)
    VTM = dscr("VTM", (S, NTM), BF16)
    KCC = dscr("KCC", (2, 64, 512), BF16); VCC = dscr("VCC", (512, 2, 64), BF16)
    YST = dscr("YST", (512, S), BF16); YNT = dscr("YNT", (512, S), BF16)
    H1 = dscr("H1", (S, D), F32)
    XBUF = dscr("XBUF", (NROWS, D), BF16)
    OBUF = dscr("OBUF", (NROWS, D), F32)

    with ExitStack() as es:
        k = KB(nc, es)
        pe, act, dve, pool, sp = k.pe, k.act, k.dve, k.pool, k.sp
        identb = TT(k.sb(es, "identb", (128, 128), BF16))
        identf = TT(k.sb(es, "identf", (128, 128), F32))
        destt = TT(k.sb(es, "destt", (128, NTB, 4), I32))
        w4t = TT(k.sb(es, "w4t", (128, NTB, 4), F32))
        k.dma(pool, identb[:], cst["ident"], writes=[identb])
        k.dma(sp, identf[:], cst["ident"], writes=[identf])

        def evac(i, out_ap, in_ap, reads, writes):
            if i % 2 == 0:
                return k.actv(out_ap, in_ap, AF.Copy, reads, writes)
            return k.cp(dve, out_ap, in_ap, reads, writes)

        if stop_after >= 1:
            with ExitStack() as st:
                zt = TT(k.sb(st, "zt", (128, 4, D), BF16))
                zf = TT(k.sb(st, "zf", (128, D), F32))
                k.ms(pool, zt[:], 0.0, [zt])
                k.ms(pool, zf[:], 0.0, [zf])
                k.dma(act, OBUF[TRASH:TRASH + 128, :], zf[:], reads=[zf])
                nblk = NROWS // 128
                for b0 in range(0, nblk, 4):
                    nb = min(4, nblk - b0)
                    k.dma(act, XBUF[b0 * 128:(b0 + nb) * 128, :].rearrange("(b p) d -> p b d", p=128), zt[:, 0:nb, :], reads=[zt])
                wfb = TT(k.sb(st, "wfb", (128, 8, NFM), BF16))
                wtb = TT(k.sb(st, "wtb", (128, 8, NTM), BF16))
                for kc in range(8):
                    k.dma(pool, wfb[:, kc, :], wfm[kc * 128:(kc + 1) * 128, :], writes=[wfb])
                    k.dma(pool, wtb[:, kc, :], wtm[kc * 128:(kc + 1) * 128, :], writes=[wtb])
                xbs = Ring([TT(k.sb(st, f"xb{i}", (128, 4, D), BF16)) for i in range(2)])
                xTs = Ring([TT(k.sb(st, f"xT{i}", (128, 8, 512), BF16)) for i in range(2)])
                cosr = Ring([TT(k.sb(st, f"cos{i}", (128, 512), F32)) for i in range(2)])
                sinr = Ring([TT(k.sb(st, f"sin{i}", (128, 512), F32)) for i in range(2)])
                obr = Ring([TT(k.sb(st, f"ob{i}", (128, 512), BF16)) for i in range(6)])
                ofr = Ring([TT(k.sb(st, f"of{i}", (128, 512), F32)) for i in range(4)])
                otr = Ring([TT(k.sb(st, f"ot{i}", (128, NTM), BF16)) for i in range(2)])
                ptr = Ring([TT(k.ps(st, f"ptr{i}", (128, 8, 128), BF16)) for i in range(2)])
                accr = Ring([TT(k.ps(st, f"acc{i}", (128, 512), F32)) for i in range(6)])
                ev = 0
                for g in range(NG):
                    xb = xbs.next(); xT = xTs.next(); cs = cosr.next(); sn = sinr.next()
                    gs = slice(g * 512, (g + 1) * 512)
                    k.dma(pool, xb[:], x[gs, :].rearrange("(tb p) d -> p tb d", p=128), writes=[xb])
                    k.dma(sp, cs[:], cst["cosT"][:, gs], writes=[cs])
                    k.dma(sp, sn[:], cst["sinT"][:, gs], writes=[sn])
                    for tb in range(4):
                        pt = ptr.next()
                        for kc in range(8):
                            k.op(pe, "transpose", [xb, identb], [pt], out=pt[:, kc, :],
                                 in_=xb[:, tb, kc * 128:(kc + 1) * 128], identity=identb[:])
                        evac(ev, xT[:, :, tb * 128:(tb + 1) * 128], pt[:], [pt], [xT]); ev += 1

                    def fm_mm(chunk, xT, width=128):
                        acc = accr.next()
                        for kc in range(8):
                            k.mm(acc[0:width, :], wfb[:, kc, chunk * 128:chunk * 128 + width], xT[:, kc, :],
                                 kc == 0, kc == 7, [wfb, xT], [acc])
                        return acc

                    for chunk, dst, r0 in [(c, QT, c * 128) for c in range(4)] + [(4 + c, KT, c * 128) for c in range(4)] + [(22, VCT, 0)]:
                        acc = fm_mm(chunk, xT)
                        ob = obr.next()
                        evac(ev, ob[:], acc[:], [acc], [ob]); ev += 1
                        k.dma(sp, dst[r0:r0 + 128, gs], ob[:], reads=[ob])
                    for ca, cb_, dst, r0 in [(8 + c, 12 + c, NQT, c * 128) for c in range(4)] + [(16, 17, KCT, 0), (18, 19, KST, 0), (20, 21, KWT, 0)]:
                        a = fm_mm(ca, xT); b = fm_mm(cb_, xT)
                        t1 = ofr.next(); t2 = ofr.next(); ob = obr.next()
                        k.tt(dve, t1[:], a[:], cs[:], ALU.mult, [a, cs], [t1])
                        k.tt(dve, t2[:], b[:], sn[:], ALU.mult, [b, sn], [t2])
                        k.tt(pool, ob[:], t1[:], t2[:], ALU.add, [t1, t2], [ob])
                        k.dma(sp, dst[r0:r0 + 128, gs], ob[:], reads=[ob])
                    for c in range(16):
                        acc = fm_mm(23 + c, xT)
                        ob = obr.next()
                        k.actv(ob[:], acc[:], AF.Sigmoid, [acc], [ob])
                        k.dma(sp, MGT[c * 128:(c + 1) * 128, gs], ob[:], reads=[ob])
                    acc = fm_mm(39, xT, 24)
                    of = ofr.next()
                    k.actv(of[0:24, :], acc[0:24, :], AF.Sigmoid, [acc], [of])
                    k.dma(sp, NGT[0:24, gs], of[0:24, :], reads=[of])
                    for tb in range(4):
                        ot = otr.next()
                        for (c0, c1) in ((0, 512), (512, 768)):
                            acc = accr.next()
                            for kc in range(8):
                                k.mm(acc[:, 0:c1 - c0], xT[:, kc, tb * 128:(tb + 1) * 128], wtb[:, kc, c0:c1],
                                     kc == 0, kc == 7, [wtb, xT], [acc])
                            evac(ev, ot[:, c0:c1], acc[:, 0:c1 - c0], [acc], [ot]); ev += 1
                        r0 = g * 512 + tb * 128
                        k.dma(sp, VTM[r0:r0 + 128, :], ot[:], reads=[ot])
                k.barrier()

        if stop_after >= 2 and 2 not in skip:
            with ExitStack() as st:
                w1 = TT(k.sb(st, "cw1", (64, 32, 64), BF16))
                w2 = TT(k.sb(st, "cw2", (64, 64), BF16))
                pe_f = TT(k.sb(st, "cpe", (32, 64), F32))
                peT = TT(k.sb(st, "cpeT", (64, 32), BF16))
                cbs = TT(k.sb(st, "ccb", (64, 1), F32))
                src = TT(k.sb(st, "csrc", (64, S), BF16))
                u = TT(k.sb(st, "cu", (64, 512), F32))
                u2 = TT(k.sb(st, "cu2", (64, 512), F32))
                sg = TT(k.sb(st, "csg", (64, 512), F32))
                gl = TT(k.sb(st, "cgl", (64, 512), BF16))
                ko = TT(k.sb(st, "cko", (128, 512), BF16))
                hid = TT(k.ps(st, "chid", (64, 512), F32))
                pp = TT(k.ps(st, "cpp", (128, 512), F32))
                for which in ("k", "v"):
                    k.dma(pool, w1[:], cmp_in["cmp_w1_" + which].rearrange("(j d) h -> d j h", d=64), writes=[w1])
                    k.dma(pool, w2[:], cmp_in["cmp_w2_" + which], writes=[w2])
                    k.dma(sp, pe_f[:], cmp_in["cmp_pe_" + which], writes=[pe_f])
                    k.op(pe, "transpose", [pe_f, identf], [pp], out=pp[0:64, 0:32], in_=pe_f[:], identity=identf[0:32, 0:32])
                    k.cp(dve, peT[:], pp[0:64, 0:32], [pp], [peT])
                    for j in range(32):
                        k.mm(pp[0:64, 0:1], w1[:, j, :], peT[:, j:j + 1], j == 0, j == 31, [w1, peT], [pp])
                    k.cp(dve, cbs[:], pp[0:64, 0:1], [pp], [cbs])
                    for g in range(2):
                        k.dma(sp, src[:], (KCT if which == "k" else VCT)[g * 64:(g + 1) * 64, :], writes=[src])
                        for j in range(32):
                            k.mm(hid[:, 0:NC], w1[:, j, :], src[:, j:j + 16 * (NC - 1) + 1:16], j == 0, j == 31, [w1, src], [hid])
                        k.actv(u[:, 0:NC], hid[:, 0:NC], AF.Identity, [hid, cbs], [u], bias=cbs[:, 0:1])
                        k.tt(dve, u2[:, 0:NC], u[:, 0:NC], u[:, 0:NC], ALU.mult, [u], [u2])
                        k.ts(dve, u2[:, 0:NC], u2[:, 0:NC], 0.044715, 1.0, ALU.mult, ALU.add, [u2], [u2])
                        k.tt(dve, u2[:, 0:NC], u2[:, 0:NC], u[:, 0:NC], ALU.mult, [u2, u], [u2])
                        k.actv(sg[:, 0:NC], u2[:, 0:NC], AF.Sigmoid, [u2], [sg], scale=1.5957691216057308)
                        k.tt(dve, gl[:, 0:NC], u[:, 0:NC], sg[:, 0:NC], ALU.mult, [u, sg], [gl])
                        if which == "k":
                            k.mm(pp[0:64, 0:NC], w2[:], gl[:, 0:NC], True, True, [w2, gl], [pp])
                            k.cp(dve, ko[0:64, 0:NC], pp[0:64, 0:NC], [pp], [ko])
                            k.dma(sp, KCC[g, :, 0:NC], ko[0:64, 0:NC], reads=[ko])
                        else:
                            for c in range((NC + 127) // 128):
                                w = min(128, NC - c * 128)
                                k.mm(pp[0:w, 0:64], gl[:, c * 128:c * 128 + w], w2[:], True, True, [w2, gl], [pp])
                                k.cp(dve, ko[0:w, 0:64], pp[0:w, 0:64], [pp], [ko])
                                k.dma(sp, VCC[c * 128:c * 128 + w, g, :], ko[0:w, 0:64], reads=[ko])
                k.barrier()

        if stop_after >= 3 and 3 not in skip:
            with ExitStack() as st:
                NCH = (NC + 127) // 128
                esel = TT(k.sb(st, "esel", (128, S), BF16))
                ble = TT(k.sb(st, "ble", (128, 512), BF16))
                bgt = TT(k.sb(st, "bgt", (128, 512), BF16))
                cmask = TT(k.sb(st, "cmask", (128, 8), F32))
                selM = TT(k.sb(st, "selM", (128, 128), F32))
                k.dma(pool, esel[:], cst["Esel"], writes=[esel])
                k.dma(pool, ble[:], cst["bias_le"], writes=[ble])
                k.dma(pool, bgt[:], cst["bias_gt"], writes=[bgt])
                k.dma(sp, cmask[:], cst["cmask"], writes=[cmask])
                k.ms(pool, selM[:], 0.0, [selM])
                k.dma(sp, selM[:, 0:64], cst["selM"], writes=[selM])
                kst = TT(k.sb(st, "kst", (128, S), BF16))
                kwt = TT(k.sb(st, "kwt", (128, S), BF16))
                kcc = TT(k.sb(st, "kcc", (128, 512), BF16))
                vcc = TT(k.sb(st, "vcc", (128, NCH, 128), BF16))
                vs = TT(k.sb(st, "vs", (128, NTB, 128), BF16))
                vw = TT(k.sb(st, "vw", (128, NTB, 128), BF16))
                qtr = Ring([TT(k.sb(st, f"nq{i}", (128, 4, 128), BF16)) for i in range(3)])
                for t_ in qtr.items:
                    k.ms(pool, t_[64:128, :, :], 0.0, [t_])
                gtr = Ring([TT(k.sb(st, f"gt{i}", (65, 3, 512), F32)) for i in range(3)])
                impP = TT(k.sb(st, "impP", (128, 528), F32))
                Ps = [TT(k.sb(st, f"P{i}", (128, 512), F32)) for i in range(4)]
                Pbs = [TT(k.sb(st, f"Pb{i}", (128, 512), BF16)) for i in range(4)]
                rss = [TT(k.sb(st, f"rs{i}", (128, 2), F32)) for i in range(4)]
                pbT = TT(k.sb(st, "pbT", (128, NCH, 4, 128), BF16))
                score = TT(k.sb(st, "score", (128, 128), F32))
                sc2 = TT(k.sb(st, "sc2", (128, 128), F32))
                m8 = TT(k.sb(st, "m8", (128, 16), F32))
                selm = TT(k.sb(st, "selm", (128, 128), F32))
                nbb = TT(k.sb(st, "nbb", (128, 128), BF16))
                nmTr = Ring([TT(k.sb(st, f"nmT{i}", (128, 4, 128), BF16)) for i in range(2)])
                pTr = Ring([TT(k.sb(st, f"pT{i}", (128, 512), BF16)) for i in range(4)])
                Rr = Ring([TT(k.sb(st, f"R{i}", (128, 512), F32)) for i in range(3)])
                for t_ in Rr.items:
                    k.ms(pool, t_[:], 0.0, [t_])
                bcs = TT(k.sb(st, "bcs", (64, 512), F32))
                yn = TT(k.sb(st, "yn", (64, 512), F32))
                ynb_r = Ring([TT(k.sb(st, f"ynb{i}", (64, 512), BF16)) for i in range(2)])
                zr = Ring([TT(k.ps(st, f"nz{i}", (128, 512), F32)) for i in range(3)])
                tpr = Ring([TT(k.ps(st, f"ntp{i}", (128, 128), BF16)) for i in range(2)])
                obank = {b: TT(k.ps(st, f"nob{b}", (128, 512), F32)) for b in range(3)}

                class Blk:
                    pass

                for g in range(2):
                    k.ms(pool, kst[64:128, :], 0.0, [kst])
                    k.dma(sp, kst[0:64, :], KST[g * 64:(g + 1) * 64, :], writes=[kst])
                    k.ms(pool, kwt[64:128, :], 0.0, [kwt])
                    k.dma(sp, kwt[0:64, :], KWT[g * 64:(g + 1) * 64, :], writes=[kwt])
                    k.ms(pool, kcc[:], 0.0, [kcc])
                    k.dma(sp, kcc[0:64, 0:NC], KCC[g, :, 0:NC], writes=[kcc])
                    k.ms(pool, vcc[:], 0.0, [vcc])
                    k.ms(pool, pbT[:], 0.0, [pbT])
                    for t_ in Pbs:
                        k.ms(pool, t_[:], 0.0, [t_])
                    for c in range(NCH):
                        w = min(128, NC - c * 128)
                        k.dma(sp, vcc[0:w, c, 0:64], VCC[c * 128:c * 128 + w, g, :], writes=[vcc])
                    k.ms(pool, vs[:, :, 64:128], 0.0, [vs])
                    k.ms(pool, vw[:, :, 64:128], 0.0, [vw])
                    k.ms(pool, vs[:, :, 64:65], 1.0, [vs])
                    k.ms(pool, vw[:, :, 64:65], 1.0, [vw])
                    for t0 in range(0, NTB, 16):
                        t1_ = min(NTB, t0 + 16)
                        k.dma(sp, vs[:, t0:t1_, 0:64], VTM[t0 * 128:t1_ * 128, 512 + g * 64:512 + (g + 1) * 64].rearrange("(tb p) d -> p tb d", p=128), writes=[vs])
                        k.dma(sp, vw[:, t0:t1_, 0:64], VTM[t0 * 128:t1_ * 128, 640 + g * 64:640 + (g + 1) * 64].rearrange("(tb p) d -> p tb d", p=128), writes=[vw])

                    def part1(i):
                        b_ = Blk(); b_.i = i
                        qs_ = slice(i * 128, (i + 1) * 128)
                        b_.qs_ = qs_
                        qt = qtr.next(); gt = gtr.next()
                        b_.qt, b_.gt = qt, gt
                        k.dma(sp, qt[0:64, :, :], NQT[g * 256:(g + 1) * 256, qs_].rearrange("(h d) q -> d h q", d=64), writes=[qt])
                        k.dma(sp, gt[64:65, :, :].rearrange("o b (h q) -> o b h q", h=4),
                              NGT[g * 12:(g + 1) * 12, qs_].rearrange("(o h b) q -> o b h q", o=1, b=3), writes=[gt])
                        b_.qflat = qt[:].rearrange("d h q -> d (h q)")
                        ncols = min(8 * i + 7, NC)
                        b_.ncols = ncols
                        b_.nch = (ncols + 127) // 128
                        k.ms(pool, impP[:], 0.0, [impP])
                        for hh in range(4):
                            z = zr.next()
                            k.mm(z[:, 0:ncols], qt[:, hh, :], kcc[:, 0:ncols], True, True, [qt, kcc], [z])
                            k.actv(Ps[hh][:, 0:ncols], z[:, 0:ncols], AF.Exp, [z], [Ps[hh]], scale=0.125)
                        for hh in range(4):
                            P = Ps[hh]
                            if i >= 1:
                                k.tt(dve, P[:, ncols - 8:ncols], P[:, ncols - 8:ncols], cmask[:, 0:8], ALU.mult, [P, cmask], [P])
                            else:
                                k.tt(dve, P[:, 0:7], P[:, 0:7], cmask[:, 1:8], ALU.mult, [P, cmask], [P])
                        for hh in range(4):
                            k.op(dve, "tensor_reduce", [Ps[hh]], [rss[hh]], out=rss[hh][:, 0:1], in_=Ps[hh][:, 0:ncols], axis=AX.X, op=ALU.add)
                        for hh in range(4):
                            k.ts(dve, rss[hh][:, 0:1], rss[hh][:, 0:1], 1e-30, None, ALU.add, ALU.bypass, [rss[hh]], [rss[hh]])
                        for hh in range(4):
                            k.op(dve, "reciprocal", [rss[hh]], [rss[hh]], out=rss[hh][:, 1:2], in_=rss[hh][:, 0:1])
                        for hh in range(4):
                            k.ts(dve, Pbs[hh][:, 0:ncols], Ps[hh][:, 0:ncols], rss[hh][:, 1:2], None, ALU.mult, ALU.bypass, [Ps[hh], rss[hh]], [Pbs[hh]])
                        for hh in range(4):
                            k.op(dve, "scalar_tensor_tensor", [Ps[hh], rss[hh], impP], [impP], out=impP[:, 1:1 + ncols], in0=Ps[hh][:, 0:ncols],
                                 scalar=rss[hh][:, 1:2], in1=impP[:, 1:1 + ncols], op0=ALU.mult, op1=ALU.add)
                        k.tt(dve, score[:, 0:NSEL], impP[:, 0:4 * NSEL:4], impP[:, 1:1 + 4 * NSEL:4], ALU.add, [impP], [score])
                        for m in (2, 3, 4):
                            k.tt(dve, score[:, 0:NSEL], score[:, 0:NSEL], impP[:, m:m + 4 * NSEL:4], ALU.add, [impP, score], [score])
                        if 2 * i + 2 < NSEL:
                            k.ms(dve, score[:, 2 * i + 2:NSEL], -1.0, [score])
                        k.ms(dve, score[:, 0:1], 100.0, [score])
                        k.ms(dve, score[:, 2 * i:2 * i + 1], 100.0, [score])
                        k.ms(dve, score[0:64, 2 * i + 1:2 * i + 2], -1.0, [score])
                        k.ms(dve, score[64:128, 2 * i + 1:2 * i + 2], 100.0, [score])
                        if i >= 1:
                            k.ms(dve, score[0:64, 2 * i - 1:2 * i], 100.0, [score])
                        k.op(dve, "max", [score], [m8], out=m8[:, 0:8], in_=score[:, 0:NSEL])
                        k.op(dve, "match_replace", [m8, score], [sc2], out=sc2[:, 0:NSEL], in_to_replace=m8[:, 0:8],
                             in_values=score[:, 0:NSEL], imm_value=-2.0)
                        k.op(dve, "max", [sc2], [m8], out=m8[:, 8:16], in_=sc2[:, 0:NSEL])
                        k.ts(dve, selm[:, 0:NSEL], score[:, 0:NSEL], m8[:, 15:16], None, ALU.is_ge, ALU.bypass, [score, m8], [selm])
                        k.op(dve, "scalar_tensor_tensor", [score, selm], [selm], out=selm[:, 0:NSEL], in0=score[:, 0:NSEL],
                             scalar=-0.5, in1=selm[:, 0:NSEL], op0=ALU.is_gt, op1=ALU.mult)
                        k.ms(pool, nbb[:], 0.0, [nbb])
                        k.ts(dve, nbb[:, 0:NSEL], selm[:, 0:NSEL], -1.0, NEGB, ALU.add, ALU.mult, [selm], [nbb])
                        return b_

                    def part2(b_):
                        ncols, nch = b_.ncols, b_.nch
                        for hh in range(4):
                            for c in range(nch):
                                tp = tpr.next()
                                k.op(pe, "transpose", [Pbs[hh], identb], [tp], out=tp[:, :], in_=Pbs[hh][:, c * 128:(c + 1) * 128], identity=identb[:])
                                if (hh + c) % 2:
                                    k.cp(dve, pbT[:, c, hh, :], tp[:, :], [tp], [pbT])
                                else:
                                    k.actv(pbT[:, c, hh, :], tp[:, :], AF.Copy, [tp], [pbT])
                        ocp = obank[0]
                        for c in range(nch):
                            k.mm(ocp[:, :], vcc[:, c, :], pbT[:, c, :, :].rearrange("n h q -> n (h q)"), c == 0, c == nch - 1, [vcc, pbT], [ocp])
                        tp = tpr.next()
                        k.op(pe, "transpose", [nbb, identb], [tp], out=tp[:, :], in_=nbb[:, :], identity=identb[:])
                        nmT = nmTr.next()
                        b_.nmT = nmT
                        for hh in range(4):
                            if hh % 2:
                                k.cp(dve, nmT[:, hh, :], tp[:, :], [tp], [nmT])
                            else:
                                k.actv(nmT[:, hh, :], tp[:, :], AF.Copy, [tp], [nmT])
                        b_.nmflat = nmT[:].rearrange("j h q -> j (h q)")

                    def att(b_):
                        i = b_.i
                        items = []
                        kbs = [kb for kb in range(i - 4, i + 1) if kb >= 0]
                        for n_i, kb in enumerate(kbs):
                            items.append(("w", kb, n_i == 0, n_i == len(kbs) - 1))
                        for kb in range(i + 1):
                            items.append(("s", kb, kb == 0, kb == i))

                        def sA(it):
                            kind, kb, first, last = it
                            z = zr.next(); pT = pTr.next()
                            ksl = slice(kb * 128, (kb + 1) * 128)
                            if kind == "s":
                                k.mm(z[:], kst[:, ksl], b_.qflat, True, False, [kst, b_.qt], [z])
                                k.mm(z[:], esel[0:NSEL, ksl], b_.nmflat[0:NSEL, :], False, kb != i, [esel, b_.nmT], [z])
                                if kb == i:
                                    k.mm(z[:], identb[:], ble[:], False, True, [identb, ble], [z])
                            else:
                                edge = (kb == i) or (kb == i - 4)
                                k.mm(z[:], kwt[:, ksl], b_.qflat, True, not edge, [kwt, b_.qt], [z])
                                if kb == i:
                                    k.mm(z[:], identb[:], ble[:], False, True, [identb, ble], [z])
                                elif kb == i - 4:
                                    k.mm(z[:], identb[:], bgt[:], False, True, [identb, bgt], [z])
                            k.actv(pT[:], z[:], AF.Exp, [z], [pT], scale=0.125)
                            return pT

                        def sC(it, pT):
                            kind, kb, first, last = it
                            if kind == "s":
                                k.mm(obank[1][:], vs[:, kb, :], pT[:], first, last, [vs, pT], [obank[1]])
                            else:
                                k.mm(obank[2][:], vw[:, kb, :], pT[:], first, last, [vw, pT], [obank[2]])

                        pts = {}
                        n_ = len(items)
                        for s_ in range(n_ + 2):
                            if s_ < n_:
                                pts[s_] = sA(items[s_])
                            if 0 <= s_ - 2 < n_:
                                sC(items[s_ - 2], pts.pop(s_ - 2))

                    def combine(b_):
                        gt = b_.gt
                        for b in range(3):
                            R_ = Rr.next()
                            ob = obank[b]
                            if b == 0:
                                k.actv(R_[0:64, :], ob[0:64, :], AF.Copy, [ob], [R_])
                                k.cp(dve, R_[64:65, :], gt[64:65, 0, :], [gt], [R_])
                            else:
                                k.actv(R_[0:65, :], ob[0:65, :], AF.Copy, [ob], [R_])
                                k.op(dve, "reciprocal", [R_], [R_], out=R_[64:65, :], in_=R_[64:65, :])
                                k.tt(dve, R_[64:65, :], R_[64:65, :], gt[64:65, b, :], ALU.mult, [R_, gt], [R_])
                            z = zr.next()
                            k.mm(z[:, :], selM[:, :], R_[:, :], True, True, [selM, R_], [z])
                            if b == 0:
                                k.tt(dve, yn[:], R_[0:64, :], z[0:64, :], ALU.mult, [R_, z], [yn])
                            else:
                                k.tt(dve, bcs[:], R_[0:64, :], z[0:64, :], ALU.mult, [R_, z], [bcs])
                                k.tt(pool, yn[:], yn[:], bcs[:], ALU.add, [yn, bcs], [yn])
                        ynb = ynb_r.next()
                        k.cp(pool, ynb[:], yn[:], [yn], [ynb])
                        k.dma(sp, YNT[g * 256:(g + 1) * 256, b_.qs_].rearrange("(h d) q -> d h q", d=64),
                              ynb[:].rearrange("d (h q) -> d h q", h=4), reads=[ynb])

                    cur = part1(0)
                    part2(cur)
                    for i in range(NTB):
                        nxt = part1(i + 1) if i + 1 < NTB else None
                        att(cur)
                        combine(cur)
                        if nxt is not None:
                            part2(nxt)
                        cur = nxt
                k.barrier()

        if stop_after >= 4 and 4 not in skip:
            with ExitStack() as st:
                negU = TT(k.sb(st, "negU", (128, 128), BF16))
                neg8 = TT(k.sb(st, "neg8", (128, 128), BF16))
                mask4 = TT(k.sb(st, "mask4", (128, 4, 512), BF16))
                k.dma(pool, negU[:], cst["negU8"], writes=[negU])
                k.dma(pool, neg8[:], cst["neg8"], writes=[neg8])
                k.dma(pool, mask4[:], cst["mask4"].rearrange("p (a b) -> p a b", a=4), writes=[mask4])
                ktr = Ring([TT(k.sb(st, f"kt{i}", (128, S), BF16)) for i in range(2)])
                qtr = Ring([TT(k.sb(st, f"qt{i}", (128, S), BF16)) for i in range(2)])
                vr = Ring([TT(k.sb(st, f"vv{i}", (128, NTB, 128), BF16)) for i in range(2)])
                for t_ in ktr.items + qtr.items:
                    k.ms(pool, t_[64:128, :], 0.0, [t_])
                for t_ in vr.items:
                    k.ms(pool, t_[:, :, 64:128], 0.0, [t_])
                er = Ring([TT(k.sb(st, f"e{i}", (128, 512), F32)) for i in range(3)])
                spr = Ring([TT(k.sb(st, f"sp{i}", (128, 512), BF16)) for i in range(5)])
                ar = Ring([TT(k.sb(st, f"a{i}", (128, 512), BF16)) for i in range(4)])
                acc32r = Ring([TT(k.sb(st, f"acc32{i}", (128, 512), F32)) for i in range(2)])
                accbr = Ring([TT(k.sb(st, f"accb{i}", (128, 512), BF16)) for i in range(4)])
                yor = Ring([TT(k.sb(st, f"yo{i}", (64, 512), BF16)) for i in range(2)])
                zar = Ring([TT(k.ps(st, f"za{i}", (128, 512), F32)) for i in range(3)])
                zbr = Ring([TT(k.ps(st, f"zb{i}", (128, 512), F32)) for i in range(3)])
                orr = Ring([TT(k.ps(st, f"o{i}", (128, 512), F32)) for i in range(2)])

                class Tl:
                    pass

                tiles = []
                heads_ld = {}
                for hh in range(NH):
                    for G in range(NG):
                        kbs = list(range(4 * G + 3, -1, -1))
                        grp = Tl(); grp.hh = hh; grp.G = G
                        for n_i, kb in enumerate(kbs):
                            t = Tl(); t.grp = grp; t.n_i = n_i; t.kb = kb; t.last = (n_i == len(kbs) - 1)
                            t.jb = kb - 4 * G
                            tiles.append(t)

                def load_head(hh):
                    kt = ktr.next(); qt = qtr.next(); vv = vr.next()
                    k.dma(sp, kt[0:64, :], KT[hh * 64:(hh + 1) * 64, :], writes=[kt])
                    k.dma(sp, qt[0:64, :], QT[hh * 64:(hh + 1) * 64, :], writes=[qt])
                    for t0 in range(0, NTB, 16):
                        t1_ = min(NTB, t0 + 16)
                        k.dma(sp, vv[:, t0:t1_, 0:64], VTM[t0 * 128:t1_ * 128, hh * 64:(hh + 1) * 64].rearrange("(tb p) d -> p tb d", p=128), writes=[vv])
                    heads_ld[hh] = (kt, qt, vv)

                def stA(t):
                    g_ = t.grp
                    kt, qt, vv = heads_ld[g_.hh]
                    if t.n_i == 0:
                        g_.ob = orr.next(); g_.acc32 = acc32r.next(); g_.accb = accbr.next()
                        k.ms(pool, g_.acc32[:], 0.0, [g_.acc32])
                        k.ms(pool, g_.accb[:], 0.0, [g_.accb])
                    z = zar.next(); e = er.next(); t.spt = spr.next()
                    t.qs = qt[:, g_.G * 512:(g_.G + 1) * 512]
                    t.ks = kt[:, t.kb * 128:(t.kb + 1) * 128]
                    t.kt, t.qt, t.vv = kt, qt, vv
                    k.mm(z[:], t.ks, t.qs, True, True, [kt, qt], [z])
                    k.actv(e[:], z[:], AF.Exp, [z], [e], scale=0.125)
                    k.actv(t.spt[:], e[:], AF.Ln, [e], [t.spt], bias=1.0)
                    if t.jb >= 0:
                        k.tt(pool, t.spt[:], t.spt[:], mask4[:, t.jb, :], ALU.mult, [t.spt, mask4], [t.spt])

                def stB(t):
                    g_ = t.grp
                    z = zbr.next(); t.a = ar.next()
                    k.mm(z[:], t.ks, t.qs, True, False, [t.kt, t.qt], [z])
                    k.mm(z[:], negU[:], t.spt[:], False, False, [negU, t.spt], [z])
                    k.mm(z[:], neg8[:], g_.accb[:], False, True, [neg8, g_.accb], [z])
                    k.actv(t.a[:], z[:], AF.Exp, [z], [t.a], scale=0.125)
                    if t.jb >= 0:
                        k.tt(pool, t.a[:], t.a[:], mask4[:, t.jb, :], ALU.mult, [t.a, mask4], [t.a])
                    if not t.last:
                        k.tt(dve, g_.acc32[:], g_.acc32[:], t.spt[:], ALU.add, [g_.acc32, t.spt], [g_.acc32])
                        g_.accb = accbr.next()
                        k.cp(dve, g_.accb[:], g_.acc32[:], [g_.acc32], [g_.accb])

                def stC(t):
                    g_ = t.grp
                    k.mm(g_.ob[:], t.vv[:, t.kb, :], t.a[:], t.n_i == 0, t.last, [t.vv, t.a], [g_.ob])
                    if t.last:
                        yo = yor.next()
                        k.cp(dve, yo[:], g_.ob[0:64, :], [g_.ob], [yo])
                        k.dma(sp, YST[g_.hh * 64:(g_.hh + 1) * 64, g_.G * 512:(g_.G + 1) * 512], yo[:], reads=[yo])

                N_ = len(tiles)
                load_head(0)
                if NH > 1:
                    load_head(1)
                for s_ in range(N_ + 3):
                    if s_ < N_:
                        stA(tiles[s_])
                    if 0 <= s_ - 2 < N_:
                        stB(tiles[s_ - 2])
                    if 0 <= s_ - 3 < N_:
                        tc_ = tiles[s_ - 3]
                        stC(tc_)
                        if tc_.last and tc_.grp.G == NG - 1 and tc_.grp.hh + 2 < NH:
                            load_head(tc_.grp.hh + 2)
                k.barrier()

        def layer_norm(st_tiles, v, gam, bet, res):
            stats, mv = st_tiles
            for c in range(2):
                k.op(dve, "bn_stats", [v], [stats], out=stats[:, c, :], in_=v[:, c * 512:(c + 1) * 512])
            k.op(dve, "bn_aggr", [stats], [mv], out=mv[:, 0:2], in_=stats[:])
            k.ts(dve, mv[:, 3:4], mv[:, 1:2], EPS, None, ALU.add, ALU.bypass, [mv], [mv])
            k.actv(mv[:, 3:4], mv[:, 3:4], AF.Sqrt, [mv], [mv])
            k.op(dve, "reciprocal", [mv], [mv], out=mv[:, 2:3], in_=mv[:, 3:4])
            k.ts(dve, res[:], v[:], mv[:, 0:1], mv[:, 2:3], ALU.subtract, ALU.mult, [v, mv], [res])
            k.tt(pool, res[:], res[:], gam[:], ALU.mult, [res, gam], [res])
            k.tt(pool, res[:], res[:], bet[:], ALU.add, [res, bet], [res])

        if stop_after >= 5 and 5 not in skip:
            with ExitStack() as st:
                wps = TT(k.sb(st, "wps", (128, 4, D), BF16))
                wpn = TT(k.sb(st, "wpn", (128, 4, D), BF16))
                wo = TT(k.sb(st, "wo", (128, 8, D), BF16))
                g1 = TT(k.sb(st, "g1", (128, D), F32)); b1 = TT(k.sb(st, "b1", (128, D), F32))
                wr = TT(k.sb(st, "wr", (128, 8, NEXP), F32)); br = TT(k.sb(st, "br", (128, NEXP), F32))
                iota = TT(k.sb(st, "iota", (128, NEXP), F32)); ecap = TT(k.sb(st, "ecap", (128, NEXP), F32))
                ustr = TT(k.sb(st, "ustr", (128, 128), F32)); ones = TT(k.sb(st, "ones", (128, 128), F32))
                cum = TT(k.sb(st, "cum", (128, NEXP), F32))
                k.dma(pool, wps[:], w_proj_sb.rearrange("(c p) n -> p c n", p=128), writes=[wps])
                k.dma(pool, wpn[:], w_proj_nsa.rearrange("(c p) n -> p c n", p=128), writes=[wpn])
                k.dma(pool, wo[:], w_out.rearrange("(kc p) n -> p kc n", p=128), writes=[wo])
                k.dma(sp, g1[:], ln1_g[0:1, :].broadcast_to((128, D)), writes=[g1])
                k.dma(sp, b1[:], ln1_b[0:1, :].broadcast_to((128, D)), writes=[b1])
                k.dma(sp, wr[:], w_router.rearrange("(kc p) e -> p kc e", p=128), writes=[wr])
                k.dma(sp, br[:], b_router[0:1, :].broadcast_to((128, NEXP)), writes=[br])
                k.dma(sp, iota[:], cst["iota32"], writes=[iota])
                k.dma(sp, ustr[:], cst["ustrict"], writes=[ustr])
                k.dma(sp, ones[:], cst["ones"], writes=[ones])
                k.ts(dve, ecap[:], iota[:], float(CAP), None, ALU.mult, ALU.bypass, [iota], [ecap])
                k.ms(pool, cum[:], 0.0, [cum])
                ystr = Ring([TT(k.sb(st, f"yst{i}", (128, 4, 512), BF16)) for i in range(1)])
                yntr = Ring([TT(k.sb(st, f"ynt{i}", (128, 4, 512), BF16)) for i in range(1)])
                mgtr = Ring([TT(k.sb(st, f"mgt{i}", (128, 16, 512), BF16)) for i in range(1)])
                mT = TT(k.sb(st, "mT", (128, 8, 512), BF16))
                t1r = Ring([TT(k.sb(st, f"mt1{i}", (128, 512), F32)) for i in range(2)])
                t2r = Ring([TT(k.sb(st, f"mt2{i}", (128, 512), F32)) for i in range(2)])
                xtr = Ring([TT(k.sb(st, f"xt{i}", (128, D), F32)) for i in range(2)])
                vt = TT(k.sb(st, "vt", (128, D), F32))
                h1r = Ring([TT(k.sb(st, f"h1{i}", (128, D), F32)) for i in range(2)])
                h1br = Ring([TT(k.sb(st, f"h1b{i}", (128, D), BF16)) for i in range(2)])
                h1T = TT(k.sb(st, "h1T", (128, 8, 128), F32))
                stats = TT(k.sb(st, "stats", (128, 2, 6), F32)); mv = TT(k.sb(st, "mv", (128, 4), F32))
                lg = TT(k.sb(st, "lg", (128, NEXP), F32)); msk = TT(k.sb(st, "msk", (128, NEXP), F32))
                v8 = TT(k.sb(st, "v8", (128, 8), F32)); i8 = TT(k.sb(st, "i8", (128, 8), U32))
                sm = TT(k.sb(st, "sm", (128, 8), F32)); idxf = TT(k.sb(st, "idxf", (128, 4), F32))
                dstf = TT(k.sb(st, "dstf", (128, NEXP), F32)); okm = TT(k.sb(st, "okm", (128, NEXP), F32))
                oh = TT(k.sb(st, "oh", (128, NEXP), F32)); dsel = TT(k.sb(st, "dsel", (128, 4), F32))
                psr = Ring([TT(k.ps(st, f"p5a{i}", (128, 512), F32)) for i in range(4)])
                ptr5 = Ring([TT(k.ps(st, f"p5t{i}", (128, 4, 128), F32)) for i in range(2)])
                pl = TT(k.ps(st, "p5l", (128, 512), F32))
                for g in range(NG):
                    gs = slice(g * 512, (g + 1) * 512)
                    yst = ystr.next(); ynt = yntr.next(); mgt = mgtr.next()
                    k.dma(sp, yst[:], YST[:, gs].rearrange("(c p) q -> p c q", p=128), writes=[yst])
                    k.dma(sp, ynt[:], YNT[:, gs].rearrange("(c p) q -> p c q", p=128), writes=[ynt])
                    k.dma(sp, mgt[:], MGT[:, gs].rearrange("(c p) q -> p c q", p=128), writes=[mgt])
                    for dc in range(8):
                        bs = psr.next(); bn = psr.next(); t1 = t1r.next(); t2 = t2r.next()
                        for hh in range(4):
                            k.mm(bs[:], wps[:, hh, dc * 128:(dc + 1) * 128], yst[:, hh, :], hh == 0, hh == 3, [wps, yst], [bs])
                        for hh in range(4):
                            k.mm(bn[:], wpn[:, hh, dc * 128:(dc + 1) * 128], ynt[:, hh, :], hh == 0, hh == 3, [wpn, ynt], [bn])
                        k.tt(dve, t1[:], bs[:], mgt[:, dc, :], ALU.mult, [bs, mgt], [t1])
                        k.tt(dve, t2[:], bn[:], mgt[:, 8 + dc, :], ALU.mult, [bn, mgt], [t2])
                        k.tt(pool, mT[:, dc, :], t1[:], t2[:], ALU.add, [t1, t2], [mT])
                    for tb in range(4):
                        tbg = g * 4 + tb
                        rows = slice(tbg * 128, (tbg + 1) * 128)
                        xt = xtr.next(); h1 = h1r.next(); h1b = h1br.next()
                        k.dma(sp, xt[:], x[rows, :], writes=[xt])
                        for half in range(2):
                            u = psr.next()
                            for dc in range(8):
                                k.mm(u[:], mT[:, dc, tb * 128:(tb + 1) * 128], wo[:, dc, half * 512:(half + 1) * 512], dc == 0, dc == 7, [mT, wo], [u])
                            k.op(dve, "scalar_tensor_tensor", [xt, u], [vt], out=vt[:, half * 512:(half + 1) * 512],
                                 in0=xt[:, half * 512:(half + 1) * 512], scalar=ALPHA, in1=u[:], op0=ALU.mult, op1=ALU.add)
                        layer_norm((stats, mv), vt, g1, b1, h1)
                        k.dma(sp, H1[rows, :], h1[:], reads=[h1])
                        k.actv(h1b[:], h1[:], AF.Copy, [h1], [h1b])
                        for q4 in range(2):
                            pt = ptr5.next()
                            for c in range(4):
                                kc = q4 * 4 + c
                                k.op(pe, "transpose", [h1, identf], [pt], out=pt[:, c, :], in_=h1[:, kc * 128:(kc + 1) * 128], identity=identf[:])
                            k.cp(dve, h1T[:, q4 * 4:(q4 + 1) * 4, :], pt[:], [pt], [h1T])
                        for kc in range(8):
                            k.mm(pl[:, 0:NEXP], h1T[:, kc, :], wr[:, kc, :], kc == 0, kc == 7, [h1T, wr], [pl])
                        k.tt(dve, lg[:], pl[:, 0:NEXP], br[:], ALU.add, [pl, br], [lg])
                        k.op(dve, "max", [lg], [v8], out=v8[:], in_=lg[:])
                        k.op(dve, "max_index", [v8, lg], [i8], out=i8[:], in_max=v8[:], in_values=lg[:])
                        k.ts(dve, msk[:], lg[:], v8[:, 3:4], None, ALU.is_ge, ALU.bypass, [lg, v8], [msk])
                        k.ts(dve, sm[:, 0:1], v8[:, 0:1], -1.0, None, ALU.mult, ALU.bypass, [v8], [sm])
                        k.actv(sm[:, 4:8], v8[:, 0:4], AF.Exp, [v8, sm], [sm], bias=sm[:, 0:1])
                        k.op(dve, "tensor_reduce", [sm], [sm], out=sm[:, 1:2], in_=sm[:, 4:8], axis=AX.X, op=ALU.add)
                        k.op(dve, "reciprocal", [sm], [sm], out=sm[:, 2:3], in_=sm[:, 1:2])
                        k.ts(dve, w4t[:, tbg, :], sm[:, 4:8], sm[:, 2:3], None, ALU.mult, ALU.bypass, [sm], [w4t])
                        k.mm(pl[:, 64:64 + NEXP], ustr[:], msk[:], True, False, [ustr, msk], [pl])
                        k.mm(pl[:, 64:64 + NEXP], ones[:], cum[:], False, True, [ones, cum], [pl])
                        k.tt(dve, dstf[:], pl[:, 64:64 + NEXP], ecap[:], ALU.add, [pl, ecap], [dstf])
                        k.ts(dve, okm[:], pl[:, 64:64 + NEXP], float(CAP) - 0.5, None, ALU.is_lt, ALU.bypass, [pl], [okm])
                        k.tt(dve, cum[:], cum[:], msk[:], ALU.add, [cum, msk], [cum])
                        k.ts(dve, dstf[:], dstf[:], -float(TRASH), None, ALU.add, ALU.bypass, [dstf], [dstf])
                        k.tt(dve, dstf[:], dstf[:], okm[:], ALU.mult, [dstf, okm], [dstf])
                        k.ts(dve, dstf[:], dstf[:], float(TRASH), None, ALU.add, ALU.bypass, [dstf], [dstf])
                        k.cp(dve, idxf[:], i8[:, 0:4], [i8], [idxf])
                        for kk in range(4):
                            k.ts(dve, oh[:], iota[:], idxf[:, kk:kk + 1], None, ALU.is_equal, ALU.bypass, [iota, idxf], [oh])
                            k.tt(dve, oh[:], oh[:], dstf[:], ALU.mult, [oh, dstf], [oh])
                            k.op(dve, "tensor_reduce", [oh], [dsel], out=dsel[:, kk:kk + 1], in_=oh[:], axis=AX.X, op=ALU.add)
                        k.cp(dve, destt[:, tbg, :], dsel[:], [dsel], [destt])
                        for kk in range(4):
                            k.idma(XBUF[:, :], bass.IndirectOffsetOnAxis(ap=destt[:, tbg, kk:kk + 1], axis=0), h1b[:], None,
                                   reads=[h1b, destt])
                k.barrier()

        if stop_after >= 6 and 6 not in skip:
            with ExitStack() as st:
                bguT = TT(k.sb(st, "bguT", (128, 16, NEXP), F32))
                with ExitStack() as st2:
                    bgs = TT(k.sb(st2, "bgs", (NEXP, 2 * D), F32))
                    gtmp = Ring([TT(k.ps(st2, f"p6b{i}", (128, 512), F32)) for i in range(2)])
                    k.dma(sp, bgs[:], b_gate_up, writes=[bgs])
                    for c in range(16):
                        pt = gtmp.next()
                        k.op(pe, "transpose", [bgs, identf], [pt], out=pt[:, 0:NEXP], in_=bgs[:, c * 128:(c + 1) * 128], identity=identf[0:NEXP, 0:NEXP])
                        k.cp(dve, bguT[:, c, :], pt[:, 0:NEXP], [pt], [bguT])
                    k.barrier()
                wgur = Ring([TT(k.sb(st, f"wgu{i}", (128, 8, 2 * D), BF16)) for i in range(2)])
                wdr = Ring([TT(k.sb(st, f"wd{i}", (128, 8, D), BF16)) for i in range(2)])
                bdr = Ring([TT(k.sb(st, f"bd{i}", (128, D), F32)) for i in range(2)])
                stg = Ring([TT(k.sb(st, f"stg{i}", (128, D), F32)) for i in range(4)])
                xrr = Ring([TT(k.sb(st, f"xr{i}", (128, D), BF16)) for i in range(3)])
                xTs = [TT(k.sb(st, f"exT{i}", (128, 8, 512), BF16)) for i in range(2)]
                hTs = [TT(k.sb(st, f"ehT{i}", (128, 8, 512), BF16)) for i in range(2)]
                ggr = Ring([TT(k.sb(st, f"gg{i}", (128, 512), F32)) for i in range(2)])
                sgr = Ring([TT(k.sb(st, f"sg{i}", (128, 512), F32)) for i in range(2)])
                uur = Ring([TT(k.sb(st, f"uu{i}", (128, 512), F32)) for i in range(2)])
                orr = Ring([TT(k.sb(st, f"orow{i}", (128, D), F32)) for i in range(2)])
                ptr6 = Ring([TT(k.ps(st, f"p6t{i}", (128, 8, 128), BF16)) for i in range(2)])
                gur = Ring([TT(k.ps(st, f"p6g{i}", (128, 512), F32)) for i in range(4)])
                opr = Ring([TT(k.ps(st, f"p6o{i}", (128, 512), F32)) for i in range(2)])
                rgs = [(r0, min(512, CAP - r0)) for r0 in range(0, CAP, 512)]
                NR = len(rgs)

                def wjobs(e):
                    wgu = wgur.next(); wd = wdr.next(); bd = bdr.next()
                    jobs = []
                    for kc in range(8):
                        for hf in range(2):
                            jobs.append((wgu[:, kc, hf * D:(hf + 1) * D], w_gate_up[e, kc * 128:(kc + 1) * 128, hf * D:(hf + 1) * D], wgu))
                    for kc in range(8):
                        jobs.append((wd[:, kc, :], w_down[e, kc * 128:(kc + 1) * 128, :], wd))
                    return (wgu, wd, bd), jobs

                def run_loads(jobs):
                    pend = []
                    for (dst, src_ap, wt) in jobs:
                        sg_t = stg.next()
                        k.dma(sp, sg_t[:], src_ap, writes=[sg_t])
                        pend.append((dst, sg_t, wt))
                    return pend

                def run_casts(pend):
                    for (dst, sg_t, wt) in pend:
                        k.actv(dst, sg_t[:], AF.Copy, [sg_t], [wt])

                def chunks(lst, n):
                    per = (len(lst) + n - 1) // n
                    return [lst[i * per:(i + 1) * per] for i in range(n)]

                steps = [(e, ri) for e in range(NEXP) for ri in range(NR)]
                ws = {}
                ws[0], jobs0 = wjobs(0)
                for j0 in range(0, len(jobs0), 4):
                    run_casts(run_loads(jobs0[j0:j0 + 4]))
                k.dma(sp, ws[0][2][:], b_down[0:1, :].broadcast_to((128, D)), writes=[ws[0][2]])
                evc = [0]

                def stX(t):
                    e, ri = steps[t]
                    r0, nr = rgs[ri]
                    row0 = e * CAP + r0
                    xT = xTs[t % 2]
                    for b in range(nr // 128):
                        xr = xrr.next()
                        k.dma(act, xr[:], XBUF[row0 + b * 128:row0 + (b + 1) * 128, :], writes=[xr])
                        pt = ptr6.next()
                        for kc in range(8):
                            k.op(pe, "transpose", [xr, identb], [pt], out=pt[:, kc, :], in_=xr[:, kc * 128:(kc + 1) * 128], identity=identb[:])
                        evac(evc[0], xT[:, :, b * 128:(b + 1) * 128], pt[:], [pt], [xT]); evc[0] += 1

                def stGU(t, fcs):
                    e, ri = steps[t]
                    r0, nr = rgs[ri]
                    wgu = ws[e][0]
                    xT = xTs[t % 2]; hT = hTs[t % 2]
                    for fc in fcs:
                        gp = gur.next(); up = gur.next(); gg = ggr.next(); sg_ = sgr.next(); uu = uur.next()
                        for kc in range(8):
                            k.mm(gp[:, 0:nr], wgu[:, kc, fc * 128:(fc + 1) * 128], xT[:, kc, 0:nr], kc == 0, kc == 7, [wgu, xT], [gp])
                        for kc in range(8):
                            k.mm(up[:, 0:nr], wgu[:, kc, D + fc * 128:D + (fc + 1) * 128], xT[:, kc, 0:nr], kc == 0, kc == 7, [wgu, xT], [up])
                        k.ts(dve, gg[:, 0:nr], gp[:, 0:nr], bguT[:, fc, e:e + 1], 7.0, ALU.add, ALU.min, [gp, bguT], [gg])
                        k.actv(sg_[:, 0:nr], gg[:, 0:nr], AF.Sigmoid, [gg], [sg_], scale=1.702)
                        k.ts(dve, uu[:, 0:nr], up[:, 0:nr], bguT[:, 8 + fc, e:e + 1], 7.0, ALU.add, ALU.min, [up, bguT], [uu])
                        k.ts(dve, uu[:, 0:nr], uu[:, 0:nr], -7.0, 1.0, ALU.max, ALU.add, [uu], [uu])
                        k.tt(pool, gg[:, 0:nr], gg[:, 0:nr], sg_[:, 0:nr], ALU.mult, [gg, sg_], [gg])
                        k.tt(dve, hT[:, fc, 0:nr], gg[:, 0:nr], uu[:, 0:nr], ALU.mult, [gg, uu], [hT])

                def stDN(t):
                    e, ri = steps[t]
                    r0, nr = rgs[ri]
                    row0 = e * CAP + r0
                    wd, bd = ws[e][1], ws[e][2]
                    hT = hTs[t % 2]
                    for b in range(nr // 128):
                        orow = orr.next()
                        for half in range(2):
                            op_ = opr.next()
                            for fc in range(8):
                                k.mm(op_[:], hT[:, fc, b * 128:(b + 1) * 128], wd[:, fc, half * 512:(half + 1) * 512], fc == 0, fc == 7, [hT, wd], [op_])
                            k.tt(dve, orow[:, half * 512:(half + 1) * 512], op_[:], bd[:, half * 512:(half + 1) * 512], ALU.add, [op_, bd], [orow])
                        k.dma(sp, OBUF[row0 + b * 128:row0 + (b + 1) * 128, :], orow[:], reads=[orow])

                stX(0)
                jparts = None
                pendB = []
                for t, (e, ri) in enumerate(steps):
                    stGU(t, range(0, 4))
                    if t > 0:
                        stDN(t - 1)
                    run_casts(pendB); pendB = []
                    if ri == 0:
                        if e + 1 < NEXP:
                            ws[e + 1], njobs = wjobs(e + 1)
                            k.dma(sp, ws[e + 1][2][:], b_down[e + 1:e + 2, :].broadcast_to((128, D)), writes=[ws[e + 1][2]])
                            jparts = chunks(njobs, NR)
                        else:
                            jparts = [[] for _ in range(NR)]
                    jl = jparts[ri]
                    pendA = run_loads(jl[0:4])
                    if t + 1 < len(steps):
                        stX(t + 1)
                    stGU(t, range(4, 8))
                    run_casts(pendA)
                    pendB = run_loads(jl[4:8])
                    for j0 in range(8, len(jl), 4):
                        run_casts(pendB)
                        pendB = run_loads(jl[j0:j0 + 4])
                    if ri == NR - 1:
                        run_casts(pendB); pendB = []
                stDN(len(steps) - 1)
                k.barrier()

        if stop_after >= 7 and 7 not in skip:
            with ExitStack() as st:
                g2 = TT(k.sb(st, "g2", (128, D), F32)); b2 = TT(k.sb(st, "b2", (128, D), F32))
                k.dma(sp, g2[:], ln2_g[0:1, :].broadcast_to((128, D)), writes=[g2])
                k.dma(sp, b2[:], ln2_b[0:1, :].broadcast_to((128, D)), writes=[b2])
                rkr = Ring([TT(k.sb(st, f"rk{i}", (128, D), F32)) for i in range(8)])
                h1r = Ring([TT(k.sb(st, f"h7{i}", (128, D), F32)) for i in range(2)])
                yr = Ring([TT(k.sb(st, f"y7{i}", (128, D), F32)) for i in range(2)])
                rr = Ring([TT(k.sb(st, f"r7{i}", (128, D), F32)) for i in range(2)])
                stats = TT(k.sb(st, "stats7", (128, 2, 6), F32)); mv = TT(k.sb(st, "mv7", (128, 4), F32))
                for tb in range(NTB):
                    rows = slice(tb * 128, (tb + 1) * 128)
                    h1 = h1r.next(); y = yr.next(); res = rr.next()
                    k.dma(sp, h1[:], H1[rows, :], writes=[h1])
                    rks = []
                    for kk in range(4):
                        rk = rkr.next()
                        k.idma(rk[:], None, OBUF[:, :], bass.IndirectOffsetOnAxis(ap=destt[:, tb, kk:kk + 1], axis=0),
                               reads=[destt], writes=[rk])
                        rks.append(rk)
                    k.ts(dve, y[:], rks[0][:], w4t[:, tb, 0:1], None, ALU.mult, ALU.bypass, [rks[0], w4t], [y])
                    for kk in range(1, 4):
                        k.op(dve, "scalar_tensor_tensor", [rks[kk], w4t, y], [y], out=y[:], in0=rks[kk][:], scalar=w4t[:, tb, kk:kk + 1],
                             in1=y[:], op0=ALU.mult, op1=ALU.add)
                    k.op(dve, "scalar_tensor_tensor", [h1, y], [y], out=y[:], in0=h1[:], scalar=ALPHA, in1=y[:], op0=ALU.mult, op1=ALU.add)
                    layer_norm((stats, mv), y, g2, b2, res)
                    k.dma(sp, out[rows, :], res[:], reads=[res])
                k.barrier()

        k.barrier()
        k.emit()
    return nc


def make_in_map(xb, p, consts, fm, tm):
    w_in = p["w_in"][0]
    im = {"x": np.ascontiguousarray(xb), "wfm": np.ascontiguousarray(w_in[:, fm]), "wtm": np.ascontiguousarray(w_in[:, tm])}
    for kk, v in consts.items():
        im["c_" + kk] = v
    for nm in ("cmp_pe_k", "cmp_w1_k", "cmp_w2_k", "cmp_pe_v", "cmp_w1_v", "cmp_w2_v", "w_proj_sb", "w_proj_nsa", "w_out",
               "w_router", "w_gate_up", "b_gate_up", "w_down", "b_down"):
        im[nm] = np.ascontiguousarray(p[nm][0])
    for nm in ("ln1_g", "ln1_b", "ln2_g", "ln2_b", "b_router"):
        im[nm] = np.ascontiguousarray(p[nm][0:1])
    return im


_NC_CACHE = {}


def kernel(**inputs):
    p = {k_: np.asarray(v, dtype=np.float32) for k_, v in inputs.items()}
    x = p["x"]
    B, S, _ = x.shape
    if S not in _NC_CACHE:
        _NC_CACHE[S] = build(S=S, CAP=(1536 if S == 8192 else max(256, S // 4)))
    nc = _NC_CACHE[S]
    consts = host_consts(S)
    fm, tm = win_layout()
    in_maps = [make_in_map(x[b], p, consts, fm, tm) for b in range(B)]
    res = run_bass_kernel_spmd(nc, in_maps, core_ids=list(range(B)))
    return np.stack([np.asarray(r["out"], dtype=np.float32) for r in res.results], axis=0)
```
